# Optimizing a Trainium2 kernel written in Bass

```python
import jax, jax.numpy as jnp
from jax import lax
import numpy as np

D_MODEL = 1024
BATCH = 4
SEQ = 4096
DEPTH = 1

N_ATT_HEADS = 8
HEAD_DIM = 64
ATT_WIDTH = N_ATT_HEADS * HEAD_DIM
Q_BLOCK = 128
CONV_WIDTH = 512
CONV_K = 3
N_GROUPS = 4
EXPERTS_PER_GROUP = 4
N_EXPERTS = N_GROUPS * EXPERTS_PER_GROUP
TOP_K_IN_GROUP = 2
D_EXPERT = 256
PLE_DIM = 256
EPS = 1e-6
NEG_INF = -1e30

PROJ_SIZES = (ATT_WIDTH, ATT_WIDTH, ATT_WIDTH, N_ATT_HEADS,
              CONV_WIDTH, CONV_WIDTH, CONV_WIDTH, D_MODEL, D_MODEL)
PROJ_WIDTH = 3 * ATT_WIDTH + N_ATT_HEADS + 3 * CONV_WIDTH + 2 * D_MODEL

kernel_name = "hybrid_fox_shortconv_hmoe_block"


def rms_norm(x, g):
    xf = x.astype(jnp.float32)
    y = xf * lax.rsqrt(jnp.mean(xf * xf, axis=-1, keepdims=True) + EPS)
    return (y * g.astype(jnp.float32)).astype(x.dtype)


def split_offsets():
    offs, acc = [], 0
    for s in PROJ_SIZES[:-1]:
        acc += s
        offs.append(acc)
    return offs


def forgetting_attention(q, k, v, log_f):
    b, s, h, d = q.shape
    n_blocks = s // Q_BLOCK
    c = jnp.cumsum(log_f, axis=1).transpose(0, 2, 1)
    qh = q.transpose(0, 2, 1, 3)
    kh = k.transpose(0, 2, 1, 3)
    vh = v.transpose(0, 2, 1, 3)
    q_blocks = jnp.moveaxis(qh.reshape(b, h, n_blocks, Q_BLOCK, d), 2, 0)
    c_blocks = jnp.moveaxis(c.reshape(b, h, n_blocks, Q_BLOCK), 2, 0)
    key_pos = jnp.arange(s)
    scale = HEAD_DIM ** -0.5

    def one_block(args):
        qb, cqb, blk = args
        q_pos = blk * Q_BLOCK + jnp.arange(Q_BLOCK)
        logits = jnp.einsum('bhqd,bhkd->bhqk', qb, kh).astype(jnp.float32) * scale
        logits = logits + (cqb[..., :, None] - c[..., None, :])
        causal = key_pos[None, :] <= q_pos[:, None]
        logits = jnp.where(causal, logits, NEG_INF)
        probs = jax.nn.softmax(logits, axis=-1)
        return jnp.einsum('bhqk,bhkd->bhqd', probs.astype(vh.dtype), vh)

    out = lax.map(one_block, (q_blocks, c_blocks, jnp.arange(n_blocks)))
    out = out.transpose(1, 0, 3, 2, 4).reshape(b, s, h * d)
    return out


def causal_short_conv(u, w):
    s = u.shape[1]
    u_pad = jnp.pad(u, ((0, 0), (CONV_K - 1, 0), (0, 0)))
    y = w[0] * u_pad[:, 0:s]
    for j in range(1, CONV_K):
        y = y + w[j] * u_pad[:, j:j + s]
    return y


def hierarchical_moe(h, w_rg, b_rg, w_re, b_re, w_gate, w_up, w_down):
    b, s, d = h.shape
    ht = h.reshape(b * s, d)
    g_logits = (ht @ w_rg + b_rg).astype(jnp.float32)
    g_prob = jax.nn.softmax(g_logits, axis=-1)
    g_val, g_idx = lax.top_k(g_prob, 1)
    e_logits = (ht @ w_re + b_re).astype(jnp.float32)
    e_logits = e_logits.reshape(-1, N_GROUPS, EXPERTS_PER_GROUP)
    e_in = jnp.take_along_axis(e_logits, g_idx[:, :, None], axis=1)[:, 0]
    e_prob = jax.nn.softmax(e_in, axis=-1)
    e_val, e_idx = lax.top_k(e_prob, TOP_K_IN_GROUP)
    e_val = e_val / jnp.sum(e_val, axis=-1, keepdims=True)
    gate = g_val * e_val
    global_idx = g_idx * EXPERTS_PER_GROUP + e_idx
    combine = jnp.sum(jax.nn.one_hot(global_idx, N_EXPERTS, dtype=jnp.float32)
                      * gate[..., None], axis=1)
    a = jnp.einsum('td,edf->tef', ht, w_gate)
    u = jnp.einsum('td,edf->tef', ht, w_up)
    hid = jax.nn.silu(a) * u * combine[..., None].astype(ht.dtype)
    out = jnp.einsum('tef,efd->td', hid, w_down)
    return out.reshape(b, s, d)


def setup_inputs(seed: int = 0) -> dict:
    key = jax.random.key(seed)
    ks = jax.random.split(key, 24)
    L, D = DEPTH, D_MODEL
    f32 = jnp.float32

    def nrm(k, shape, scale):
        return jax.random.normal(k, shape, f32) * scale

    def gain(k, shape):
        return 1.0 + 0.05 * jax.random.normal(k, shape, f32)

    return {
        "x": nrm(ks[0], (BATCH, SEQ, D), 1.0),
        "p": nrm(ks[1], (DEPTH, BATCH, SEQ, PLE_DIM), 1.0),
        "attn_norm_g": gain(ks[2], (L, D)),
        "w_in": nrm(ks[3], (L, D, PROJ_WIDTH), D ** -0.5),
        "b_f": 1.0 + 3.0 * jax.random.uniform(ks[4], (L, N_ATT_HEADS), f32),
        "q_norm_g": gain(ks[5], (L, HEAD_DIM)),
        "k_norm_g": gain(ks[6], (L, HEAD_DIM)),
        "conv_w": nrm(ks[7], (L, CONV_K, CONV_WIDTH), CONV_K ** -0.5),
        "w_out_att": nrm(ks[8], (L, ATT_WIDTH, D), ATT_WIDTH ** -0.5),
        "w_out_conv": nrm(ks[9], (L, CONV_WIDTH, D), CONV_WIDTH ** -0.5),
        "w_o": nrm(ks[10], (L, D, D), D ** -0.5),
        "ffn_norm_g": gain(ks[11], (L, D)),
        "w_rg": nrm(ks[12], (L, D, N_GROUPS), D ** -0.5),
        "b_rg": nrm(ks[13], (L, N_GROUPS), 0.01),
        "w_re": nrm(ks[14], (L, D, N_EXPERTS), D ** -0.5),
        "b_re": nrm(ks[15], (L, N_EXPERTS), 0.01),
        "w_gate": nrm(ks[16], (L, N_EXPERTS, D, D_EXPERT), D ** -0.5),
        "w_up": nrm(ks[17], (L, N_EXPERTS, D, D_EXPERT), D ** -0.5),
        "w_down": nrm(ks[18], (L, N_EXPERTS, D_EXPERT, D), D_EXPERT ** -0.5),
        "ple_norm_g": gain(ks[19], (L, D)),
        "w_pg": nrm(ks[20], (L, D, D), D ** -0.5),
        "w_ple": nrm(ks[21], (L, PLE_DIM, D), PLE_DIM ** -0.5),
    }


def reference(x, p, attn_norm_g, w_in, b_f, q_norm_g, k_norm_g, conv_w, w_out_att,
              w_out_conv, w_o, ffn_norm_g, w_rg, b_rg, w_re, b_re, w_gate, w_up, w_down,
              ple_norm_g, w_pg, w_ple):
    b, s, _ = x.shape
    offs = split_offsets()
    for i in range(DEPTH):
        h = rms_norm(x, attn_norm_g[i])
        proj = h @ w_in[i]
        q, k, v, f_logit, cb, cc, cu, ga, gb = jnp.split(proj, offs, axis=-1)

        q = rms_norm(q.reshape(b, s, N_ATT_HEADS, HEAD_DIM), q_norm_g[i])
        k = rms_norm(k.reshape(b, s, N_ATT_HEADS, HEAD_DIM), k_norm_g[i])
        v = v.reshape(b, s, N_ATT_HEADS, HEAD_DIM)
        log_f = jax.nn.log_sigmoid(f_logit.astype(jnp.float32) + b_f[i].astype(jnp.float32))
        y_att = forgetting_attention(q, k, v, log_f)

        y_conv = cb * causal_short_conv(cc * cu, conv_w[i])

        merged = (jax.nn.sigmoid(ga) * (y_att @ w_out_att[i])
                  + jax.nn.sigmoid(gb) * (y_conv @ w_out_conv[i]))
        x = x + merged @ w_o[i]

        h2 = rms_norm(x, ffn_norm_g[i])
        x = x + hierarchical_moe(h2, w_rg[i], b_rg[i], w_re[i], b_re[i],
                                 w_gate[i], w_up[i], w_down[i])

        h3 = rms_norm(x, ple_norm_g[i])
        x = x + jax.nn.sigmoid(h3 @ w_pg[i]) * (p[i] @ w_ple[i])
    return x
```

```python
import numpy as np
import ml_dtypes
import concourse.bass as bass
import concourse.mybir as mybir
from concourse.bass_utils import run_bass_kernel_spmd

F32 = mybir.dt.float32
BF16 = mybir.dt.bfloat16
AF = mybir.ActivationFunctionType
ALU = mybir.AluOpType
AX = mybir.AxisListType

ENGS = ("pe", "act", "dve", "pool", "sp")
NDMASEM = 20
SB_BASE = 16512
SB_END = 229376 - 2048
EPS = 1e-6

D = 1024
S = 4096
NH = 8
HD = 64
NTOK = 2048
NE = 16
DE = 256
CHUNKS = ((0, 3, 4, 7), (1, 2, 5, 6))


class Tk:
    __slots__ = ("sem", "val")

    def __init__(self, sem, val):
        self.sem = sem
        self.val = val


class Buf:
    __slots__ = ("name", "w", "r")

    def __init__(self, name=""):
        self.name = name
        self.w = None
        self.r = []


class Prog:
    def __init__(self, nc):
        self.nc = nc
        self.ops = {e: [] for e in ENGS}
        self.cnt = {e: 0 for e in ENGS}
        self.seen = {e: {} for e in ENGS}
        self.pend = {e: [] for e in ENGS}
        self.dma_n = {e: 0 for e in ENGS}
        self.dma_last = {}

    def _need(self, eng, waits, t):
        if t is None:
            return
        if t.sem == "pe" and eng == "pe":
            return
        if t.val is None:
            raise RuntimeError("dependency on op without resolved ticket (missing inc)")
        if self.seen[eng].get(t.sem, 0) >= t.val:
            return
        if waits.get(t.sem, 0) < t.val:
            waits[t.sem] = t.val

    def _deps(self, eng, reads, writes, waits):
        for b in reads:
            self._need(eng, waits, b.w)
        for b in writes:
            self._need(eng, waits, b.w)
            for t in b.r:
                self._need(eng, waits, t)
        for s, v in waits.items():
            self.seen[eng][s] = v

    def _mark(self, tk, reads, writes):
        for b in reads:
            b.r.append(tk)
            if len(b.r) > 64:
                b.r = b.r[-48:]
        for b in writes:
            b.w = tk
            b.r = []

    def op(self, eng, fn, reads=(), writes=(), inc=True):
        waits = {}
        self._deps(eng, reads, writes, waits)
        if inc:
            self.cnt[eng] += 1
            tk = Tk(eng, self.cnt[eng])
            for p in self.pend[eng]:
                p.val = tk.val
            self.pend[eng] = []
        else:
            tk = Tk(eng, None)
            self.pend[eng].append(tk)
        self._mark(tk, reads, writes)
        self.ops[eng].append((fn, list(waits.items()), (eng, 1) if inc else None))
        return tk

    def dma(self, eng, fn, reads=(), writes=()):
        n = self.dma_n[eng]
        self.dma_n[eng] += 1
        semname = "d_%s_%d" % (eng, n % NDMASEM)
        waits = {}
        prev = self.dma_last.get(semname)
        if prev is not None:
            self._need(eng, waits, prev)
        self._deps(eng, reads, writes, waits)
        tk = Tk(semname, 16 * (n // NDMASEM + 1))
        self.dma_last[semname] = tk
        self._mark(tk, reads, writes)
        self.ops[eng].append((fn, list(waits.items()), (semname, 16)))
        return tk

    def barrier(self):
        for e in ENGS:
            assert not self.pend[e], "barrier with pending un-inc'ed ops on " + e
        tks = [Tk(e, self.cnt[e]) for e in ENGS if self.cnt[e] > 0]
        tks += list(self.dma_last.values())
        for e in ENGS:
            waits = {}
            for t in tks:
                if t.sem != e:
                    self._need(e, waits, t)
            for s, v in waits.items():
                self.seen[e][s] = v
            self.ops[e].append((None, list(waits.items()), None))

    def emit(self):
        nc = self.nc
        from contextlib import ExitStack
        semnames = set()
        for e in ENGS:
            for fn, waits, inc in self.ops[e]:
                for s, v in waits:
                    semnames.add(s)
                if inc:
                    semnames.add(inc[0])
        with ExitStack() as st:
            sems = {}
            for s in sorted(semnames):
                sems[s] = st.enter_context(nc.semaphore("s_" + s))
            block = st.enter_context(nc.Block())

            def run(e, engobj):
                for fn, waits, inc in self.ops[e]:
                    for s, v in waits:
                        engobj.wait_ge(sems[s], v)
                    if fn is None:
                        continue
                    ins = fn(engobj)
                    if inc:
                        ins.then_inc(sems[inc[0]], inc[1])

            @block.tensor
            def _(eng):
                run("pe", eng)

            @block.scalar
            def _(eng):
                run("act", eng)

            @block.vector
            def _(eng):
                run("dve", eng)

            @block.gpsimd
            def _(eng):
                run("pool", eng)

            @block.sync
            def _(eng):
                run("sp", eng)


class Arena:
    def __init__(self, nc):
        self.nc = nc
        self.off = SB_BASE
        self.n = 0

    def mark(self):
        return self.off

    def release(self, m):
        self.off = m

    def alloc(self, shape, dt):
        nbytes = int(np.prod(shape[1:])) * (4 if dt == F32 else 2)
        nbytes = (nbytes + 63) // 64 * 64
        assert self.off + nbytes <= SB_END, "SBUF overflow: %d + %d" % (self.off, nbytes)
        self.n += 1
        t = self.nc.alloc_sbuf_tensor_at("t%d" % self.n, list(shape), dt, offset=self.off)
        self.off += nbytes
        return t


def bc_mid(ap2, n):
    p, a = ap2.shape
    return ap2.unsqueeze(2).to_broadcast([p, a, n])


def build(stage=99, nkv=32, nq=16):
    nc = bass.Bass("TRN2", target_bir_lowering=False)
    P = Prog(nc)
    A = Arena(nc)

    def finish_debug(items):
        P.barrier()
        for name, ap, shape, dt in items:
            o = nc.dram_tensor(name, list(shape), dt, kind="ExternalOutput").ap()
            P.dma("sp", lambda e, o=o, ap=ap: e.dma_start(out=o, in_=ap))
        P.barrier()
        P.emit()
        return nc

    def din(name, shape, dt=F32):
        return nc.dram_tensor(name, list(shape), dt, kind="ExternalInput").ap()

    xT_all = din("xT_all", [D, S])
    xT_own = din("xT_own", [D, NTOK])
    xhT = din("xhT", [D, 8])
    pT_own = din("pT_own", [256, NTOK])
    w_in = din("w_in", [D, 5128])
    w_oa = din("w_oa", [512, D])
    w_oc = din("w_oc", [512, D])
    w_o = din("w_o", [D, D])
    w_r = din("w_r", [D, 20])
    w_gate = din("w_gate", [NE, D, DE])
    w_up = din("w_up", [NE, D, DE])
    w_down = din("w_down", [NE, DE, D])
    w_pg = din("w_pg", [D, D])
    w_ple = din("w_ple", [256, D])
    g1T_d = din("g1T", [128, 8])
    g2T_d = din("g2T", [128, 8])
    g3T_d = din("g3T", [128, 8])
    bf_d = din("bf_bc", [128, 8])
    gq_d = din("gq_col", [64, 1])
    gk_d = din("gk_col", [64, 1])
    conv_d = din("convT", [128, 4, 3])
    br_d = din("br_bc", [128, 20])
    sel_d = din("sel_own", [128, 16, 32])
    mask_d = din("mask2", [128, 2, 8, 512], BF16)
    if stage == 2:
        dbg = nc.dram_tensor("dbg", [64, 8, NTOK], BF16, kind="ExternalOutput").ap()
    elif stage == 99:
        outT = nc.dram_tensor("outT", [D, NTOK], F32, kind="ExternalOutput").ap()

    import os
    ps = [nc.alloc_psum_tensor("ps%d" % i, [128, 512], F32) for i in range(int(os.environ.get("NPS", "8")))]
    Bps = [Buf("ps%d" % i) for i in range(len(ps))]

    ident = A.alloc([128, 128], BF16)
    tmpf = A.alloc([128, 128], F32)
    tri = A.alloc([128, 128], F32)
    Emat = A.alloc([128, 128], F32)
    ones_bf = A.alloc([128, 128], BF16)
    ones_f = A.alloc([128, 64], F32)
    g1T = A.alloc([128, 8], F32)
    g2T = A.alloc([128, 8], F32)
    g3T = A.alloc([128, 8], F32)
    bf_bc = A.alloc([128, 8], F32)
    gqkT = A.alloc([65, 1], F32)
    gk_t = A.alloc([64, 1], F32)
    Bconst = Buf("const")
    Btmpf = Buf("tmpf")

    P.op("pool", lambda e: e.memset(tmpf[:], 1.0), writes=[Btmpf])
    P.op("pool", lambda e: e.affine_select(out=tmpf[:], in_=tmpf[:], pattern=[[-1, 128]], compare_op=ALU.is_equal,
                                           fill=0.0, base=0, channel_multiplier=1), reads=[Btmpf], writes=[Btmpf])
    P.op("dve", lambda e: e.tensor_copy(out=ident[:], in_=tmpf[:]), reads=[Btmpf], writes=[Bconst])
    P.op("pool", lambda e: e.memset(tri[:], 1.0), writes=[Bconst])
    P.op("pool", lambda e: e.affine_select(out=tri[:], in_=tri[:], pattern=[[1, 128]], compare_op=ALU.is_ge,
                                           fill=0.0, base=0, channel_multiplier=-1), reads=[Bconst], writes=[Bconst])
    P.op("pool", lambda e: e.memset(Emat[:], 1.0), writes=[Bconst])
    P.op("pool", lambda e: e.affine_select(out=Emat[:], in_=Emat[:], pattern=[[0, 128]], compare_op=ALU.is_equal,
                                           fill=0.0, base=-127, channel_multiplier=1), reads=[Bconst], writes=[Bconst])
    P.op("pool", lambda e: e.memset(ones_bf[:], 1.0), writes=[Bconst])
    P.op("pool", lambda e: e.memset(ones_f[:], 1.0), writes=[Bconst])
    P.op("pool", lambda e: e.memset(gqkT[:], 1.0), writes=[Bconst])
    P.dma("sp", lambda e: e.dma_start(out=gqkT[0:64, :], in_=gq_d), writes=[Bconst])
    for dst, src in ((g1T, g1T_d), (g2T, g2T_d), (g3T, g3T_d), (bf_bc, bf_d), (gk_t, gk_d)):
        P.dma("sp", lambda e, dst=dst, src=src: e.dma_start(out=dst[:], in_=src), writes=[Bconst])
    P.op("dve", lambda e: e.scalar_tensor_tensor(out=gqkT[0:64, :], in0=gqkT[0:64, :], scalar=HD ** -0.5, in1=gk_t[:],
                                                 op0=ALU.mult, op1=ALU.mult), reads=[Bconst], writes=[Bconst])
    P.barrier()
    if stage == "c":
        return finish_debug([("tri", tri[:], [128, 128], F32), ("Emat", Emat[:], [128, 128], F32),
                             ("ident", ident[:], [128, 128], BF16), ("gqk", gqkT[:], [65, 1], F32)])

    C0 = A.mark()
    yT = A.alloc([64, NH, NTOK], BF16)
    ByT = [Buf("yT%d" % j) for j in range(4)]
    m_attn = A.mark()
    KT = A.alloc([65, NH, S], BF16)
    QT = A.alloc([65, NH, NTOK], BF16)
    V = A.alloc([128, 32, NH, 65], BF16)
    negc = A.alloc([128, 32, NH], F32)
    BKT = [Buf("KT%d" % i) for i in range(32)]
    BQT = [Buf("QT%d" % i) for i in range(16)]
    BV = [Buf("V%d" % i) for i in range(32)]
    Bnegc = [Buf("negc%d" % i) for i in range(32)]

    m_p1 = A.mark()
    A.release(C0)
    Wqkv = A.alloc([128, 8, 1536], BF16)
    Wf = A.alloc([128, 8, 8], BF16)
    sqk = [A.alloc([128, 512], F32) for _ in range(2)]
    assert A.off <= C0 + 32768
    A.release(m_p1)
    wst = [A.alloc([128, 8, 256], F32) for _ in range(2)]
    Bwst = [Buf("wst0"), Buf("wst1")]
    BW = Buf("Wqkv")
    xst = [A.alloc([128, 8, 128], F32) for _ in range(2)]
    xb = [A.alloc([128, 8, 128], BF16) for _ in range(2)]
    sq = [A.alloc([128, 8, 128], BF16) for _ in range(2)]
    Bxst = [Buf("xst0"), Buf("xst1")]
    Bxb = [Buf("xb0"), Buf("xb1")]
    Bsq = [Buf("sq0"), Buf("sq1")]
    Bsqk = [Buf("sqk0"), Buf("sqk1")]
    Kaug = [A.alloc([128, NH, 65], BF16) for _ in range(2)]
    BKaug = [Buf("Kaug0"), Buf("Kaug1")]
    tmpq = A.alloc([128, 512], F32)
    Btmpq = Buf("tmpq")
    selw = A.alloc([128, 16, 32], F32)
    seltmp = A.alloc([128, 32, NH], F32)
    Bseltmp = Buf("seltmp")
    NSM = 4
    sm = [A.alloc([128, 64], F32) for _ in range(NSM)]
    Bsm = [Buf("sm%d" % i) for i in range(NSM)]

    w_in_v = w_in.rearrange("(k p) n -> p k n", p=128)
    BWq, BWk, BWv, BWf = Buf("Wq"), Buf("Wk"), Buf("Wv"), Buf("Wf")
    wpiece_n = [0]

    def load_piece(piece, Bdst):
        n_ = wpiece_n[0]
        wpiece_n[0] += 1
        wb = wst[n_ % 2]
        P.dma("sp", lambda e: e.dma_start(out=wb[:], in_=w_in_v[:, :, piece * 256:(piece + 1) * 256]), writes=[Bwst[n_ % 2]])
        for kc in range(8):
            P.op("act", lambda e, kc=kc: e.activation(out=Wqkv[:, kc, piece * 256:(piece + 1) * 256], in_=wb[:, kc, :], func=AF.Copy,
                                                      scale=g1T[:, kc:kc + 1]), reads=[Bwst[n_ % 2], Bconst], writes=[Bdst])

    def load_wf():
        n_ = wpiece_n[0]
        wpiece_n[0] += 1
        wb = wst[n_ % 2]
        P.dma("sp", lambda e: e.dma_start(out=wb[:, :, 0:8], in_=w_in_v[:, :, 1536:1544]), writes=[Bwst[n_ % 2]])
        for kc in range(8):
            P.op("act", lambda e, kc=kc: e.activation(out=Wf[:, kc, :], in_=wb[:, kc, 0:8], func=AF.Copy, scale=g1T[:, kc:kc + 1]),
                 reads=[Bwst[n_ % 2], Bconst], writes=[BWf])
    load_wf()
    load_piece(2, BWk)
    load_piece(3, BWk)
    load_piece(4, BWv)
    load_piece(5, BWv)
    P.dma("sp", lambda e: e.dma_start(out=selw[:], in_=sel_d), writes=[Bconst])
    for j in range(2):
        P.op("pool", lambda e, j=j: e.memset(Kaug[j][:, :, 64:65], 1.0), writes=[BKaug[j]])
    for i0 in range(0, 32, 8):
        P.op("pool", lambda e, i0=i0: e.memset(V[:, i0:i0 + 8, :, 64:65], 1.0), writes=[BV[i] for i in range(i0, i0 + 8)])

    xT_all_v = xT_all.rearrange("(k p) t -> p k t", p=128)
    xT_own_v = xT_own.rearrange("(k p) t -> p k t", p=128)

    def load_cast(src_v, gi, cnt):
        b = cnt % 2
        P.dma("sp", lambda e: e.dma_start(out=xst[b][:], in_=src_v[:, :, gi * 128:(gi + 1) * 128]), writes=[Bxst[b]])
        P.op("pool", lambda e: e.tensor_copy(out=xb[b][:], in_=xst[b][:]), reads=[Bxst[b]], writes=[Bxb[b]])
        P.op("act", lambda e: e.activation(out=sq[b][:], in_=xst[b][:], func=AF.Square), reads=[Bxst[b]], writes=[Bsq[b]])
        return b

    BmiscS = [Buf("mS0"), Buf("mS1")]
    BmiscF = [Buf("mF0"), Buf("mF1")]
    BmiscC = [Buf("mC0"), Buf("mC1")]

    def rms_stats_pe(b, misc, BmS):
        for kc in range(8):
            P.op("pe", lambda e, kc=kc: e.matmul(misc[:, 0:1], lhsT=sq[b][:, kc, :],
                                                  rhs=ones_bf[:, 0:1], start=(kc == 0), stop=(kc == 7)),
                 reads=[Bsq[b], Bconst], writes=[BmS], inc=(kc == 7))

    def rms_stats_act(misc, BmS, smt, Bs):
        P.op("act", lambda e: e.activation(out=smt[:, 0:1], in_=misc[:, 0:1], func=AF.Ln, bias=EPS, scale=1.0 / D),
             reads=[BmS], writes=[Bs])
        P.op("act", lambda e: e.activation(out=smt[:, 1:2], in_=smt[:, 0:1], func=AF.Exp, scale=-0.5), reads=[Bs], writes=[Bs])
        P.op("act", lambda e: e.activation(out=smt[:, 2:3], in_=smt[:, 0:1], func=AF.Exp, scale=-1.0,
                                           bias=float(-np.log(64.0))), reads=[Bs], writes=[Bs])

    def head_norm_scale(pX, BpX, smt, Bs, sqb, Bsqb):
        P.op("act", lambda e: e.activation(out=sqb[:], in_=pX[:], func=AF.Square), reads=[BpX], writes=[Bsqb])
        P.op("dve", lambda e: e.tensor_reduce(out=smt[:, 24:32], in_=sqb[:].rearrange("p (h d) -> p h d", h=NH),
                                              axis=AX.X, op=ALU.add), reads=[Bsqb], writes=[Bs])
        P.op("dve", lambda e: e.tensor_scalar(out=smt[:, 32:40], in0=smt[:, 24:32], scalar1=smt[:, 2:3], scalar2=None,
                                              op0=ALU.mult), reads=[Bs], writes=[Bs])
        P.op("act", lambda e: e.activation(out=smt[:, 32:40], in_=smt[:, 32:40], func=AF.Ln, bias=EPS), reads=[Bs], writes=[Bs])
        P.op("act", lambda e: e.activation(out=smt[:, 40:48], in_=smt[:, 32:40], func=AF.Exp, scale=-0.5), reads=[Bs], writes=[Bs])
        P.op("dve", lambda e: e.tensor_scalar(out=smt[:, 40:48], in0=smt[:, 40:48], scalar1=smt[:, 1:2], scalar2=None,
                                              op0=ALU.mult), reads=[Bs], writes=[Bs])

    if stage == "w":
        return finish_debug([("Wqkv", Wqkv[:], [128, 8, 1536], BF16), ("Wf", Wf[:], [128, 8, 8], BF16)])
    def kv_A(i):
        b = load_cast(xT_all_v, i, i)
        par = i % 2
        pK, BpK = ps[par], Bps[par]
        pV, BpV = ps[2 + par], Bps[2 + par]
        misc = ps[4 + par]
        BmS = BmiscS[par]
        for kc in range(8):
            st, sp_ = (kc == 0), (kc == 7)
            P.op("pe", lambda e, kc=kc, st=st, sp_=sp_: e.matmul(misc[:, 8:16], lhsT=xb[b][:, kc, :], rhs=Wf[:, kc, :], start=st, stop=sp_),
                 reads=[Bxb[b], BWf], writes=[BmS], inc=sp_)
        rms_stats_pe(b, misc, BmS)
        for kc in range(8):
            st, sp_ = (kc == 0), (kc == 7)
            P.op("pe", lambda e, kc=kc, st=st, sp_=sp_: e.matmul(pK[:], lhsT=xb[b][:, kc, :], rhs=Wqkv[:, kc, 512:1024], start=st, stop=sp_),
                 reads=[Bxb[b], BWk], writes=[BpK], inc=sp_)
            P.op("pe", lambda e, kc=kc, st=st, sp_=sp_: e.matmul(pV[:], lhsT=xb[b][:, kc, :], rhs=Wqkv[:, kc, 1024:1536], start=st, stop=sp_),
                 reads=[Bxb[b], BWv], writes=[BpV], inc=sp_)

    def kv_B1(i):
        par = i % 2
        pK, BpK = ps[par], Bps[par]
        pV, BpV = ps[2 + par], Bps[2 + par]
        misc = ps[4 + par]
        BmS = BmF = BmiscS[par]
        smt, Bs = sm[i % NSM], Bsm[i % NSM]
        rms_stats_act(misc, BmS, smt, Bs)
        P.op("dve", lambda e: e.scalar_tensor_tensor(out=smt[:, 8:16], in0=misc[:, 8:16], scalar=smt[:, 1:2], in1=bf_bc[:],
                                                     op0=ALU.mult, op1=ALU.add), reads=[BmF, Bs, Bconst], writes=[Bs])
        P.op("act", lambda e: e.activation(out=smt[:, 16:24], in_=smt[:, 8:16], func=AF.Exp, scale=-1.0), reads=[Bs], writes=[Bs])
        P.op("act", lambda e: e.activation(out=smt[:, 16:24], in_=smt[:, 16:24], func=AF.Ln, bias=1.0), reads=[Bs], writes=[Bs])
        head_norm_scale(pK, BpK, smt, Bs, sqk[par], Bsqk[par])
        P.op("dve", lambda e: e.tensor_tensor(out=Kaug[par][:, :, 0:64], in0=pK[:].rearrange("p (h d) -> p h d", h=NH),
                                              in1=bc_mid(smt[:, 40:48], 64), op=ALU.mult),
             reads=[BpK, Bs], writes=[BKaug[par]])
        P.op("act", lambda e: e.activation(out=V[:, i, :, 0:64], in_=pV[:].rearrange("p (h d) -> p h d", h=NH),
                                           func=AF.Copy, scale=smt[:, 1:2]), reads=[BpV, Bs], writes=[BV[i]])

    def kv_B2(i):
        par = i % 2
        misc = ps[4 + par]
        BmC = BmiscS[par]
        pT, BpT = ps[6 + par], Bps[6 + par]
        smt, Bs = sm[i % NSM], Bsm[i % NSM]
        P.op("pe", lambda e: e.matmul(misc[:, 16:24], lhsT=tri[:], rhs=smt[:, 16:24], start=True, stop=(i == 0)),
             reads=[Bs, Bconst], writes=[BmC], inc=(i == 0))
        if i > 0:
            P.op("pe", lambda e: e.matmul(misc[:, 16:24], lhsT=Emat[:], rhs=negc[:, i - 1, :], start=False, stop=True),
                 reads=[Bnegc[i - 1], Bconst], writes=[BmC], inc=True)
        P.op("dve", lambda e: e.tensor_copy(out=negc[:, i, :], in_=misc[:, 16:24]), reads=[BmC], writes=[Bnegc[i]])
        pTb = pT[:].bitcast(BF16).rearrange("p (h t) -> p h t", h=NH)
        for h in range(NH):
            P.op("pe", lambda e, h=h: e.transpose(out=pTb[0:65, h, :], in_=Kaug[par][:, h, :], identity=ident[:]),
                 reads=[BKaug[par], Bconst], writes=[BpT], inc=(h == NH - 1))
        P.op("dve", lambda e: e.tensor_copy(out=KT[:, :, i * 128:(i + 1) * 128], in_=pTb[0:65, :, :]),
             reads=[BpT], writes=[BKT[i]])

    def q_A(t):
        b = load_cast(xT_own_v, t, 32 + t)
        par = t % 2
        pQ, BpQ = ps[t % 4], Bps[t % 4]
        misc = ps[4 + par]
        rms_stats_pe(b, misc, BmiscS[par])
        for kc in range(8):
            st, sp_ = (kc == 0), (kc == 7)
            P.op("pe", lambda e, kc=kc, st=st, sp_=sp_: e.matmul(pQ[:], lhsT=xb[b][:, kc, :], rhs=Wqkv[:, kc, 0:512], start=st, stop=sp_),
                 reads=[Bxb[b], BWq], writes=[BpQ], inc=sp_)

    def q_B1(t):
        par = t % 2
        pQ, BpQ = ps[t % 4], Bps[t % 4]
        misc = ps[4 + par]
        smt, Bs = sm[t % NSM], Bsm[t % NSM]
        rms_stats_act(misc, BmiscS[par], smt, Bs)
        head_norm_scale(pQ, BpQ, smt, Bs, sqk[par], Bsqk[par])
        P.op("dve", lambda e: e.tensor_tensor(out=Kaug[par][:, :, 0:64], in0=pQ[:].rearrange("p (h d) -> p h d", h=NH),
                                              in1=bc_mid(smt[:, 40:48], 64), op=ALU.mult),
             reads=[BpQ, Bs], writes=[BKaug[par]])
        P.op("dve", lambda e: e.tensor_tensor(out=seltmp[:], in0=negc[:], in1=bc_mid(selw[:, t, :], NH), op=ALU.mult),
             reads=Bnegc + [Bconst], writes=[Bseltmp])
        P.op("dve", lambda e: e.tensor_reduce(out=smt[:, 48:56], in_=seltmp[:].rearrange("p i h -> p h i"), axis=AX.X, op=ALU.add),
             reads=[Bseltmp], writes=[Bs])
        P.op("dve", lambda e: e.tensor_scalar(out=Kaug[par][:, :, 64:65], in0=smt[:, 48:56].unsqueeze(2), scalar1=-1.0, scalar2=None,
                                              op0=ALU.mult), reads=[Bs], writes=[BKaug[par]])

    def q_B2(t):
        par = t % 2
        pT, BpT = ps[6 + par], Bps[6 + par]
        pTb = pT[:].bitcast(BF16).rearrange("p (h t) -> p h t", h=NH)
        for h in range(NH):
            P.op("pe", lambda e, h=h: e.transpose(out=pTb[0:65, h, :], in_=Kaug[par][:, h, :], identity=ident[:]),
                 reads=[BKaug[par], Bconst], writes=[BpT], inc=(h == NH - 1))
        P.op("act", lambda e: e.activation(out=QT[:, :, t * 128:(t + 1) * 128], in_=pTb[0:65, :, :], func=AF.Copy,
                                           scale=gqkT[0:65, 0:1]), reads=[BpT, Bconst], writes=[BQT[t]])

    tiles = [(kv_A, kv_B1, kv_B2, i) for i in range(nkv)] + [(q_A, q_B1, q_B2, t) for t in range(nq)]
    if stage == "k":
        tiles = tiles[:nkv]
    tiles[0][0](tiles[0][3])
    for n_, (fa, fb1, fb2, ix) in enumerate(tiles):
        if n_ + 1 < len(tiles):
            tiles[n_ + 1][0](tiles[n_ + 1][3])
        if n_ >= 1 and n_ == nkv:
            tiles[n_ - 1][2](tiles[n_ - 1][3])
        fb1(ix)
        if n_ >= 1 and n_ != nkv:
            tiles[n_ - 1][2](tiles[n_ - 1][3])
        if n_ == 2:
            load_piece(0, BWq)
        if n_ == 4:
            load_piece(1, BWq)
    tiles[-1][2](tiles[-1][3])
    if stage == "k":
        return finish_debug([("negc", negc[:], [128, 32, NH], F32), ("KT", KT[:, :, 0:nkv * 128], [65, NH, nkv * 128], BF16),
                             ("V", V[:, 0:nkv], [128, nkv, NH, 65], BF16)])
    if stage == "q":
        return finish_debug([("QT", QT[:, :, 0:nq * 128], [65, NH, nq * 128], BF16)])
    P.barrier()
    A.release(m_p1)

    m_p2 = A.mark()
    NPT = 4
    PT = [A.alloc([128, 512], BF16) for _ in range(NPT)]
    BPT = [Buf("PT%d" % i) for i in range(NPT)]
    maskt = A.alloc([128, 2, 8, 512], BF16)
    Osb = [A.alloc([65, 512], F32) for _ in range(2)]
    BOsb = [Buf("Osb0"), Buf("Osb1")]
    rden = [A.alloc([65, 512], F32) for _ in range(2)]
    Brden = [Buf("rden0"), Buf("rden1")]
    Bmask = Buf("mask")
    P.dma("sp", lambda e: e.dma_start(out=maskt[:], in_=mask_d), writes=[Bmask])

    PSB = (0, 1, 2, 7)
    LA = 3
    steps = []
    hj_of = {}
    for h in range(NH):
        for j in range(4):
            hj_of[(h, j)] = len(hj_of)
            for kb in range(8 * (j + 1)):
                steps.append((h, j, kb))
    nst = len(steps)

    def emit_qk(s_):
        h, j, kb = steps[s_]
        pS, BpS = ps[PSB[s_ % 4]], Bps[PSB[s_ % 4]]
        mk = kb - 8 * j
        P.op("pe", lambda e: e.matmul(pS[:], lhsT=KT[:, h, kb * 128:(kb + 1) * 128],
                                      rhs=QT[:, h, j * 512:(j + 1) * 512], start=True, stop=(mk < 0)),
             reads=[BKT[kb]] + BQT[4 * j:4 * j + 4], writes=[BpS], inc=(mk < 0))
        if mk >= 0:
            P.op("pe", lambda e: e.matmul(pS[:], lhsT=ident[:], rhs=maskt[:, j % 2, mk, :], start=False, stop=True),
                 reads=[Bmask, Bconst], writes=[BpS], inc=True)

    def emit_exp_pv(s_):
        h, j, kb = steps[s_]
        nkb = 8 * (j + 1)
        hj = hj_of[(h, j)]
        pS, BpS = ps[PSB[s_ % 4]], Bps[PSB[s_ % 4]]
        pt, Bpt = PT[s_ % NPT], BPT[s_ % NPT]
        pO, BpO = ps[3 + (hj % 2)], Bps[3 + (hj % 2)]
        P.op("act", lambda e: e.activation(out=pt[:], in_=pS[:], func=AF.Exp, bias=negc[:, kb, h:h + 1], scale=1.0),
             reads=[BpS, Bnegc[kb]], writes=[Bpt])
        P.op("pe", lambda e: e.matmul(pO[0:65, :], lhsT=V[:, kb, h, :], rhs=pt[:], start=(kb == 0), stop=(kb == nkb - 1)),
             reads=[Bpt, BV[kb]], writes=[BpO], inc=(kb == nkb - 1))

    def norm_a(h, j):
        hj = hj_of[(h, j)]
        pO, BpO = ps[3 + (hj % 2)], Bps[3 + (hj % 2)]
        ob, Bob = Osb[hj % 2], BOsb[hj % 2]
        rd, Brd = rden[hj % 2], Brden[hj % 2]
        P.op("dve", lambda e: e.tensor_copy(out=ob[:], in_=pO[0:65, :]), reads=[BpO], writes=[Bob])
        P.op("act", lambda e: e.activation(out=rd[64:65, :], in_=ob[64:65, :], func=AF.Ln), reads=[Bob], writes=[Brd])
        P.op("act", lambda e: e.activation(out=rd[64:65, :], in_=rd[64:65, :], func=AF.Exp, scale=-1.0), reads=[Brd], writes=[Brd])

    def norm_b(h, j):
        hj = hj_of[(h, j)]
        ob, Bob = Osb[hj % 2], BOsb[hj % 2]
        rd, Brd = rden[hj % 2], Brden[hj % 2]
        pB, BpB = ps[5 + (hj % 2)], Bps[5 + (hj % 2)]
        P.op("pe", lambda e: e.matmul(pB[0:64, :], lhsT=ones_f[64:65, 0:64], rhs=rd[64:65, :], start=True, stop=True),
             reads=[Brd, Bconst], writes=[BpB], inc=True)
        P.op("dve", lambda e: e.tensor_tensor(out=yT[:, h, j * 512:(j + 1) * 512], in0=ob[0:64, :], in1=pB[0:64, :], op=ALU.mult),
             reads=[Bob, BpB], writes=[ByT[j]])

    for s_ in range(min(LA, nst)):
        emit_qk(s_)
    deferred = []
    for s_ in range(nst):
        if s_ + LA < nst:
            emit_qk(s_ + LA)
        emit_exp_pv(s_)
        h, j, kb = steps[s_]
        if kb == 8 * (j + 1) - 1:
            norm_a(h, j)
            deferred.append((s_ + 4, h, j))
        while deferred and deferred[0][0] <= s_:
            _, h2_, j2_ = deferred.pop(0)
            norm_b(h2_, j2_)
    for _, h2_, j2_ in deferred:
        norm_b(h2_, j2_)
    P.barrier()
    A.release(m_p2)

    if stage == 2:
        Bd = Buf("dbg")
        P.dma("sp", lambda e: e.dma_start(out=dbg, in_=yT[:]), reads=ByT, writes=[Bd])
        P.barrier()
        P.emit()
        return nc

    A.release(C0 + 65536)
    NSTG = 3
    stg = [A.alloc([128, 2048], F32) for _ in range(NSTG)]
    Bstg = [Buf("stg%d" % i) for i in range(NSTG)]
    stg_n = [0]

    wbufs = {}

    def load_w(dst, src, K, N, gT=None, parts=128, key=None):
        i = stg_n[0] % NSTG
        stg_n[0] += 1
        st_ = stg[i][0:parts, 0:K * N].rearrange("p (k n) -> p k n", k=K)
        Bst = Bstg[i]
        if key is None:
            Bd_ = Buf("w")
        else:
            Bd_ = wbufs.setdefault(key, Buf("w" + key))
        P.dma("sp", lambda e: e.dma_start(out=st_, in_=src), writes=[Bst])
        if gT is None:
            P.op("act", lambda e: e.activation(out=dst, in_=st_, func=AF.Copy), reads=[Bst], writes=[Bd_])
        else:
            for kc in range(K):
                P.op("act", lambda e, kc=kc: e.activation(out=dst[:, kc, :], in_=st_[:, kc, :], func=AF.Copy, scale=gT[:, kc:kc + 1]),
                     reads=[Bst, Bconst], writes=[Bd_])
        return Bd_

    def rnorm_chunk(src_f32, Bsrc, dst_bf, Bdst, sqc, Bsqc, Rt, BR, pR, BpR, ntok):
        Bsrc_l = Bsrc if isinstance(Bsrc, list) else [Bsrc]
        P.op("act", lambda e: e.activation(out=sqc, in_=src_f32, func=AF.Square), reads=Bsrc_l, writes=[Bsqc])
        for kc in range(8):
            P.op("pe", lambda e, kc=kc: e.matmul(pR[:, 0:ntok], lhsT=ones_bf[:], rhs=sqc[:, kc, :], start=(kc == 0), stop=(kc == 7)),
                 reads=[Bsqc, Bconst], writes=[BpR], inc=(kc == 7))
        P.op("act", lambda e: e.activation(out=Rt[:, 0:ntok], in_=pR[:, 0:ntok], func=AF.Ln, bias=EPS, scale=1.0 / D), reads=[BpR], writes=[BR])
        P.op("act", lambda e: e.activation(out=Rt[:, 0:ntok], in_=Rt[:, 0:ntok], func=AF.Exp, scale=-0.5), reads=[BR], writes=[BR])
        P.op("dve", lambda e: e.tensor_tensor(out=dst_bf, in0=src_f32, in1=Rt[:, 0:ntok].unsqueeze(1).to_broadcast([128, 8, ntok]), op=ALU.mult),
             reads=Bsrc_l + [BR], writes=[Bdst])

    m_p3 = A.mark()
    h1T = A.alloc([128, 8, NTOK], BF16)
    Bh1T = [Buf("h1T%d" % j) for j in range(4)]
    hhT = A.alloc([128, 8, 8], BF16)
    BhhT = Buf("hhT")
    BmT_ = None
    m_3a = A.mark()
    xc = [A.alloc([128, 8, 512], F32) for _ in range(2)]
    Bxc = [Buf("xc0"), Buf("xc1")]
    sqc_l = [A.alloc([128, 8, 512], BF16) for _ in range(2)]
    Bsqc_l = [Buf("sqc0"), Buf("sqc1")]
    Rt_l = [A.alloc([128, 512], F32) for _ in range(2)]
    BR_l = [Buf("R0"), Buf("R1")]
    sqc, Bsqc, Rt, BR = sqc_l[0], Bsqc_l[0], Rt_l[0], BR_l[0]
    xh = A.alloc([128, 8, 8], F32)
    Bxh = Buf("xh")

    def p3a_a(j):
        b = j % 2
        P.dma("sp", lambda e: e.dma_start(out=xc[b][:], in_=xT_own_v[:, :, j * 512:(j + 1) * 512]), writes=[Bxc[b]])
        P.op("act", lambda e: e.activation(out=sqc_l[b][:], in_=xc[b][:], func=AF.Square), reads=[Bxc[b]], writes=[Bsqc_l[b]])
        pR, BpR = ps[b], Bps[b]
        for kc in range(8):
            P.op("pe", lambda e, kc=kc: e.matmul(pR[:], lhsT=ones_bf[:], rhs=sqc_l[b][:, kc, :], start=(kc == 0), stop=(kc == 7)),
                 reads=[Bsqc_l[b], Bconst], writes=[BpR], inc=(kc == 7))

    def p3a_b(j):
        b = j % 2
        pR, BpR = ps[b], Bps[b]
        P.op("act", lambda e: e.activation(out=Rt_l[b][:], in_=pR[:], func=AF.Ln, bias=EPS, scale=1.0 / D), reads=[BpR], writes=[BR_l[b]])
        P.op("act", lambda e: e.activation(out=Rt_l[b][:], in_=Rt_l[b][:], func=AF.Exp, scale=-0.5), reads=[BR_l[b]], writes=[BR_l[b]])
        P.op("dve", lambda e: e.tensor_tensor(out=h1T[:, :, j * 512:(j + 1) * 512], in0=xc[b][:],
                                              in1=Rt_l[b][:].unsqueeze(1).to_broadcast([128, 8, 512]), op=ALU.mult),
             reads=[Bxc[b], BR_l[b]], writes=[Bh1T[j]])
    p3a_a(0)
    for j in range(4):
        if j + 1 < 4:
            p3a_a(j + 1)
        p3a_b(j)
    P.dma("sp", lambda e: e.dma_start(out=xh[:], in_=xhT.rearrange("(k p) t -> p k t", p=128)), writes=[Bxh])
    rnorm_chunk(xh[:], Bxh, hhT[:], BhhT, sqc[:, :, 0:8], Bsqc, Rt, BR, ps[2], Bps[2], 8)
    P.barrier()
    if stage == "3a":
        return finish_debug([("h1T", h1T[:], [128, 8, NTOK], BF16), ("hhT", hhT[:], [128, 8, 8], BF16)])
    A.release(m_3a)

    BmT = A.alloc([128, 4, NTOK], BF16)
    BBmT = [Buf("BmT%d" % j) for j in range(4)]
    m_3b = A.mark()
    convw = A.alloc([128, 4, 3], F32)
    P.dma("sp", lambda e: e.dma_start(out=convw[:], in_=conv_d), writes=[Bconst])
    wcv = [[A.alloc([128, 8, 128], BF16) for _ in range(3)] for _ in range(2)]
    ubuf = [A.alloc([128, 514], F32) for _ in range(2)]
    Bubuf = [Buf("u0"), Buf("u1")]
    cct = [A.alloc([128, 514], F32) for _ in range(2)]
    Bcct = [Buf("cct0"), Buf("cct1")]
    tcv = [A.alloc([128, 512], F32) for _ in range(2)]
    Btcv = [Buf("tcv0"), Buf("tcv1")]

    def p3b(ci, j, n, Bw):
        b = n % 2
        pcb, pcc, pcu = ps[3 * b], ps[3 * b + 1], ps[3 * b + 2]
        Bpcb, Bpcc, Bpcu = Bps[3 * b], Bps[3 * b + 1], Bps[3 * b + 2]
        ph, Bph = ps[6 + b], Bps[6 + b]
        w3 = wcv[ci % 2]
        for wi, (pp, Bpp) in enumerate(((pcb, Bpcb), (pcc, Bpcc), (pcu, Bpcu))):
            for kc in range(8):
                P.op("pe", lambda e, kc=kc, wi=wi, pp=pp: e.matmul(pp[:], lhsT=w3[wi][:, kc, :], rhs=h1T[:, kc, j * 512:(j + 1) * 512],
                                                                    start=(kc == 0), stop=(kc == 7)),
                     reads=[Bw[wi], Bh1T[j]], writes=[Bpp], inc=(kc == 7))
        for wi in (1, 2):
            for kc in range(8):
                P.op("pe", lambda e, kc=kc, wi=wi: e.matmul(ph[:, 2 * wi:2 * wi + 2], lhsT=w3[wi][:, kc, :], rhs=hhT[:, kc, 2 * j:2 * j + 2],
                                                             start=(kc == 0), stop=(kc == 7)),
                     reads=[Bw[wi], BhhT], writes=[Bph], inc=(kc == 7))
        ct, Bct = cct[b], Bcct[b]
        ub, Bub = ubuf[b], Bubuf[b]
        tv, Btv = tcv[b], Btcv[b]
        P.op("act", lambda e: e.activation(out=ct[:, 2:514], in_=pcc[:], func=AF.Copy), reads=[Bpcc], writes=[Bct])
        P.op("act", lambda e: e.activation(out=ct[:, 0:2], in_=ph[:, 2:4], func=AF.Copy), reads=[Bph], writes=[Bct])
        P.op("dve", lambda e: e.tensor_tensor(out=ub[:, 2:514], in0=ct[:, 2:514], in1=pcu[:], op=ALU.mult), reads=[Bct, Bpcu], writes=[Bub])
        P.op("dve", lambda e: e.tensor_tensor(out=ub[:, 0:2], in0=ct[:, 0:2], in1=ph[:, 4:6], op=ALU.mult), reads=[Bct, Bph], writes=[Bub])
        P.op("dve", lambda e: e.tensor_scalar(out=tv[:], in0=ub[:, 0:512], scalar1=convw[:, ci, 0:1], scalar2=None, op0=ALU.mult),
             reads=[Bub, Bconst], writes=[Btv])
        P.op("dve", lambda e: e.scalar_tensor_tensor(out=tv[:], in0=ub[:, 1:513], scalar=convw[:, ci, 1:2], in1=tv[:], op0=ALU.mult, op1=ALU.add),
             reads=[Bub, Btv, Bconst], writes=[Btv])
        P.op("dve", lambda e: e.scalar_tensor_tensor(out=tv[:], in0=ub[:, 2:514], scalar=convw[:, ci, 2:3], in1=tv[:], op0=ALU.mult, op1=ALU.add),
             reads=[Bub, Btv, Bconst], writes=[Btv])
        P.op("dve", lambda e: e.tensor_tensor(out=BmT[:, ci, j * 512:(j + 1) * 512], in0=tv[:], in1=pcb[:], op=ALU.mult),
             reads=[Btv, Bpcb], writes=[BBmT[j]])

    def load_3b(ci):
        Bw_ = []
        for wi, base in enumerate((1544, 2056, 2568)):
            c0 = base + ci * 128
            Bw_.append(load_w(wcv[ci % 2][wi][:], w_in_v[:, :, c0:c0 + 128], 8, 128, g1T, key="cv%d_%d" % (ci % 2, wi)))
        return Bw_
    n3b = 0
    Bw_next = load_3b(0)
    for ci in range(4):
        Bw = Bw_next
        if ci + 1 < 4:
            Bw_next = load_3b(ci + 1)
        for j in range(4):
            p3b(ci, j, n3b, Bw)
            n3b += 1
    P.barrier()
    if stage == "3b":
        return finish_debug([("BmT", BmT[:], [128, 4, NTOK], BF16)])
    A.release(m_3b)

    mT = nc.alloc_sbuf_tensor_at("mT", [128, 8, NTOK], BF16, offset=SB_END - 32768)
    BmTb = [Buf("mT%d" % j) for j in range(4)]
    m_3c = A.mark()
    wga = [A.alloc([128, 8, 128], BF16) for _ in range(2)]
    wgb = [A.alloc([128, 8, 128], BF16) for _ in range(2)]
    wA = [A.alloc([64, 8, 128], BF16) for _ in range(2)]
    wB = [A.alloc([128, 4, 128], BF16) for _ in range(2)]
    sga = [A.alloc([128, 512], F32) for _ in range(2)]
    sgb = [A.alloc([128, 512], F32) for _ in range(2)]
    Bsga = [Buf("sga0"), Buf("sga1")]
    Bsgb = [Buf("sgb0"), Buf("sgb1")]
    w_oa_v = w_oa.rearrange("(h p) n -> p h n", p=64)
    w_oc_v = w_oc.rearrange("(k p) n -> p k n", p=128)

    def p3c(m, j, n, Bw):
        b = n % 2
        pga, pgb, pA, pB_ = ps[4 * b], ps[4 * b + 1], ps[4 * b + 2], ps[4 * b + 3]
        Bpga, Bpgb, BpA, BpB_ = Bps[4 * b], Bps[4 * b + 1], Bps[4 * b + 2], Bps[4 * b + 3]
        wb_ = m % 2
        tok = slice(j * 512, (j + 1) * 512)
        for kc in range(8):
            P.op("pe", lambda e, kc=kc: e.matmul(pga[:], lhsT=wga[wb_][:, kc, :], rhs=h1T[:, kc, tok], start=(kc == 0), stop=(kc == 7)),
                 reads=[Bw[0], Bh1T[j]], writes=[Bpga], inc=(kc == 7))
        for kc in range(8):
            P.op("pe", lambda e, kc=kc: e.matmul(pgb[:], lhsT=wgb[wb_][:, kc, :], rhs=h1T[:, kc, tok], start=(kc == 0), stop=(kc == 7)),
                 reads=[Bw[1], Bh1T[j]], writes=[Bpgb], inc=(kc == 7))
        for h in range(8):
            P.op("pe", lambda e, h=h: e.matmul(pA[:], lhsT=wA[wb_][:, h, :], rhs=yT[:, h, tok], start=(h == 0), stop=(h == 7)),
                 reads=[Bw[2], ByT[j]], writes=[BpA], inc=(h == 7))
        for kc in range(4):
            P.op("pe", lambda e, kc=kc: e.matmul(pB_[:], lhsT=wB[wb_][:, kc, :], rhs=BmT[:, kc, tok], start=(kc == 0), stop=(kc == 3)),
                 reads=[Bw[3], BBmT[j]], writes=[BpB_], inc=(kc == 3))
        P.op("act", lambda e: e.activation(out=sga[b][:], in_=pga[:], func=AF.Sigmoid), reads=[Bpga], writes=[Bsga[b]])
        P.op("act", lambda e: e.activation(out=sgb[b][:], in_=pgb[:], func=AF.Sigmoid), reads=[Bpgb], writes=[Bsgb[b]])
        P.op("dve", lambda e: e.tensor_tensor(out=sga[b][:], in0=sga[b][:], in1=pA[:], op=ALU.mult), reads=[Bsga[b], BpA], writes=[Bsga[b]])
        P.op("dve", lambda e: e.tensor_tensor(out=sgb[b][:], in0=sgb[b][:], in1=pB_[:], op=ALU.mult), reads=[Bsgb[b], BpB_], writes=[Bsgb[b]])
        P.op("dve", lambda e: e.tensor_tensor(out=mT[:, m, tok], in0=sga[b][:], in1=sgb[b][:], op=ALU.add),
             reads=[Bsga[b], Bsgb[b]], writes=[BmTb[j]])

    def load_3c(m):
        c = m * 128
        return [load_w(wga[m % 2][:], w_in_v[:, :, 3080 + c:3080 + c + 128], 8, 128, g1T, key="ga%d" % (m % 2)),
                load_w(wgb[m % 2][:], w_in_v[:, :, 4104 + c:4104 + c + 128], 8, 128, g1T, key="gb%d" % (m % 2)),
                load_w(wA[m % 2][:], w_oa_v[:, :, c:c + 128], 8, 128, None, parts=64, key="wA%d" % (m % 2)),
                load_w(wB[m % 2][:], w_oc_v[:, :, c:c + 128], 4, 128, None, key="wB%d" % (m % 2))]
    n3c = 0
    Bw_next = load_3c(0)
    for m in range(8):
        Bw = Bw_next
        if m + 1 < 8:
            Bw_next = load_3c(m + 1)
        for j in range(4):
            p3c(m, j, n3c, Bw)
            n3c += 1
    P.barrier()
    A.release(m_p3)
    if stage == "3c":
        return finish_debug([("mT", mT[:], [128, 8, NTOK], BF16)])


    xT = nc.alloc_sbuf_tensor_at("xTres", [128, 8, NTOK], F32, offset=C0)
    BxT = [[Buf("xT%d_%d" % (m, j)) for j in range(4)] for m in range(8)]
    m_3d = A.mark()
    wo = [A.alloc([128, 8, 128], BF16) for _ in range(2)]
    w_o_v = w_o.rearrange("(k p) n -> p k n", p=128)

    def p3d(m, j, n, Bw):
        pp, Bpp = ps[n % 4], Bps[n % 4]
        tok = slice(j * 512, (j + 1) * 512)
        for kc in range(8):
            P.op("pe", lambda e, kc=kc: e.matmul(pp[:], lhsT=wo[m % 2][:, kc, :], rhs=mT[:, kc, tok], start=(kc == 0), stop=(kc == 7)),
                 reads=[Bw, BmTb[j]], writes=[Bpp], inc=(kc == 7))
        P.op("dve", lambda e: e.tensor_tensor(out=xT[:, m, tok], in0=pp[:], in1=xT[:, m, tok], op=ALU.add),
             reads=[Bpp, BxT[m][j]], writes=[BxT[m][j]])

    def p3d_load(m):
        Bw_ = load_w(wo[m % 2][:], w_o_v[:, :, m * 128:(m + 1) * 128], 8, 128, None, key="wo%d" % (m % 2))
        P.dma("sp", lambda e: e.dma_start(out=xT[:, m, :], in_=xT_own[m * 128:(m + 1) * 128, :]), writes=BxT[m])
        return Bw_
    Bw_nx = p3d_load(0)
    for m in range(8):
        Bw_cur = Bw_nx
        if m + 1 < 8:
            Bw_nx = p3d_load(m + 1)
        for j in range(4):
            p3d(m, j, m * 4 + j, Bw_cur)
    P.barrier()
    A.release(m_3d)

    h2T = A.alloc([128, 8, NTOK], BF16)
    Bh2T = [Buf("h2T%d" % j) for j in range(4)]
    comb = A.alloc([128, 16, 16], F32)
    Bcomb = [Buf("comb%d" % t) for t in range(16)]
    m_3e = A.mark()
    sqc2 = A.alloc([128, 8, 512], BF16)
    Bsqc2 = Buf("sqc2")
    Rt2 = A.alloc([128, 512], F32)
    BR2 = Buf("R2")
    Wr = A.alloc([128, 8, 20], BF16)
    br_t = A.alloc([128, 20], F32)
    BWr = load_w(Wr[:], w_r.rearrange("(k p) n -> p k n", p=128), 8, 20, g2T)
    P.dma("sp", lambda e: e.dma_start(out=br_t[:], in_=br_d), writes=[Bconst])

    def p3e(j):
        tok = slice(j * 512, (j + 1) * 512)
        rnorm_chunk(xT[:, :, tok], [BxT[m][j] for m in range(8)], h2T[:, :, tok], Bh2T[j], sqc2[:], Bsqc2, Rt2, BR2, ps[j % 2], Bps[j % 2], 512)
    for j in range(4):
        p3e(j)

    pl, Bpl = ps[4], Bps[4]

    def route_mm(t):
        for kc in range(8):
            P.op("pe", lambda e, kc=kc: e.matmul(pl[:, t * 20:(t + 1) * 20], lhsT=h2T[:, kc, t * 128:(t + 1) * 128], rhs=Wr[:, kc, :],
                                                 start=(kc == 0), stop=(kc == 7)),
                 reads=[Bh2T[t // 4], BWr], writes=[Bpl], inc=(kc == 7))
    for t in range(16):
        route_mm(t)

    def ra(n):
        return A.alloc([128, 16, n], F32)
    Lb, dg, ge, oh, tmpr, ein, d1, mk1, e2, d2, sel, wv = ra(20), ra(4), ra(4), ra(4), ra(16), ra(4), ra(4), ra(4), ra(4), ra(4), ra(4), ra(4)
    gmax, gsum, gval, m1, m2, wsum, sc = (A.alloc([128, 16], F32) for _ in range(7))
    BRr = Buf("route")

    def dv(fn, rd=()):
        P.op("dve", fn, reads=[BRr] + list(rd), writes=[BRr])

    def bc4(ap2):
        return ap2.unsqueeze(2).to_broadcast([128, 16, 4])
    dv(lambda e: e.tensor_tensor(out=Lb[:], in0=pl[:, 0:320].rearrange("p (t c) -> p t c", c=20),
                                 in1=br_t[:].unsqueeze(1).to_broadcast([128, 16, 20]), op=ALU.add), rd=[Bpl, Bconst])
    dv(lambda e: e.tensor_reduce(out=gmax[:], in_=Lb[:, :, 0:4], axis=AX.X, op=ALU.max))
    dv(lambda e: e.tensor_tensor(out=dg[:], in0=Lb[:, :, 0:4], in1=bc4(gmax[:]), op=ALU.subtract))
    P.op("act", lambda e: e.activation(out=ge[:], in_=dg[:], func=AF.Exp), reads=[BRr], writes=[BRr])
    dv(lambda e: e.tensor_reduce(out=gsum[:], in_=ge[:], axis=AX.X, op=ALU.add))
    dv(lambda e: e.reciprocal(out=gval[:], in_=gsum[:]))
    dv(lambda e: e.tensor_scalar(out=oh[:], in0=dg[:], scalar1=0.0, scalar2=None, op0=ALU.is_equal))
    dv(lambda e: e.tensor_tensor(out=tmpr[:].rearrange("p t (g j) -> p t g j", g=4), in0=Lb[:, :, 4:20].rearrange("p t (g j) -> p t g j", g=4),
                                 in1=oh[:].unsqueeze(3).to_broadcast([128, 16, 4, 4]), op=ALU.mult))
    dv(lambda e: e.tensor_reduce(out=ein[:], in_=tmpr[:].rearrange("p t (g j) -> p t j g", g=4), axis=AX.X, op=ALU.add))
    dv(lambda e: e.tensor_reduce(out=m1[:], in_=ein[:], axis=AX.X, op=ALU.max))
    dv(lambda e: e.tensor_tensor(out=d1[:], in0=ein[:], in1=bc4(m1[:]), op=ALU.subtract))
    dv(lambda e: e.tensor_scalar(out=mk1[:], in0=d1[:], scalar1=0.0, scalar2=None, op0=ALU.is_equal))
    dv(lambda e: e.scalar_tensor_tensor(out=e2[:], in0=mk1[:], scalar=-1e30, in1=d1[:], op0=ALU.mult, op1=ALU.add))
    dv(lambda e: e.tensor_reduce(out=m2[:], in_=e2[:], axis=AX.X, op=ALU.max))
    dv(lambda e: e.tensor_tensor(out=d2[:], in0=e2[:], in1=bc4(m2[:]), op=ALU.subtract))
    dv(lambda e: e.tensor_scalar(out=sel[:], in0=d2[:], scalar1=0.0, scalar2=None, op0=ALU.is_equal))
    dv(lambda e: e.tensor_tensor(out=sel[:], in0=sel[:], in1=mk1[:], op=ALU.add))
    P.op("act", lambda e: e.activation(out=wv[:], in_=d1[:], func=AF.Exp), reads=[BRr], writes=[BRr])
    dv(lambda e: e.tensor_tensor(out=wv[:], in0=wv[:], in1=sel[:], op=ALU.mult))
    dv(lambda e: e.tensor_reduce(out=wsum[:], in_=wv[:], axis=AX.X, op=ALU.add))
    dv(lambda e: e.reciprocal(out=sc[:], in_=wsum[:]))
    dv(lambda e: e.tensor_tensor(out=sc[:], in0=sc[:], in1=gval[:], op=ALU.mult))
    dv(lambda e: e.tensor_tensor(out=wv[:], in0=wv[:], in1=bc4(sc[:]), op=ALU.mult))
    P.op("dve", lambda e: e.tensor_tensor(out=comb[:].rearrange("p t (g j) -> p t g j", g=4),
                                          in0=oh[:].unsqueeze(3).to_broadcast([128, 16, 4, 4]),
                                          in1=wv[:].unsqueeze(2).to_broadcast([128, 16, 4, 4]), op=ALU.mult),
         reads=[BRr], writes=Bcomb)
    P.barrier()
    if stage == "3e":
        return finish_debug([("xT", xT[:], [128, 8, NTOK], F32), ("comb", comb[:], [128, 16, 16], F32)])
    A.release(m_3e)

    m_p4 = A.mark()
    Wgu = [A.alloc([128, 8, 512], BF16) for _ in range(2)]
    Wd = [A.alloc([128, 2, 1024], BF16) for _ in range(2)]
    BWg = [Buf("Wg0"), Buf("Wg1")]
    BWu = [Buf("Wu0"), Buf("Wu1")]
    BWd = [Buf("Wd0"), Buf("Wd1")]
    mstg = [A.alloc([128, 2048], F32) for _ in range(6)]
    Bmstg = [Buf("mstg%d" % i) for i in range(6)]
    sa = [A.alloc([128, 256], F32) for _ in range(2)]
    Bsa = [Buf("sa0"), Buf("sa1")]
    hid = [A.alloc([128, 256], BF16) for _ in range(2)]
    Bhid = [Buf("hid0"), Buf("hid1")]
    hidT = [A.alloc([128, 2, 512], BF16) for _ in range(2)]
    BhidT = [Buf("hidT0"), Buf("hidT1")]

    def moe_wdma(e):
        base = (e % 2) * 3
        srcs = (w_gate[e].rearrange("(k p) n -> p k n", p=128), w_up[e].rearrange("(k p) n -> p k n", p=128),
                w_down[e].rearrange("(k p) n -> p k n", p=128))
        shp = ((8, 256), (8, 256), (2, 1024))
        for q_ in range(3):
            K_, N_ = shp[q_]
            st_ = mstg[base + q_][:, 0:K_ * N_].rearrange("p (k n) -> p k n", k=K_)
            P.dma("sp", lambda e_, st_=st_, src=srcs[q_]: e_.dma_start(out=st_, in_=src), writes=[Bmstg[base + q_]])

    def moe_wcast_ops(e):
        base = (e % 2) * 3
        we = e % 2
        sg_ = mstg[base][:, 0:2048].rearrange("p (k n) -> p k n", k=8)
        su_ = mstg[base + 1][:, 0:2048].rearrange("p (k n) -> p k n", k=8)
        sd_ = mstg[base + 2][:, 0:2048].rearrange("p (k n) -> p k n", k=2)
        ops_ = []
        for kc in range(8):
            ops_.append(lambda kc=kc: P.op("act", lambda e_: e_.activation(out=Wgu[we][:, kc, 0:256], in_=sg_[:, kc, :], func=AF.Copy,
                                                                      scale=g2T[:, kc:kc + 1]),
                                           reads=[Bmstg[base], Bconst], writes=[BWg[we]]))
        for kc in range(8):
            ops_.append(lambda kc=kc: P.op("act", lambda e_: e_.activation(out=Wgu[we][:, kc, 256:512], in_=su_[:, kc, :], func=AF.Copy,
                                                                      scale=g2T[:, kc:kc + 1]),
                                           reads=[Bmstg[base + 1], Bconst], writes=[BWu[we]]))
        for fc in range(2):
            ops_.append(lambda fc=fc: P.op("act", lambda e_: e_.activation(out=Wd[we][:, fc, :], in_=sd_[:, fc, :], func=AF.Copy),
                                           reads=[Bmstg[base + 2]], writes=[BWd[we]]))
        return ops_

    def moe_wcast(e):
        for f_ in moe_wcast_ops(e):
            f_()

    def moe_A(u):
        e, t = u // 16, u % 16
        j = t // 4
        we = e % 2
        pau, Bpau = ps[u % 3], Bps[u % 3]
        for kc in range(8):
            P.op("pe", lambda e_, kc=kc: e_.matmul(pau[:], lhsT=h2T[:, kc, t * 128:(t + 1) * 128], rhs=Wgu[we][:, kc, :],
                                                   start=(kc == 0), stop=(kc == 7)),
                 reads=[Bh2T[j], BWg[we], BWu[we]], writes=[Bpau], inc=(kc == 7))

    def moe_B(u):
        e, t = u // 16, u % 16
        j, tt_ = t // 4, t % 4
        b = u % 2
        pau, Bpau = ps[u % 3], Bps[u % 3]
        ptr, Bptr = ps[3 + b], Bps[3 + b]
        P.op("act", lambda e_: e_.activation(out=sa[b][:], in_=pau[:, 0:256], func=AF.Silu), reads=[Bpau], writes=[Bsa[b]])
        P.op("dve", lambda e_: e_.scalar_tensor_tensor(out=hid[b][:], in0=sa[b][:], scalar=comb[:, t, e:e + 1], in1=pau[:, 256:512],
                                                       op0=ALU.mult, op1=ALU.mult), reads=[Bsa[b], Bpau, Bcomb[t]], writes=[Bhid[b]])
        ptb = ptr[:].bitcast(BF16).rearrange("p (f t) -> p f t", t=128)
        for fc in range(2):
            P.op("pe", lambda e_, fc=fc: e_.transpose(out=ptb[:, fc, :], in_=hid[b][:, fc * 128:(fc + 1) * 128], identity=ident[:]),
                 reads=[Bhid[b], Bconst], writes=[Bptr], inc=(fc == 1))
        hb = (e * 4 + j) % 2
        P.op("act", lambda e_: e_.activation(out=hidT[hb][:, :, tt_ * 128:(tt_ + 1) * 128], in_=ptb[:, 0:2, :], func=AF.Copy),
             reads=[Bptr], writes=[BhidT[hb]])

    dn_n = [0]

    def moe_C1(e, j, m):
        we = e % 2
        hb = (e * 4 + j) % 2
        tok = slice(j * 512, (j + 1) * 512)
        n = dn_n[0]
        dn_n[0] += 1
        pd, Bpd = ps[5 + n % 3], Bps[5 + n % 3]
        for fc in range(2):
            P.op("pe", lambda e_, fc=fc: e_.matmul(pd[:], lhsT=Wd[we][:, fc, m * 128:(m + 1) * 128], rhs=hidT[hb][:, fc, :],
                                                   start=(fc == 0), stop=(fc == 1)),
                 reads=[BWd[we], BhidT[hb]], writes=[Bpd], inc=(fc == 1))
        P.op("dve", lambda e_: e_.tensor_tensor(out=xT[:, m, tok], in0=pd[:], in1=xT[:, m, tok], op=ALU.add),
             reads=[Bpd, BxT[m][j]], writes=[BxT[m][j]])

    NU = NE * 16
    moe_wdma(0)
    moe_wcast(0)
    moe_wdma(1)
    moe_A(0)
    moe_A(1)
    pendC = []
    pendW = []
    for u in range(NU):
        e, t = u // 16, u % 16
        if u + 2 < NU:
            moe_A(u + 2)
        moe_B(u)
        k_ = 0
        while pendC and pendC[0][0] <= u and k_ < 2:
            _, e2_, j2_, m2_ = pendC.pop(0)
            moe_C1(e2_, j2_, m2_)
            k_ += 1
        if t % 4 == 3:
            for m_ in range(8):
                pendC.append((u + 1, e, t // 4, m_))
        if t == 5:
            assert not [c for c in pendC if c[1] < e]
            if e + 1 < NE:
                pendW = moe_wcast_ops(e + 1)
            if e + 2 < NE:
                moe_wdma(e + 2)
        for _ in range(2):
            if pendW:
                pendW.pop(0)()
    for _, e2_, j2_, m2_ in pendC:
        moe_C1(e2_, j2_, m2_)
    P.barrier()
    if stage == "4":
        return finish_debug([("xT", xT[:], [128, 8, NTOK], F32)])
    A.release(m_p4)
    A.release(C0 + 65536)

    stg5 = [A.alloc([128, 2048], F32) for _ in range(NSTG)]
    for i_ in range(NSTG):
        stg[i_] = stg5[i_]
    Wpg = A.alloc([128, 8, 1024], BF16)
    Wple = A.alloc([128, 2, 1024], BF16)
    h3T = [A.alloc([128, 8, 512], BF16) for _ in range(2)]
    Bh3T = [Buf("h3T0"), Buf("h3T1")]
    sqc3 = A.alloc([128, 8, 512], BF16)
    Bsqc3 = Buf("sqc3")
    Rt3 = A.alloc([128, 512], F32)
    BR3 = Buf("R3")
    pst = [A.alloc([128, 2, 512], F32) for _ in range(2)]
    Bpst = [Buf("pst0"), Buf("pst1")]
    ptb5 = [A.alloc([128, 2, 512], BF16) for _ in range(2)]
    Bptb5 = [Buf("ptb0"), Buf("ptb1")]
    sg = [A.alloc([128, 512], F32) for _ in range(2)]
    Bsg = [Buf("sg0"), Buf("sg1")]
    ost = [A.alloc([128, 512], F32) for _ in range(3)]
    Bost = [Buf("ost%d" % i) for i in range(3)]
    w_pg_v = w_pg.rearrange("(k p) n -> p k n", p=128)
    pT_v = pT_own.rearrange("(k p) t -> p k t", p=128)
    Bout = Buf("out")

    def p5_m(j, m, n):
        b = j % 2
        tok = slice(j * 512, (j + 1) * 512)
        ppg, Bppg = ps[2 + 2 * (n % 3)], Bps[2 + 2 * (n % 3)]
        ppe, Bppe = ps[3 + 2 * (n % 3)], Bps[3 + 2 * (n % 3)]
        for kc in range(8):
            P.op("pe", lambda e, kc=kc: e.matmul(ppg[:], lhsT=Wpg[:, kc, m * 128:(m + 1) * 128], rhs=h3T[b][:, kc, :], start=(kc == 0), stop=(kc == 7)),
                 reads=[BWpg[m // 2], Bh3T[b]], writes=[Bppg], inc=(kc == 7))
        for kc in range(2):
            P.op("pe", lambda e, kc=kc: e.matmul(ppe[:], lhsT=Wple[:, kc, m * 128:(m + 1) * 128], rhs=ptb5[b][:, kc, :], start=(kc == 0), stop=(kc == 1)),
                 reads=[BWple, Bptb5[b]], writes=[Bppe], inc=(kc == 1))
        sb_, o_ = n % 2, n % 3
        P.op("act", lambda e: e.activation(out=sg[sb_][:], in_=ppg[:], func=AF.Sigmoid), reads=[Bppg], writes=[Bsg[sb_]])
        P.op("dve", lambda e: e.tensor_tensor(out=sg[sb_][:], in0=sg[sb_][:], in1=ppe[:], op=ALU.mult), reads=[Bsg[sb_], Bppe], writes=[Bsg[sb_]])
        P.op("dve", lambda e: e.tensor_tensor(out=ost[o_][:], in0=sg[sb_][:], in1=xT[:, m, tok], op=ALU.add),
             reads=[Bsg[sb_], BxT[m][j]], writes=[Bost[o_]])
        P.dma("sp", lambda e: e.dma_start(out=outT[m * 128:(m + 1) * 128, tok], in_=ost[o_][:]), reads=[Bost[o_]], writes=[Buf("o")])

    def p5_pre(j):
        b = j % 2
        tok = slice(j * 512, (j + 1) * 512)
        P.dma("sp", lambda e: e.dma_start(out=pst[b][:], in_=pT_v[:, :, tok]), writes=[Bpst[b]])
        P.op("pool", lambda e: e.tensor_copy(out=ptb5[b][:], in_=pst[b][:]), reads=[Bpst[b]], writes=[Bptb5[b]])
        rnorm_chunk(xT[:, :, tok], [BxT[m][j] for m in range(8)], h3T[b][:], Bh3T[b], sqc3[:], Bsqc3, Rt3, BR3, ps[j % 2], Bps[j % 2], 512)

    p5_pre(0)
    BWpg = [load_w(Wpg[:, :, c * 256:(c + 1) * 256], w_pg_v[:, :, c * 256:(c + 1) * 256], 8, 256, g3T) for c in range(4)]
    BWple = load_w(Wple[:], w_ple.rearrange("(k p) n -> p k n", p=128), 2, 1024, None)
    for j in range(4):
        if j + 1 < 4:
            p5_pre(j + 1)
        for m in range(8):
            p5_m(j, m, j * 8 + m)
    P.barrier()
    P.emit()
    return nc


def make_masks():
    k = np.arange(128)[:, None]
    q = np.arange(512)[None, :]

    def diag(jb):
        return np.where((jb * 128 + k) <= q, 0.0, -30000.0).astype(np.float32)
    ones = np.zeros((128, 512), np.float32)
    zeros = np.full((128, 512), -30000.0, np.float32)
    E = [diag(0), diag(1), diag(2), diag(3), zeros, zeros, zeros, zeros]
    O = [ones, ones, ones, ones, diag(0), diag(1), diag(2), diag(3)]
    return np.stack(E, 0), np.stack(O, 0)


def prep_inputs(inp):
    x = np.asarray(inp["x"], np.float32)
    p = np.asarray(inp["p"], np.float32)[0]
    E, O = make_masks()
    shared = {
        "w_in": np.ascontiguousarray(inp["w_in"][0]),
        "w_oa": np.ascontiguousarray(inp["w_out_att"][0]),
        "w_oc": np.ascontiguousarray(inp["w_out_conv"][0]),
        "w_o": np.ascontiguousarray(inp["w_o"][0]),
        "w_r": np.ascontiguousarray(np.concatenate([inp["w_rg"][0], inp["w_re"][0]], axis=1)),
        "w_gate": np.ascontiguousarray(inp["w_gate"][0]),
        "w_up": np.ascontiguousarray(inp["w_up"][0]),
        "w_down": np.ascontiguousarray(inp["w_down"][0]),
        "w_pg": np.ascontiguousarray(inp["w_pg"][0]),
        "w_ple": np.ascontiguousarray(inp["w_ple"][0]),
        "g1T": np.ascontiguousarray(inp["attn_norm_g"][0].reshape(8, 128).T),
        "g2T": np.ascontiguousarray(inp["ffn_norm_g"][0].reshape(8, 128).T),
        "g3T": np.ascontiguousarray(inp["ple_norm_g"][0].reshape(8, 128).T),
        "bf_bc": np.ascontiguousarray(np.broadcast_to(inp["b_f"][0][None, :], (128, 8))),
        "gq_col": np.ascontiguousarray(inp["q_norm_g"][0].reshape(64, 1)),
        "gk_col": np.ascontiguousarray(inp["k_norm_g"][0].reshape(64, 1)),
        "convT": np.ascontiguousarray(inp["conv_w"][0].reshape(3, 4, 128).transpose(2, 1, 0)),
        "br_bc": np.ascontiguousarray(np.broadcast_to(
            np.concatenate([inp["b_rg"][0], inp["b_re"][0]])[None, :], (128, 20))),
    }
    shared = {k: np.asarray(v, np.float32) for k, v in shared.items()}
    maps = []
    for c in range(8):
        b, par = c // 2, c % 2
        chunks = CHUNKS[par]
        xb_T = np.ascontiguousarray(x[b].T)
        own_cols = np.concatenate([np.arange(ci * 512, (ci + 1) * 512) for ci in chunks])
        xh = np.zeros((D, 8), np.float32)
        for j, ci in enumerate(chunks):
            if ci > 0:
                xh[:, 2 * j:2 * j + 2] = xb_T[:, ci * 512 - 2:ci * 512]
        sel = np.zeros((16, 32), np.float32)
        for j, ci in enumerate(chunks):
            for tt in range(4):
                sel[4 * j + tt, 4 * ci + tt] = 1.0
        types = [(E, O)[ci % 2] for ci in chunks]
        mask2 = np.stack([types[0], types[1]], 0)
        assert np.array_equal(types[0], types[2]) and np.array_equal(types[1], types[3])
        m = dict(shared)
        m.update({
            "xT_all": xb_T,
            "xT_own": np.ascontiguousarray(xb_T[:, own_cols]),
            "xhT": xh,
            "pT_own": np.ascontiguousarray(p[b].T[:, own_cols]),
            "sel_own": np.ascontiguousarray(np.broadcast_to(sel[None], (128, 16, 32))),
            "mask2": np.ascontiguousarray(mask2.transpose(2, 0, 1, 3)).astype(ml_dtypes.bfloat16),
        })
        maps.append(m)
    return maps


_NC_CACHE = {}


def kernel(**inputs):
    maps = prep_inputs(inputs)
    if "nc" not in _NC_CACHE:
        _NC_CACHE["nc"] = build()
    nc = _NC_CACHE["nc"]
    res = run_bass_kernel_spmd(nc, maps, core_ids=list(range(8)))
    out = np.empty((4, S, D), np.float32)
    for c in range(8):
        b, par = c // 2, c % 2
        oT = np.asarray(res.results[c]["outT"])
        for j, ci in enumerate(CHUNKS[par]):
            out[b, ci * 512:(ci + 1) * 512, :] = oT[:, j * 512:(j + 1) * 512].T
    return out
```

```python
import numpy as np
import ml_dtypes
import concourse.bass as bass
import concourse.mybir as mybir
from concourse.bass_utils import run_bass_kernel_spmd

F32 = mybir.dt.float32
BF16 = mybir.dt.bfloat16
AF = mybir.ActivationFunctionType
ALU = mybir.AluOpType
AX = mybir.AxisListType

ENGS = ("pe", "act", "dve", "pool", "sp")
NDMASEM = 20
SB_BASE = 16512
SB_END = 229376 - 2048
EPS = 1e-6

D = 1024
S = 4096
NH = 8
HD = 64
NTOK = 2048
NE = 16
DE = 256
CHUNKS = ((0, 3, 4, 7), (1, 2, 5, 6))


class Tk:
    __slots__ = ("sem", "val")

    def __init__(self, sem, val):
        self.sem = sem
        self.val = val


class Buf:
    __slots__ = ("name", "w", "r")

    def __init__(self, name=""):
        self.name = name
        self.w = None
        self.r = []


class Prog:
    def __init__(self, nc):
        self.nc = nc
        self.ops = {e: [] for e in ENGS}
        self.cnt = {e: 0 for e in ENGS}
        self.seen = {e: {} for e in ENGS}
        self.pend = {e: [] for e in ENGS}
        self.dma_n = {e: 0 for e in ENGS}
        self.dma_last = {}

    def _need(self, eng, waits, t):
        if t is None:
            return
        if t.sem == "pe" and eng == "pe":
            return
        if t.val is None:
            raise RuntimeError("dependency on op without resolved ticket (missing inc)")
        if self.seen[eng].get(t.sem, 0) >= t.val:
            return
        if waits.get(t.sem, 0) < t.val:
            waits[t.sem] = t.val

    def _deps(self, eng, reads, writes, waits):
        for b in reads:
            self._need(eng, waits, b.w)
        for b in writes:
            self._need(eng, waits, b.w)
            for t in b.r:
                self._need(eng, waits, t)
        for s, v in waits.items():
            self.seen[eng][s] = v

    def _mark(self, tk, reads, writes):
        for b in reads:
            b.r.append(tk)
            if len(b.r) > 64:
                b.r = b.r[-48:]
        for b in writes:
            b.w = tk
            b.r = []

    def op(self, eng, fn, reads=(), writes=(), inc=True):
        waits = {}
        self._deps(eng, reads, writes, waits)
        if inc:
            self.cnt[eng] += 1
            tk = Tk(eng, self.cnt[eng])
            for p in self.pend[eng]:
                p.val = tk.val
            self.pend[eng] = []
        else:
            tk = Tk(eng, None)
            self.pend[eng].append(tk)
        self._mark(tk, reads, writes)
        self.ops[eng].append((fn, list(waits.items()), (eng, 1) if inc else None))
        return tk

    def dma(self, eng, fn, reads=(), writes=()):
        n = self.dma_n[eng]
        self.dma_n[eng] += 1
        semname = "d_%s_%d" % (eng, n % NDMASEM)
        waits = {}
        prev = self.dma_last.get(semname)
        if prev is not None:
            self._need(eng, waits, prev)
        self._deps(eng, reads, writes, waits)
        tk = Tk(semname, 16 * (n // NDMASEM + 1))
        self.dma_last[semname] = tk
        self._mark(tk, reads, writes)
        self.ops[eng].append((fn, list(waits.items()), (semname, 16)))
        return tk

    def barrier(self):
        for e in ENGS:
            assert not self.pend[e], "barrier with pending un-inc'ed ops on " + e
        tks = [Tk(e, self.cnt[e]) for e in ENGS if self.cnt[e] > 0]
        tks += list(self.dma_last.values())
        for e in ENGS:
            waits = {}
            for t in tks:
                if t.sem != e:
                    self._need(e, waits, t)
            for s, v in waits.items():
                self.seen[e][s] = v
            self.ops[e].append((None, list(waits.items()), None))

    def emit(self):
        nc = self.nc
        from contextlib import ExitStack
        semnames = set()
        for e in ENGS:
            for fn, waits, inc in self.ops[e]:
                for s, v in waits:
                    semnames.add(s)
                if inc:
                    semnames.add(inc[0])
        with ExitStack() as st:
            sems = {}
            for s in sorted(semnames):
                sems[s] = st.enter_context(nc.semaphore("s_" + s))
            block = st.enter_context(nc.Block())

            def run(e, engobj):
                for fn, waits, inc in self.ops[e]:
                    for s, v in waits:
                        engobj.wait_ge(sems[s], v)
                    if fn is None:
                        continue
                    ins = fn(engobj)
                    if inc:
                        ins.then_inc(sems[inc[0]], inc[1])

            @block.tensor
            def _(eng):
                run("pe", eng)

            @block.scalar
            def _(eng):
                run("act", eng)

            @block.vector
            def _(eng):
                run("dve", eng)

            @block.gpsimd
            def _(eng):
                run("pool", eng)

            @block.sync
            def _(eng):
                run("sp", eng)


class Arena:
    def __init__(self, nc):
        self.nc = nc
        self.off = SB_BASE
        self.n = 0

    def mark(self):
        return self.off

    def release(self, m):
        self.off = m

    def alloc(self, shape, dt):
        nbytes = int(np.prod(shape[1:])) * (4 if dt == F32 else 2)
        nbytes = (nbytes + 63) // 64 * 64
        assert self.off + nbytes <= SB_END, "SBUF overflow: %d + %d" % (self.off, nbytes)
        self.n += 1
        t = self.nc.alloc_sbuf_tensor_at("t%d" % self.n, list(shape), dt, offset=self.off)
        self.off += nbytes
        return t


def bc_mid(ap2, n):
    p, a = ap2.shape
    return ap2.unsqueeze(2).to_broadcast([p, a, n])


def build(stage=99, nkv=32, nq=16):
    nc = bass.Bass("TRN2", target_bir_lowering=False)
    P = Prog(nc)
    A = Arena(nc)

    def finish_debug(items):
        P.barrier()
        for name, ap, shape, dt in items:
            o = nc.dram_tensor(name, list(shape), dt, kind="ExternalOutput").ap()
            P.dma("sp", lambda e, o=o, ap=ap: e.dma_start(out=o, in_=ap))
        P.barrier()
        P.emit()
        return nc

    def din(name, shape, dt=F32):
        return nc.dram_tensor(name, list(shape), dt, kind="ExternalInput").ap()

    xT_all = din("xT_all", [D, S])
    xT_own = din("xT_own", [D, NTOK])
    xhT = din("xhT", [D, 8])
    pT_own = din("pT_own", [256, NTOK])
    w_in = din("w_in", [D, 5128])
    w_oa = din("w_oa", [512, D])
    w_oc = din("w_oc", [512, D])
    w_o = din("w_o", [D, D])
    w_r = din("w_r", [D, 20])
    w_gate = din("w_gate", [NE, D, DE])
    w_up = din("w_up", [NE, D, DE])
    w_down = din("w_down", [NE, DE, D])
    w_pg = din("w_pg", [D, D])
    w_ple = din("w_ple", [256, D])
    g1T_d = din("g1T", [128, 8])
    g2T_d = din("g2T", [128, 8])
    g3T_d = din("g3T", [128, 8])
    bf_d = din("bf_bc", [128, 8])
    gq_d = din("gq_col", [64, 1])
    gk_d = din("gk_col", [64, 1])
    conv_d = din("convT", [128, 4, 3])
    br_d = din("br_bc", [128, 20])
    sel_d = din("sel_own", [128, 16, 32])
    mask_d = din("mask2", [128, 2, 8, 512], BF16)
    if stage == 2:
        dbg = nc.dram_tensor("dbg", [64, 8, NTOK], BF16, kind="ExternalOutput").ap()
    elif stage == 99:
        outT = nc.dram_tensor("outT", [D, NTOK], F32, kind="ExternalOutput").ap()

    import os
    ps = [nc.alloc_psum_tensor("ps%d" % i, [128, 512], F32) for i in range(int(os.environ.get("NPS", "8")))]
    Bps = [Buf("ps%d" % i) for i in range(len(ps))]

    ident = A.alloc([128, 128], BF16)
    tmpf = A.alloc([128, 128], F32)
    tri = A.alloc([128, 128], F32)
    Emat = A.alloc([128, 128], F32)
    ones_bf = A.alloc([128, 128], BF16)
    ones_f = A.alloc([128, 64], F32)
    g1T = A.alloc([128, 8], F32)
    g2T = A.alloc([128, 8], F32)
    g3T = A.alloc([128, 8], F32)
    bf_bc = A.alloc([128, 8], F32)
    gqkT = A.alloc([65, 1], F32)
    gk_t = A.alloc([64, 1], F32)
    Bconst = Buf("const")
    Btmpf = Buf("tmpf")

    P.op("pool", lambda e: e.memset(tmpf[:], 1.0), writes=[Btmpf])
    P.op("pool", lambda e: e.affine_select(out=tmpf[:], in_=tmpf[:], pattern=[[-1, 128]], compare_op=ALU.is_equal,
                                           fill=0.0, base=0, channel_multiplier=1), reads=[Btmpf], writes=[Btmpf])
    P.op("dve", lambda e: e.tensor_copy(out=ident[:], in_=tmpf[:]), reads=[Btmpf], writes=[Bconst])
    P.op("pool", lambda e: e.memset(tri[:], 1.0), writes=[Bconst])
    P.op("pool", lambda e: e.affine_select(out=tri[:], in_=tri[:], pattern=[[1, 128]], compare_op=ALU.is_ge,
                                           fill=0.0, base=0, channel_multiplier=-1), reads=[Bconst], writes=[Bconst])
    P.op("pool", lambda e: e.memset(Emat[:], 1.0), writes=[Bconst])
    P.op("pool", lambda e: e.affine_select(out=Emat[:], in_=Emat[:], pattern=[[0, 128]], compare_op=ALU.is_equal,
                                           fill=0.0, base=-127, channel_multiplier=1), reads=[Bconst], writes=[Bconst])
    P.op("pool", lambda e: e.memset(ones_bf[:], 1.0), writes=[Bconst])
    P.op("pool", lambda e: e.memset(ones_f[:], 1.0), writes=[Bconst])
    P.op("pool", lambda e: e.memset(gqkT[:], 1.0), writes=[Bconst])
    P.dma("sp", lambda e: e.dma_start(out=gqkT[0:64, :], in_=gq_d), writes=[Bconst])
    for dst, src in ((g1T, g1T_d), (g2T, g2T_d), (g3T, g3T_d), (bf_bc, bf_d), (gk_t, gk_d)):
        P.dma("sp", lambda e, dst=dst, src=src: e.dma_start(out=dst[:], in_=src), writes=[Bconst])
    P.op("dve", lambda e: e.scalar_tensor_tensor(out=gqkT[0:64, :], in0=gqkT[0:64, :], scalar=HD ** -0.5, in1=gk_t[:],
                                                 op0=ALU.mult, op1=ALU.mult), reads=[Bconst], writes=[Bconst])
    P.barrier()
    if stage == "c":
        return finish_debug([("tri", tri[:], [128, 128], F32), ("Emat", Emat[:], [128, 128], F32),
                             ("ident", ident[:], [128, 128], BF16), ("gqk", gqkT[:], [65, 1], F32)])

    C0 = A.mark()
    yT = A.alloc([64, NH, NTOK], BF16)
    ByT = [Buf("yT%d" % j) for j in range(4)]
    m_attn = A.mark()
    KT = A.alloc([65, NH, S], BF16)
    QT = A.alloc([65, NH, NTOK], BF16)
    V = A.alloc([128, 32, NH, 65], BF16)
    negc = A.alloc([128, 32, NH], F32)
    BKT = [Buf("KT%d" % i) for i in range(32)]
    BQT = [Buf("QT%d" % i) for i in range(16)]
    BV = [Buf("V%d" % i) for i in range(32)]
    Bnegc = [Buf("negc%d" % i) for i in range(32)]

    m_p1 = A.mark()
    A.release(C0)
    Wqkv = A.alloc([128, 8, 1536], BF16)
    Wf = A.alloc([128, 8, 8], BF16)
    sqk = [A.alloc([128, 512], F32) for _ in range(2)]
    assert A.off <= C0 + 32768
    A.release(m_p1)
    wst = [A.alloc([128, 8, 256], F32) for _ in range(2)]
    Bwst = [Buf("wst0"), Buf("wst1")]
    BW = Buf("Wqkv")
    xst = [A.alloc([128, 8, 128], F32) for _ in range(2)]
    xb = [A.alloc([128, 8, 128], BF16) for _ in range(2)]
    sq = [A.alloc([128, 8, 128], BF16) for _ in range(2)]
    Bxst = [Buf("xst0"), Buf("xst1")]
    Bxb = [Buf("xb0"), Buf("xb1")]
    Bsq = [Buf("sq0"), Buf("sq1")]
    Bsqk = [Buf("sqk0"), Buf("sqk1")]
    Kaug = [A.alloc([128, NH, 65], BF16) for _ in range(2)]
    BKaug = [Buf("Kaug0"), Buf("Kaug1")]
    tmpq = A.alloc([128, 512], F32)
    Btmpq = Buf("tmpq")
    selw = A.alloc([128, 16, 32], F32)
    seltmp = A.alloc([128, 32, NH], F32)
    Bseltmp = Buf("seltmp")
    NSM = 4
    sm = [A.alloc([128, 64], F32) for _ in range(NSM)]
    Bsm = [Buf("sm%d" % i) for i in range(NSM)]

    w_in_v = w_in.rearrange("(k p) n -> p k n", p=128)
    BWq, BWk, BWv, BWf = Buf("Wq"), Buf("Wk"), Buf("Wv"), Buf("Wf")
    wpiece_n = [0]

    def load_piece(piece, Bdst):
        n_ = wpiece_n[0]
        wpiece_n[0] += 1
        wb = wst[n_ % 2]
        P.dma("sp", lambda e: e.dma_start(out=wb[:], in_=w_in_v[:, :, piece * 256:(piece + 1) * 256]), writes=[Bwst[n_ % 2]])
        for kc in range(8):
            P.op("act", lambda e, kc=kc: e.activation(out=Wqkv[:, kc, piece * 256:(piece + 1) * 256], in_=wb[:, kc, :], func=AF.Copy,
                                                      scale=g1T[:, kc:kc + 1]), reads=[Bwst[n_ % 2], Bconst], writes=[Bdst])

    def load_wf():
        n_ = wpiece_n[0]
        wpiece_n[0] += 1
        wb = wst[n_ % 2]
        P.dma("sp", lambda e: e.dma_start(out=wb[:, :, 0:8], in_=w_in_v[:, :, 1536:1544]), writes=[Bwst[n_ % 2]])
        for kc in range(8):
            P.op("act", lambda e, kc=kc: e.activation(out=Wf[:, kc, :], in_=wb[:, kc, 0:8], func=AF.Copy, scale=g1T[:, kc:kc + 1]),
                 reads=[Bwst[n_ % 2], Bconst], writes=[BWf])
    load_wf()
    load_piece(2, BWk)
    load_piece(3, BWk)
    load_piece(4, BWv)
    load_piece(5, BWv)
    P.dma("sp", lambda e: e.dma_start(out=selw[:], in_=sel_d), writes=[Bconst])
    for j in range(2):
        P.op("pool", lambda e, j=j: e.memset(Kaug[j][:, :, 64:65], 1.0), writes=[BKaug[j]])
    for i0 in range(0, 32, 8):
        P.op("pool", lambda e, i0=i0: e.memset(V[:, i0:i0 + 8, :, 64:65], 1.0), writes=[BV[i] for i in range(i0, i0 + 8)])

    xT_all_v = xT_all.rearrange("(k p) t -> p k t", p=128)
    xT_own_v = xT_own.rearrange("(k p) t -> p k t", p=128)

    def load_cast(src_v, gi, cnt):
        b = cnt % 2
        P.dma("sp", lambda e: e.dma_start(out=xst[b][:], in_=src_v[:, :, gi * 128:(gi + 1) * 128]), writes=[Bxst[b]])
        P.op("pool", lambda e: e.tensor_copy(out=xb[b][:], in_=xst[b][:]), reads=[Bxst[b]], writes=[Bxb[b]])
        P.op("act", lambda e: e.activation(out=sq[b][:], in_=xst[b][:], func=AF.Square), reads=[Bxst[b]], writes=[Bsq[b]])
        return b

    BmiscS = [Buf("mS0"), Buf("mS1")]
    BmiscF = [Buf("mF0"), Buf("mF1")]
    BmiscC = [Buf("mC0"), Buf("mC1")]

    def rms_stats_pe(b, misc, BmS):
        for kc in range(8):
            P.op("pe", lambda e, kc=kc: e.matmul(misc[:, 0:1], lhsT=sq[b][:, kc, :],
                                                  rhs=ones_bf[:, 0:1], start=(kc == 0), stop=(kc == 7)),
                 reads=[Bsq[b], Bconst], writes=[BmS], inc=(kc == 7))

    def rms_stats_act(misc, BmS, smt, Bs):
        P.op("act", lambda e: e.activation(out=smt[:, 0:1], in_=misc[:, 0:1], func=AF.Ln, bias=EPS, scale=1.0 / D),
             reads=[BmS], writes=[Bs])
        P.op("act", lambda e: e.activation(out=smt[:, 1:2], in_=smt[:, 0:1], func=AF.Exp, scale=-0.5), reads=[Bs], writes=[Bs])
        P.op("act", lambda e: e.activation(out=smt[:, 2:3], in_=smt[:, 0:1], func=AF.Exp, scale=-1.0,
                                           bias=float(-np.log(64.0))), reads=[Bs], writes=[Bs])

    def head_norm_scale(pX, BpX, smt, Bs, sqb, Bsqb):
        P.op("act", lambda e: e.activation(out=sqb[:], in_=pX[:], func=AF.Square), reads=[BpX], writes=[Bsqb])
        P.op("dve", lambda e: e.tensor_reduce(out=smt[:, 24:32], in_=sqb[:].rearrange("p (h d) -> p h d", h=NH),
                                              axis=AX.X, op=ALU.add), reads=[Bsqb], writes=[Bs])
        P.op("dve", lambda e: e.tensor_scalar(out=smt[:, 32:40], in0=smt[:, 24:32], scalar1=smt[:, 2:3], scalar2=None,
                                              op0=ALU.mult), reads=[Bs], writes=[Bs])
        P.op("act", lambda e: e.activation(out=smt[:, 32:40], in_=smt[:, 32:40], func=AF.Ln, bias=EPS), reads=[Bs], writes=[Bs])
        P.op("act", lambda e: e.activation(out=smt[:, 40:48], in_=smt[:, 32:40], func=AF.Exp, scale=-0.5), reads=[Bs], writes=[Bs])
        P.op("dve", lambda e: e.tensor_scalar(out=smt[:, 40:48], in0=smt[:, 40:48], scalar1=smt[:, 1:2], scalar2=None,
                                              op0=ALU.mult), reads=[Bs], writes=[Bs])

    if stage == "w":
        return finish_debug([("Wqkv", Wqkv[:], [128, 8, 1536], BF16), ("Wf", Wf[:], [128, 8, 8], BF16)])
    def kv_A(i):
        b = load_cast(xT_all_v, i, i)
        par = i % 2
        pK, BpK = ps[par], Bps[par]
        pV, BpV = ps[2 + par], Bps[2 + par]
        misc = ps[4 + par]
        BmS = BmiscS[par]
        for kc in range(8):
            st, sp_ = (kc == 0), (kc == 7)
            P.op("pe", lambda e, kc=kc, st=st, sp_=sp_: e.matmul(misc[:, 8:16], lhsT=xb[b][:, kc, :], rhs=Wf[:, kc, :], start=st, stop=sp_),
                 reads=[Bxb[b], BWf], writes=[BmS], inc=sp_)
        rms_stats_pe(b, misc, BmS)
        for kc in range(8):
            st, sp_ = (kc == 0), (kc == 7)
            P.op("pe", lambda e, kc=kc, st=st, sp_=sp_: e.matmul(pK[:], lhsT=xb[b][:, kc, :], rhs=Wqkv[:, kc, 512:1024], start=st, stop=sp_),
                 reads=[Bxb[b], BWk], writes=[BpK], inc=sp_)
            P.op("pe", lambda e, kc=kc, st=st, sp_=sp_: e.matmul(pV[:], lhsT=xb[b][:, kc, :], rhs=Wqkv[:, kc, 1024:1536], start=st, stop=sp_),
                 reads=[Bxb[b], BWv], writes=[BpV], inc=sp_)

    def kv_B1(i):
        par = i % 2
        pK, BpK = ps[par], Bps[par]
        pV, BpV = ps[2 + par], Bps[2 + par]
        misc = ps[4 + par]
        BmS = BmF = BmiscS[par]
        smt, Bs = sm[i % NSM], Bsm[i % NSM]
        rms_stats_act(misc, BmS, smt, Bs)
        P.op("dve", lambda e: e.scalar_tensor_tensor(out=smt[:, 8:16], in0=misc[:, 8:16], scalar=smt[:, 1:2], in1=bf_bc[:],
                                                     op0=ALU.mult, op1=ALU.add), reads=[BmF, Bs, Bconst], writes=[Bs])
        P.op("act", lambda e: e.activation(out=smt[:, 16:24], in_=smt[:, 8:16], func=AF.Exp, scale=-1.0), reads=[Bs], writes=[Bs])
        P.op("act", lambda e: e.activation(out=smt[:, 16:24], in_=smt[:, 16:24], func=AF.Ln, bias=1.0), reads=[Bs], writes=[Bs])
        head_norm_scale(pK, BpK, smt, Bs, sqk[par], Bsqk[par])
        P.op("dve", lambda e: e.tensor_tensor(out=Kaug[par][:, :, 0:64], in0=pK[:].rearrange("p (h d) -> p h d", h=NH),
                                              in1=bc_mid(smt[:, 40:48], 64), op=ALU.mult),
             reads=[BpK, Bs], writes=[BKaug[par]])
        P.op("act", lambda e: e.activation(out=V[:, i, :, 0:64], in_=pV[:].rearrange("p (h d) -> p h d", h=NH),
                                           func=AF.Copy, scale=smt[:, 1:2]), reads=[BpV, Bs], writes=[BV[i]])

    def kv_B2(i):
        par = i % 2
        misc = ps[4 + par]
        BmC = BmiscS[par]
        pT, BpT = ps[6 + par], Bps[6 + par]
        smt, Bs = sm[i % NSM], Bsm[i % NSM]
        P.op("pe", lambda e: e.matmul(misc[:, 16:24], lhsT=tri[:], rhs=smt[:, 16:24], start=True, stop=(i == 0)),
             reads=[Bs, Bconst], writes=[BmC], inc=(i == 0))
        if i > 0:
            P.op("pe", lambda e: e.matmul(misc[:, 16:24], lhsT=Emat[:], rhs=negc[:, i - 1, :], start=False, stop=True),
                 reads=[Bnegc[i - 1], Bconst], writes=[BmC], inc=True)
        P.op("dve", lambda e: e.tensor_copy(out=negc[:, i, :], in_=misc[:, 16:24]), reads=[BmC], writes=[Bnegc[i]])
        pTb = pT[:].bitcast(BF16).rearrange("p (h t) -> p h t", h=NH)
        for h in range(NH):
            P.op("pe", lambda e, h=h: e.transpose(out=pTb[0:65, h, :], in_=Kaug[par][:, h, :], identity=ident[:]),
                 reads=[BKaug[par], Bconst], writes=[BpT], inc=(h == NH - 1))
        P.op("dve", lambda e: e.tensor_copy(out=KT[:, :, i * 128:(i + 1) * 128], in_=pTb[0:65, :, :]),
             reads=[BpT], writes=[BKT[i]])

    def q_A(t):
        b = load_cast(xT_own_v, t, 32 + t)
        par = t % 2
        pQ, BpQ = ps[t % 4], Bps[t % 4]
        misc = ps[4 + par]
        rms_stats_pe(b, misc, BmiscS[par])
        for kc in range(8):
            st, sp_ = (kc == 0), (kc == 7)
            P.op("pe", lambda e, kc=kc, st=st, sp_=sp_: e.matmul(pQ[:], lhsT=xb[b][:, kc, :], rhs=Wqkv[:, kc, 0:512], start=st, stop=sp_),
                 reads=[Bxb[b], BWq], writes=[BpQ], inc=sp_)

    def q_B1(t):
        par = t % 2
        pQ, BpQ = ps[t % 4], Bps[t % 4]
        misc = ps[4 + par]
        smt, Bs = sm[t % NSM], Bsm[t % NSM]
        rms_stats_act(misc, BmiscS[par], smt, Bs)
        head_norm_scale(pQ, BpQ, smt, Bs, sqk[par], Bsqk[par])
        P.op("dve", lambda e: e.tensor_tensor(out=Kaug[par][:, :, 0:64], in0=pQ[:].rearrange("p (h d) -> p h d", h=NH),
                                              in1=bc_mid(smt[:, 40:48], 64), op=ALU.mult),
             reads=[BpQ, Bs], writes=[BKaug[par]])
        P.op("dve", lambda e: e.tensor_tensor(out=seltmp[:], in0=negc[:], in1=bc_mid(selw[:, t, :], NH), op=ALU.mult),
             reads=Bnegc + [Bconst], writes=[Bseltmp])
        P.op("dve", lambda e: e.tensor_reduce(out=smt[:, 48:56], in_=seltmp[:].rearrange("p i h -> p h i"), axis=AX.X, op=ALU.add),
             reads=[Bseltmp], writes=[Bs])
        P.op("dve", lambda e: e.tensor_scalar(out=Kaug[par][:, :, 64:65], in0=smt[:, 48:56].unsqueeze(2), scalar1=-1.0, scalar2=None,
                                              op0=ALU.mult), reads=[Bs], writes=[BKaug[par]])

    def q_B2(t):
        par = t % 2
        pT, BpT = ps[6 + par], Bps[6 + par]
        pTb = pT[:].bitcast(BF16).rearrange("p (h t) -> p h t", h=NH)
        for h in range(NH):
            P.op("pe", lambda e, h=h: e.transpose(out=pTb[0:65, h, :], in_=Kaug[par][:, h, :], identity=ident[:]),
                 reads=[BKaug[par], Bconst], writes=[BpT], inc=(h == NH - 1))
        P.op("act", lambda e: e.activation(out=QT[:, :, t * 128:(t + 1) * 128], in_=pTb[0:65, :, :], func=AF.Copy,
                                           scale=gqkT[0:65, 0:1]), reads=[BpT, Bconst], writes=[BQT[t]])

    tiles = [(kv_A, kv_B1, kv_B2, i) for i in range(nkv)] + [(q_A, q_B1, q_B2, t) for t in range(nq)]
    if stage == "k":
        tiles = tiles[:nkv]
    tiles[0][0](tiles[0][3])
    LAGQ = True
    for n_, (fa, fb1, fb2, ix) in enumerate(tiles):
        if n_ + 1 < len(tiles):
            tiles[n_ + 1][0](tiles[n_ + 1][3])
        fb1(ix)
        if n_ < nkv or not LAGQ:
            fb2(ix)
        elif n_ - 1 >= nkv:
            tiles[n_ - 1][2](tiles[n_ - 1][3])
        if n_ == 2:
            load_piece(0, BWq)
        if n_ == 4:
            load_piece(1, BWq)
    if LAGQ and len(tiles) > nkv:
        tiles[-1][2](tiles[-1][3])
    if stage == "k":
        return finish_debug([("negc", negc[:], [128, 32, NH], F32), ("KT", KT[:, :, 0:nkv * 128], [65, NH, nkv * 128], BF16),
                             ("V", V[:, 0:nkv], [128, nkv, NH, 65], BF16)])
    if stage == "q":
        return finish_debug([("QT", QT[:, :, 0:nq * 128], [65, NH, nq * 128], BF16)])
    P.barrier()
    A.release(m_p1)

    m_p2 = A.mark()
    NPT = 4
    PT = [A.alloc([128, 512], BF16) for _ in range(NPT)]
    BPT = [Buf("PT%d" % i) for i in range(NPT)]
    maskt = A.alloc([128, 2, 8, 512], BF16)
    Osb = [A.alloc([65, 512], F32) for _ in range(2)]
    BOsb = [Buf("Osb0"), Buf("Osb1")]
    rden = [A.alloc([65, 512], F32) for _ in range(2)]
    Brden = [Buf("rden0"), Buf("rden1")]
    Bmask = Buf("mask")
    P.dma("sp", lambda e: e.dma_start(out=maskt[:], in_=mask_d), writes=[Bmask])

    PSB = (0, 1, 2, 7)
    LA = 3
    steps = []
    hj_of = {}
    for h in range(NH):
        for j in range(4):
            hj_of[(h, j)] = len(hj_of)
            for kb in range(8 * (j + 1)):
                steps.append((h, j, kb))
    nst = len(steps)

    def emit_qk(s_):
        h, j, kb = steps[s_]
        pS, BpS = ps[PSB[s_ % 4]], Bps[PSB[s_ % 4]]
        mk = kb - 8 * j
        P.op("pe", lambda e: e.matmul(pS[:], lhsT=KT[:, h, kb * 128:(kb + 1) * 128],
                                      rhs=QT[:, h, j * 512:(j + 1) * 512], start=True, stop=(mk < 0)),
             reads=[BKT[kb]] + BQT[4 * j:4 * j + 4], writes=[BpS], inc=(mk < 0))
        if mk >= 0:
            P.op("pe", lambda e: e.matmul(pS[:], lhsT=ident[:], rhs=maskt[:, j % 2, mk, :], start=False, stop=True),
                 reads=[Bmask, Bconst], writes=[BpS], inc=True)

    def emit_exp_pv(s_):
        h, j, kb = steps[s_]
        nkb = 8 * (j + 1)
        hj = hj_of[(h, j)]
        pS, BpS = ps[PSB[s_ % 4]], Bps[PSB[s_ % 4]]
        pt, Bpt = PT[s_ % NPT], BPT[s_ % NPT]
        pO, BpO = ps[3 + (hj % 2)], Bps[3 + (hj % 2)]
        P.op("act", lambda e: e.activation(out=pt[:], in_=pS[:], func=AF.Exp, bias=negc[:, kb, h:h + 1], scale=1.0),
             reads=[BpS, Bnegc[kb]], writes=[Bpt])
        P.op("pe", lambda e: e.matmul(pO[0:65, :], lhsT=V[:, kb, h, :], rhs=pt[:], start=(kb == 0), stop=(kb == nkb - 1)),
             reads=[Bpt, BV[kb]], writes=[BpO], inc=(kb == nkb - 1))

    def norm_a(h, j):
        hj = hj_of[(h, j)]
        pO, BpO = ps[3 + (hj % 2)], Bps[3 + (hj % 2)]
        ob, Bob = Osb[hj % 2], BOsb[hj % 2]
        rd, Brd = rden[hj % 2], Brden[hj % 2]
        P.op("dve", lambda e: e.tensor_copy(out=ob[:], in_=pO[0:65, :]), reads=[BpO], writes=[Bob])
        P.op("act", lambda e: e.activation(out=rd[64:65, :], in_=ob[64:65, :], func=AF.Ln), reads=[Bob], writes=[Brd])
        P.op("act", lambda e: e.activation(out=rd[64:65, :], in_=rd[64:65, :], func=AF.Exp, scale=-1.0), reads=[Brd], writes=[Brd])

    def norm_b(h, j):
        hj = hj_of[(h, j)]
        ob, Bob = Osb[hj % 2], BOsb[hj % 2]
        rd, Brd = rden[hj % 2], Brden[hj % 2]
        pB, BpB = ps[5 + (hj % 2)], Bps[5 + (hj % 2)]
        P.op("pe", lambda e: e.matmul(pB[0:64, :], lhsT=ones_f[64:65, 0:64], rhs=rd[64:65, :], start=True, stop=True),
             reads=[Brd, Bconst], writes=[BpB], inc=True)
        P.op("dve", lambda e: e.tensor_tensor(out=yT[:, h, j * 512:(j + 1) * 512], in0=ob[0:64, :], in1=pB[0:64, :], op=ALU.mult),
             reads=[Bob, BpB], writes=[ByT[j]])

    for s_ in range(min(LA, nst)):
        emit_qk(s_)
    deferred = []
    for s_ in range(nst):
        if s_ + LA < nst:
            emit_qk(s_ + LA)
        emit_exp_pv(s_)
        h, j, kb = steps[s_]
        if kb == 8 * (j + 1) - 1:
            norm_a(h, j)
            deferred.append((s_ + 4, h, j))
        while deferred and deferred[0][0] <= s_:
            _, h2_, j2_ = deferred.pop(0)
            norm_b(h2_, j2_)
    for _, h2_, j2_ in deferred:
        norm_b(h2_, j2_)
    P.barrier()
    A.release(m_p2)

    if stage == 2:
        Bd = Buf("dbg")
        P.dma("sp", lambda e: e.dma_start(out=dbg, in_=yT[:]), reads=ByT, writes=[Bd])
        P.barrier()
        P.emit()
        return nc

    A.release(C0 + 65536)
    NSTG = 3
    stg = [A.alloc([128, 2048], F32) for _ in range(NSTG)]
    Bstg = [Buf("stg%d" % i) for i in range(NSTG)]
    stg_n = [0]

    wbufs = {}

    def load_w(dst, src, K, N, gT=None, parts=128, key=None):
        i = stg_n[0] % NSTG
        stg_n[0] += 1
        st_ = stg[i][0:parts, 0:K * N].rearrange("p (k n) -> p k n", k=K)
        Bst = Bstg[i]
        if key is None:
            Bd_ = Buf("w")
        else:
            Bd_ = wbufs.setdefault(key, Buf("w" + key))
        P.dma("sp", lambda e: e.dma_start(out=st_, in_=src), writes=[Bst])
        if gT is None:
            P.op("act", lambda e: e.activation(out=dst, in_=st_, func=AF.Copy), reads=[Bst], writes=[Bd_])
        else:
            for kc in range(K):
                P.op("act", lambda e, kc=kc: e.activation(out=dst[:, kc, :], in_=st_[:, kc, :], func=AF.Copy, scale=gT[:, kc:kc + 1]),
                     reads=[Bst, Bconst], writes=[Bd_])
        return Bd_

    def rnorm_chunk(src_f32, Bsrc, dst_bf, Bdst, sqc, Bsqc, Rt, BR, pR, BpR, ntok):
        Bsrc_l = Bsrc if isinstance(Bsrc, list) else [Bsrc]
        P.op("act", lambda e: e.activation(out=sqc, in_=src_f32, func=AF.Square), reads=Bsrc_l, writes=[Bsqc])
        for kc in range(8):
            P.op("pe", lambda e, kc=kc: e.matmul(pR[:, 0:ntok], lhsT=ones_bf[:], rhs=sqc[:, kc, :], start=(kc == 0), stop=(kc == 7)),
                 reads=[Bsqc, Bconst], writes=[BpR], inc=(kc == 7))
        P.op("act", lambda e: e.activation(out=Rt[:, 0:ntok], in_=pR[:, 0:ntok], func=AF.Ln, bias=EPS, scale=1.0 / D), reads=[BpR], writes=[BR])
        P.op("act", lambda e: e.activation(out=Rt[:, 0:ntok], in_=Rt[:, 0:ntok], func=AF.Exp, scale=-0.5), reads=[BR], writes=[BR])
        P.op("dve", lambda e: e.tensor_tensor(out=dst_bf, in0=src_f32, in1=Rt[:, 0:ntok].unsqueeze(1).to_broadcast([128, 8, ntok]), op=ALU.mult),
             reads=Bsrc_l + [BR], writes=[Bdst])

    m_p3 = A.mark()
    h1T = A.alloc([128, 8, NTOK], BF16)
    Bh1T = [Buf("h1T%d" % j) for j in range(4)]
    hhT = A.alloc([128, 8, 8], BF16)
    BhhT = Buf("hhT")
    BmT_ = None
    m_3a = A.mark()
    xc = [A.alloc([128, 8, 512], F32) for _ in range(2)]
    Bxc = [Buf("xc0"), Buf("xc1")]
    sqc_l = [A.alloc([128, 8, 512], BF16) for _ in range(2)]
    Bsqc_l = [Buf("sqc0"), Buf("sqc1")]
    Rt_l = [A.alloc([128, 512], F32) for _ in range(2)]
    BR_l = [Buf("R0"), Buf("R1")]
    sqc, Bsqc, Rt, BR = sqc_l[0], Bsqc_l[0], Rt_l[0], BR_l[0]
    xh = A.alloc([128, 8, 8], F32)
    Bxh = Buf("xh")

    def p3a_a(j):
        b = j % 2
        P.dma("sp", lambda e: e.dma_start(out=xc[b][:], in_=xT_own_v[:, :, j * 512:(j + 1) * 512]), writes=[Bxc[b]])
        P.op("act", lambda e: e.activation(out=sqc_l[b][:], in_=xc[b][:], func=AF.Square), reads=[Bxc[b]], writes=[Bsqc_l[b]])
        pR, BpR = ps[b], Bps[b]
        for kc in range(8):
            P.op("pe", lambda e, kc=kc: e.matmul(pR[:], lhsT=ones_bf[:], rhs=sqc_l[b][:, kc, :], start=(kc == 0), stop=(kc == 7)),
                 reads=[Bsqc_l[b], Bconst], writes=[BpR], inc=(kc == 7))

    def p3a_b(j):
        b = j % 2
        pR, BpR = ps[b], Bps[b]
        P.op("act", lambda e: e.activation(out=Rt_l[b][:], in_=pR[:], func=AF.Ln, bias=EPS, scale=1.0 / D), reads=[BpR], writes=[BR_l[b]])
        P.op("act", lambda e: e.activation(out=Rt_l[b][:], in_=Rt_l[b][:], func=AF.Exp, scale=-0.5), reads=[BR_l[b]], writes=[BR_l[b]])
        P.op("dve", lambda e: e.tensor_tensor(out=h1T[:, :, j * 512:(j + 1) * 512], in0=xc[b][:],
                                              in1=Rt_l[b][:].unsqueeze(1).to_broadcast([128, 8, 512]), op=ALU.mult),
             reads=[Bxc[b], BR_l[b]], writes=[Bh1T[j]])
    p3a_a(0)
    for j in range(4):
        if j + 1 < 4:
            p3a_a(j + 1)
        p3a_b(j)
    P.dma("sp", lambda e: e.dma_start(out=xh[:], in_=xhT.rearrange("(k p) t -> p k t", p=128)), writes=[Bxh])
    rnorm_chunk(xh[:], Bxh, hhT[:], BhhT, sqc[:, :, 0:8], Bsqc, Rt, BR, ps[2], Bps[2], 8)
    P.barrier()
    if stage == "3a":
        return finish_debug([("h1T", h1T[:], [128, 8, NTOK], BF16), ("hhT", hhT[:], [128, 8, 8], BF16)])
    A.release(m_3a)

    BmT = A.alloc([128, 4, NTOK], BF16)
    BBmT = [Buf("BmT%d" % j) for j in range(4)]
    m_3b = A.mark()
    convw = A.alloc([128, 4, 3], F32)
    P.dma("sp", lambda e: e.dma_start(out=convw[:], in_=conv_d), writes=[Bconst])
    wcv = [[A.alloc([128, 8, 128], BF16) for _ in range(3)] for _ in range(2)]
    ubuf = [A.alloc([128, 514], F32) for _ in range(2)]
    Bubuf = [Buf("u0"), Buf("u1")]
    cct = [A.alloc([128, 514], F32) for _ in range(2)]
    Bcct = [Buf("cct0"), Buf("cct1")]
    tcv = [A.alloc([128, 512], F32) for _ in range(2)]
    Btcv = [Buf("tcv0"), Buf("tcv1")]

    def p3b(ci, j, n, Bw):
        b = n % 2
        pcb, pcc, pcu = ps[3 * b], ps[3 * b + 1], ps[3 * b + 2]
        Bpcb, Bpcc, Bpcu = Bps[3 * b], Bps[3 * b + 1], Bps[3 * b + 2]
        ph, Bph = ps[6 + b], Bps[6 + b]
        w3 = wcv[ci % 2]
        for wi, (pp, Bpp) in enumerate(((pcb, Bpcb), (pcc, Bpcc), (pcu, Bpcu))):
            for kc in range(8):
                P.op("pe", lambda e, kc=kc, wi=wi, pp=pp: e.matmul(pp[:], lhsT=w3[wi][:, kc, :], rhs=h1T[:, kc, j * 512:(j + 1) * 512],
                                                                    start=(kc == 0), stop=(kc == 7)),
                     reads=[Bw[wi], Bh1T[j]], writes=[Bpp], inc=(kc == 7))
        for wi in (1, 2):
            for kc in range(8):
                P.op("pe", lambda e, kc=kc, wi=wi: e.matmul(ph[:, 2 * wi:2 * wi + 2], lhsT=w3[wi][:, kc, :], rhs=hhT[:, kc, 2 * j:2 * j + 2],
                                                             start=(kc == 0), stop=(kc == 7)),
                     reads=[Bw[wi], BhhT], writes=[Bph], inc=(kc == 7))
        ct, Bct = cct[b], Bcct[b]
        ub, Bub = ubuf[b], Bubuf[b]
        tv, Btv = tcv[b], Btcv[b]
        P.op("act", lambda e: e.activation(out=ct[:, 2:514], in_=pcc[:], func=AF.Copy), reads=[Bpcc], writes=[Bct])
        P.op("act", lambda e: e.activation(out=ct[:, 0:2], in_=ph[:, 2:4], func=AF.Copy), reads=[Bph], writes=[Bct])
        P.op("dve", lambda e: e.tensor_tensor(out=ub[:, 2:514], in0=ct[:, 2:514], in1=pcu[:], op=ALU.mult), reads=[Bct, Bpcu], writes=[Bub])
        P.op("dve", lambda e: e.tensor_tensor(out=ub[:, 0:2], in0=ct[:, 0:2], in1=ph[:, 4:6], op=ALU.mult), reads=[Bct, Bph], writes=[Bub])
        P.op("dve", lambda e: e.tensor_scalar(out=tv[:], in0=ub[:, 0:512], scalar1=convw[:, ci, 0:1], scalar2=None, op0=ALU.mult),
             reads=[Bub, Bconst], writes=[Btv])
        P.op("dve", lambda e: e.scalar_tensor_tensor(out=tv[:], in0=ub[:, 1:513], scalar=convw[:, ci, 1:2], in1=tv[:], op0=ALU.mult, op1=ALU.add),
             reads=[Bub, Btv, Bconst], writes=[Btv])
        P.op("dve", lambda e: e.scalar_tensor_tensor(out=tv[:], in0=ub[:, 2:514], scalar=convw[:, ci, 2:3], in1=tv[:], op0=ALU.mult, op1=ALU.add),
             reads=[Bub, Btv, Bconst], writes=[Btv])
        P.op("dve", lambda e: e.tensor_tensor(out=BmT[:, ci, j * 512:(j + 1) * 512], in0=tv[:], in1=pcb[:], op=ALU.mult),
             reads=[Btv, Bpcb], writes=[BBmT[j]])

    def load_3b(ci):
        Bw_ = []
        for wi, base in enumerate((1544, 2056, 2568)):
            c0 = base + ci * 128
            Bw_.append(load_w(wcv[ci % 2][wi][:], w_in_v[:, :, c0:c0 + 128], 8, 128, g1T, key="cv%d_%d" % (ci % 2, wi)))
        return Bw_
    n3b = 0
    Bw_next = load_3b(0)
    for ci in range(4):
        Bw = Bw_next
        if ci + 1 < 4:
            Bw_next = load_3b(ci + 1)
        for j in range(4):
            p3b(ci, j, n3b, Bw)
            n3b += 1
    P.barrier()
    if stage == "3b":
        return finish_debug([("BmT", BmT[:], [128, 4, NTOK], BF16)])
    A.release(m_3b)

    mT = nc.alloc_sbuf_tensor_at("mT", [128, 8, NTOK], BF16, offset=SB_END - 32768)
    BmTb = [Buf("mT%d" % j) for j in range(4)]
    m_3c = A.mark()
    wga = [A.alloc([128, 8, 128], BF16) for _ in range(2)]
    wgb = [A.alloc([128, 8, 128], BF16) for _ in range(2)]
    wA = [A.alloc([64, 8, 128], BF16) for _ in range(2)]
    wB = [A.alloc([128, 4, 128], BF16) for _ in range(2)]
    sga = [A.alloc([128, 512], F32) for _ in range(2)]
    sgb = [A.alloc([128, 512], F32) for _ in range(2)]
    Bsga = [Buf("sga0"), Buf("sga1")]
    Bsgb = [Buf("sgb0"), Buf("sgb1")]
    w_oa_v = w_oa.rearrange("(h p) n -> p h n", p=64)
    w_oc_v = w_oc.rearrange("(k p) n -> p k n", p=128)

    def p3c(m, j, n, Bw):
        b = n % 2
        pga, pgb, pA, pB_ = ps[4 * b], ps[4 * b + 1], ps[4 * b + 2], ps[4 * b + 3]
        Bpga, Bpgb, BpA, BpB_ = Bps[4 * b], Bps[4 * b + 1], Bps[4 * b + 2], Bps[4 * b + 3]
        wb_ = m % 2
        tok = slice(j * 512, (j + 1) * 512)
        for kc in range(8):
            P.op("pe", lambda e, kc=kc: e.matmul(pga[:], lhsT=wga[wb_][:, kc, :], rhs=h1T[:, kc, tok], start=(kc == 0), stop=(kc == 7)),
                 reads=[Bw[0], Bh1T[j]], writes=[Bpga], inc=(kc == 7))
        for kc in range(8):
            P.op("pe", lambda e, kc=kc: e.matmul(pgb[:], lhsT=wgb[wb_][:, kc, :], rhs=h1T[:, kc, tok], start=(kc == 0), stop=(kc == 7)),
                 reads=[Bw[1], Bh1T[j]], writes=[Bpgb], inc=(kc == 7))
        for h in range(8):
            P.op("pe", lambda e, h=h: e.matmul(pA[:], lhsT=wA[wb_][:, h, :], rhs=yT[:, h, tok], start=(h == 0), stop=(h == 7)),
                 reads=[Bw[2], ByT[j]], writes=[BpA], inc=(h == 7))
        for kc in range(4):
            P.op("pe", lambda e, kc=kc: e.matmul(pB_[:], lhsT=wB[wb_][:, kc, :], rhs=BmT[:, kc, tok], start=(kc == 0), stop=(kc == 3)),
                 reads=[Bw[3], BBmT[j]], writes=[BpB_], inc=(kc == 3))
        P.op("act", lambda e: e.activation(out=sga[b][:], in_=pga[:], func=AF.Sigmoid), reads=[Bpga], writes=[Bsga[b]])
        P.op("act", lambda e: e.activation(out=sgb[b][:], in_=pgb[:], func=AF.Sigmoid), reads=[Bpgb], writes=[Bsgb[b]])
        P.op("dve", lambda e: e.tensor_tensor(out=sga[b][:], in0=sga[b][:], in1=pA[:], op=ALU.mult), reads=[Bsga[b], BpA], writes=[Bsga[b]])
        P.op("dve", lambda e: e.tensor_tensor(out=sgb[b][:], in0=sgb[b][:], in1=pB_[:], op=ALU.mult), reads=[Bsgb[b], BpB_], writes=[Bsgb[b]])
        P.op("dve", lambda e: e.tensor_tensor(out=mT[:, m, tok], in0=sga[b][:], in1=sgb[b][:], op=ALU.add),
             reads=[Bsga[b], Bsgb[b]], writes=[BmTb[j]])

    def load_3c(m):
        c = m * 128
        return [load_w(wga[m % 2][:], w_in_v[:, :, 3080 + c:3080 + c + 128], 8, 128, g1T, key="ga%d" % (m % 2)),
                load_w(wgb[m % 2][:], w_in_v[:, :, 4104 + c:4104 + c + 128], 8, 128, g1T, key="gb%d" % (m % 2)),
                load_w(wA[m % 2][:], w_oa_v[:, :, c:c + 128], 8, 128, None, parts=64, key="wA%d" % (m % 2)),
                load_w(wB[m % 2][:], w_oc_v[:, :, c:c + 128], 4, 128, None, key="wB%d" % (m % 2))]
    n3c = 0
    Bw_next = load_3c(0)
    for m in range(8):
        Bw = Bw_next
        if m + 1 < 8:
            Bw_next = load_3c(m + 1)
        for j in range(4):
            p3c(m, j, n3c, Bw)
            n3c += 1
    P.barrier()
    A.release(m_p3)
    if stage == "3c":
        return finish_debug([("mT", mT[:], [128, 8, NTOK], BF16)])


    xT = nc.alloc_sbuf_tensor_at("xTres", [128, 8, NTOK], F32, offset=C0)
    BxT = [[Buf("xT%d_%d" % (m, j)) for j in range(4)] for m in range(8)]
    m_3d = A.mark()
    wo = [A.alloc([128, 8, 128], BF16) for _ in range(2)]
    w_o_v = w_o.rearrange("(k p) n -> p k n", p=128)

    def p3d(m, j, n, Bw):
        pp, Bpp = ps[n % 4], Bps[n % 4]
        tok = slice(j * 512, (j + 1) * 512)
        for kc in range(8):
            P.op("pe", lambda e, kc=kc: e.matmul(pp[:], lhsT=wo[m % 2][:, kc, :], rhs=mT[:, kc, tok], start=(kc == 0), stop=(kc == 7)),
                 reads=[Bw, BmTb[j]], writes=[Bpp], inc=(kc == 7))
        P.op("dve", lambda e: e.tensor_tensor(out=xT[:, m, tok], in0=pp[:], in1=xT[:, m, tok], op=ALU.add),
             reads=[Bpp, BxT[m][j]], writes=[BxT[m][j]])

    def p3d_load(m):
        Bw_ = load_w(wo[m % 2][:], w_o_v[:, :, m * 128:(m + 1) * 128], 8, 128, None, key="wo%d" % (m % 2))
        P.dma("sp", lambda e: e.dma_start(out=xT[:, m, :], in_=xT_own[m * 128:(m + 1) * 128, :]), writes=BxT[m])
        return Bw_
    Bw_nx = p3d_load(0)
    for m in range(8):
        Bw_cur = Bw_nx
        if m + 1 < 8:
            Bw_nx = p3d_load(m + 1)
        for j in range(4):
            p3d(m, j, m * 4 + j, Bw_cur)
    P.barrier()
    A.release(m_3d)

    h2T = A.alloc([128, 8, NTOK], BF16)
    Bh2T = [Buf("h2T%d" % j) for j in range(4)]
    comb = A.alloc([128, 16, 16], F32)
    Bcomb = [Buf("comb%d" % t) for t in range(16)]
    m_3e = A.mark()
    sqc2 = A.alloc([128, 8, 512], BF16)
    Bsqc2 = Buf("sqc2")
    Rt2 = A.alloc([128, 512], F32)
    BR2 = Buf("R2")
    Wr = A.alloc([128, 8, 20], BF16)
    br_t = A.alloc([128, 20], F32)
    BWr = load_w(Wr[:], w_r.rearrange("(k p) n -> p k n", p=128), 8, 20, g2T)
    P.dma("sp", lambda e: e.dma_start(out=br_t[:], in_=br_d), writes=[Bconst])

    def p3e(j):
        tok = slice(j * 512, (j + 1) * 512)
        rnorm_chunk(xT[:, :, tok], [BxT[m][j] for m in range(8)], h2T[:, :, tok], Bh2T[j], sqc2[:], Bsqc2, Rt2, BR2, ps[j % 2], Bps[j % 2], 512)
    for j in range(4):
        p3e(j)

    pl, Bpl = ps[4], Bps[4]

    def route_mm(t):
        for kc in range(8):
            P.op("pe", lambda e, kc=kc: e.matmul(pl[:, t * 20:(t + 1) * 20], lhsT=h2T[:, kc, t * 128:(t + 1) * 128], rhs=Wr[:, kc, :],
                                                 start=(kc == 0), stop=(kc == 7)),
                 reads=[Bh2T[t // 4], BWr], writes=[Bpl], inc=(kc == 7))
    for t in range(16):
        route_mm(t)

    def ra(n):
        return A.alloc([128, 16, n], F32)
    Lb, dg, ge, oh, tmpr, ein, d1, mk1, e2, d2, sel, wv = ra(20), ra(4), ra(4), ra(4), ra(16), ra(4), ra(4), ra(4), ra(4), ra(4), ra(4), ra(4)
    gmax, gsum, gval, m1, m2, wsum, sc = (A.alloc([128, 16], F32) for _ in range(7))
    BRr = Buf("route")

    def dv(fn, rd=()):
        P.op("dve", fn, reads=[BRr] + list(rd), writes=[BRr])

    def bc4(ap2):
        return ap2.unsqueeze(2).to_broadcast([128, 16, 4])
    dv(lambda e: e.tensor_tensor(out=Lb[:], in0=pl[:, 0:320].rearrange("p (t c) -> p t c", c=20),
                                 in1=br_t[:].unsqueeze(1).to_broadcast([128, 16, 20]), op=ALU.add), rd=[Bpl, Bconst])
    dv(lambda e: e.tensor_reduce(out=gmax[:], in_=Lb[:, :, 0:4], axis=AX.X, op=ALU.max))
    dv(lambda e: e.tensor_tensor(out=dg[:], in0=Lb[:, :, 0:4], in1=bc4(gmax[:]), op=ALU.subtract))
    P.op("act", lambda e: e.activation(out=ge[:], in_=dg[:], func=AF.Exp), reads=[BRr], writes=[BRr])
    dv(lambda e: e.tensor_reduce(out=gsum[:], in_=ge[:], axis=AX.X, op=ALU.add))
    dv(lambda e: e.reciprocal(out=gval[:], in_=gsum[:]))
    dv(lambda e: e.tensor_scalar(out=oh[:], in0=dg[:], scalar1=0.0, scalar2=None, op0=ALU.is_equal))
    dv(lambda e: e.tensor_tensor(out=tmpr[:].rearrange("p t (g j) -> p t g j", g=4), in0=Lb[:, :, 4:20].rearrange("p t (g j) -> p t g j", g=4),
                                 in1=oh[:].unsqueeze(3).to_broadcast([128, 16, 4, 4]), op=ALU.mult))
    dv(lambda e: e.tensor_reduce(out=ein[:], in_=tmpr[:].rearrange("p t (g j) -> p t j g", g=4), axis=AX.X, op=ALU.add))
    dv(lambda e: e.tensor_reduce(out=m1[:], in_=ein[:], axis=AX.X, op=ALU.max))
    dv(lambda e: e.tensor_tensor(out=d1[:], in0=ein[:], in1=bc4(m1[:]), op=ALU.subtract))
    dv(lambda e: e.tensor_scalar(out=mk1[:], in0=d1[:], scalar1=0.0, scalar2=None, op0=ALU.is_equal))
    dv(lambda e: e.scalar_tensor_tensor(out=e2[:], in0=mk1[:], scalar=-1e30, in1=d1[:], op0=ALU.mult, op1=ALU.add))
    dv(lambda e: e.tensor_reduce(out=m2[:], in_=e2[:], axis=AX.X, op=ALU.max))
    dv(lambda e: e.tensor_tensor(out=d2[:], in0=e2[:], in1=bc4(m2[:]), op=ALU.subtract))
    dv(lambda e: e.tensor_scalar(out=sel[:], in0=d2[:], scalar1=0.0, scalar2=None, op0=ALU.is_equal))
    dv(lambda e: e.tensor_tensor(out=sel[:], in0=sel[:], in1=mk1[:], op=ALU.add))
    P.op("act", lambda e: e.activation(out=wv[:], in_=d1[:], func=AF.Exp), reads=[BRr], writes=[BRr])
    dv(lambda e: e.tensor_tensor(out=wv[:], in0=wv[:], in1=sel[:], op=ALU.mult))
    dv(lambda e: e.tensor_reduce(out=wsum[:], in_=wv[:], axis=AX.X, op=ALU.add))
    dv(lambda e: e.reciprocal(out=sc[:], in_=wsum[:]))
    dv(lambda e: e.tensor_tensor(out=sc[:], in0=sc[:], in1=gval[:], op=ALU.mult))
    dv(lambda e: e.tensor_tensor(out=wv[:], in0=wv[:], in1=bc4(sc[:]), op=ALU.mult))
    P.op("dve", lambda e: e.tensor_tensor(out=comb[:].rearrange("p t (g j) -> p t g j", g=4),
                                          in0=oh[:].unsqueeze(3).to_broadcast([128, 16, 4, 4]),
                                          in1=wv[:].unsqueeze(2).to_broadcast([128, 16, 4, 4]), op=ALU.mult),
         reads=[BRr], writes=Bcomb)
    P.barrier()
    if stage == "3e":
        return finish_debug([("xT", xT[:], [128, 8, NTOK], F32), ("comb", comb[:], [128, 16, 16], F32)])
    A.release(m_3e)

    m_p4 = A.mark()
    Wgu = [A.alloc([128, 8, 512], BF16) for _ in range(2)]
    Wd = [A.alloc([128, 2, 1024], BF16) for _ in range(2)]
    BWg = [Buf("Wg0"), Buf("Wg1")]
    BWu = [Buf("Wu0"), Buf("Wu1")]
    BWd = [Buf("Wd0"), Buf("Wd1")]
    mstg = [A.alloc([128, 2048], F32) for _ in range(6)]
    Bmstg = [Buf("mstg%d" % i) for i in range(6)]
    sa = [A.alloc([128, 256], F32) for _ in range(2)]
    Bsa = [Buf("sa0"), Buf("sa1")]
    hid = [A.alloc([128, 256], BF16) for _ in range(2)]
    Bhid = [Buf("hid0"), Buf("hid1")]
    hidT = [A.alloc([128, 2, 512], BF16) for _ in range(2)]
    BhidT = [Buf("hidT0"), Buf("hidT1")]

    def moe_wdma(e):
        base = (e % 2) * 3
        srcs = (w_gate[e].rearrange("(k p) n -> p k n", p=128), w_up[e].rearrange("(k p) n -> p k n", p=128),
                w_down[e].rearrange("(k p) n -> p k n", p=128))
        shp = ((8, 256), (8, 256), (2, 1024))
        for q_ in range(3):
            K_, N_ = shp[q_]
            st_ = mstg[base + q_][:, 0:K_ * N_].rearrange("p (k n) -> p k n", k=K_)
            P.dma("sp", lambda e_, st_=st_, src=srcs[q_]: e_.dma_start(out=st_, in_=src), writes=[Bmstg[base + q_]])

    def moe_wcast_ops(e):
        base = (e % 2) * 3
        we = e % 2
        sg_ = mstg[base][:, 0:2048].rearrange("p (k n) -> p k n", k=8)
        su_ = mstg[base + 1][:, 0:2048].rearrange("p (k n) -> p k n", k=8)
        sd_ = mstg[base + 2][:, 0:2048].rearrange("p (k n) -> p k n", k=2)
        ops_ = []
        for kc in range(8):
            ops_.append(lambda kc=kc: P.op("act", lambda e_: e_.activation(out=Wgu[we][:, kc, 0:256], in_=sg_[:, kc, :], func=AF.Copy,
                                                                      scale=g2T[:, kc:kc + 1]),
                                           reads=[Bmstg[base], Bconst], writes=[BWg[we]]))
        for kc in range(8):
            ops_.append(lambda kc=kc: P.op("act", lambda e_: e_.activation(out=Wgu[we][:, kc, 256:512], in_=su_[:, kc, :], func=AF.Copy,
                                                                      scale=g2T[:, kc:kc + 1]),
                                           reads=[Bmstg[base + 1], Bconst], writes=[BWu[we]]))
        for fc in range(2):
            ops_.append(lambda fc=fc: P.op("act", lambda e_: e_.activation(out=Wd[we][:, fc, :], in_=sd_[:, fc, :], func=AF.Copy),
                                           reads=[Bmstg[base + 2]], writes=[BWd[we]]))
        return ops_

    def moe_wcast(e):
        for f_ in moe_wcast_ops(e):
            f_()

    def moe_A(u):
        e, t = u // 16, u % 16
        j = t // 4
        we = e % 2
        pau, Bpau = ps[u % 3], Bps[u % 3]
        for kc in range(8):
            P.op("pe", lambda e_, kc=kc: e_.matmul(pau[:], lhsT=h2T[:, kc, t * 128:(t + 1) * 128], rhs=Wgu[we][:, kc, :],
                                                   start=(kc == 0), stop=(kc == 7)),
                 reads=[Bh2T[j], BWg[we], BWu[we]], writes=[Bpau], inc=(kc == 7))

    def moe_B(u):
        e, t = u // 16, u % 16
        j, tt_ = t // 4, t % 4
        b = u % 2
        pau, Bpau = ps[u % 3], Bps[u % 3]
        ptr, Bptr = ps[3 + b], Bps[3 + b]
        P.op("act", lambda e_: e_.activation(out=sa[b][:], in_=pau[:, 0:256], func=AF.Silu), reads=[Bpau], writes=[Bsa[b]])
        P.op("dve", lambda e_: e_.scalar_tensor_tensor(out=hid[b][:], in0=sa[b][:], scalar=comb[:, t, e:e + 1], in1=pau[:, 256:512],
                                                       op0=ALU.mult, op1=ALU.mult), reads=[Bsa[b], Bpau, Bcomb[t]], writes=[Bhid[b]])
        ptb = ptr[:].bitcast(BF16).rearrange("p (f t) -> p f t", t=128)
        for fc in range(2):
            P.op("pe", lambda e_, fc=fc: e_.transpose(out=ptb[:, fc, :], in_=hid[b][:, fc * 128:(fc + 1) * 128], identity=ident[:]),
                 reads=[Bhid[b], Bconst], writes=[Bptr], inc=(fc == 1))
        hb = (e * 4 + j) % 2
        P.op("act", lambda e_: e_.activation(out=hidT[hb][:, :, tt_ * 128:(tt_ + 1) * 128], in_=ptb[:, 0:2, :], func=AF.Copy),
             reads=[Bptr], writes=[BhidT[hb]])

    dn_n = [0]

    def moe_C1(e, j, m):
        we = e % 2
        hb = (e * 4 + j) % 2
        tok = slice(j * 512, (j + 1) * 512)
        n = dn_n[0]
        dn_n[0] += 1
        pd, Bpd = ps[5 + n % 3], Bps[5 + n % 3]
        for fc in range(2):
            P.op("pe", lambda e_, fc=fc: e_.matmul(pd[:], lhsT=Wd[we][:, fc, m * 128:(m + 1) * 128], rhs=hidT[hb][:, fc, :],
                                                   start=(fc == 0), stop=(fc == 1)),
                 reads=[BWd[we], BhidT[hb]], writes=[Bpd], inc=(fc == 1))
        P.op("dve", lambda e_: e_.tensor_tensor(out=xT[:, m, tok], in0=pd[:], in1=xT[:, m, tok], op=ALU.add),
             reads=[Bpd, BxT[m][j]], writes=[BxT[m][j]])

    NU = NE * 16
    moe_wdma(0)
    moe_wcast(0)
    moe_wdma(1)
    moe_A(0)
    moe_A(1)
    pendC = []
    pendW = []
    for u in range(NU):
        e, t = u // 16, u % 16
        if u + 2 < NU:
            moe_A(u + 2)
        moe_B(u)
        k_ = 0
        while pendC and pendC[0][0] <= u and k_ < 2:
            _, e2_, j2_, m2_ = pendC.pop(0)
            moe_C1(e2_, j2_, m2_)
            k_ += 1
        if t % 4 == 3:
            for m_ in range(8):
                pendC.append((u + 1, e, t // 4, m_))
        if t == 5:
            assert not [c for c in pendC if c[1] < e]
            if e + 1 < NE:
                pendW = moe_wcast_ops(e + 1)
            if e + 2 < NE:
                moe_wdma(e + 2)
        for _ in range(2):
            if pendW:
                pendW.pop(0)()
    for _, e2_, j2_, m2_ in pendC:
        moe_C1(e2_, j2_, m2_)
    P.barrier()
    if stage == "4":
        return finish_debug([("xT", xT[:], [128, 8, NTOK], F32)])
    A.release(m_p4)
    A.release(C0 + 65536)

    stg5 = [A.alloc([128, 2048], F32) for _ in range(NSTG)]
    for i_ in range(NSTG):
        stg[i_] = stg5[i_]
    Wpg = A.alloc([128, 8, 1024], BF16)
    Wple = A.alloc([128, 2, 1024], BF16)
    h3T = [A.alloc([128, 8, 512], BF16) for _ in range(2)]
    Bh3T = [Buf("h3T0"), Buf("h3T1")]
    sqc3 = A.alloc([128, 8, 512], BF16)
    Bsqc3 = Buf("sqc3")
    Rt3 = A.alloc([128, 512], F32)
    BR3 = Buf("R3")
    pst = [A.alloc([128, 2, 512], F32) for _ in range(2)]
    Bpst = [Buf("pst0"), Buf("pst1")]
    ptb5 = [A.alloc([128, 2, 512], BF16) for _ in range(2)]
    Bptb5 = [Buf("ptb0"), Buf("ptb1")]
    sg = [A.alloc([128, 512], F32) for _ in range(2)]
    Bsg = [Buf("sg0"), Buf("sg1")]
    ost = [A.alloc([128, 512], F32) for _ in range(3)]
    Bost = [Buf("ost%d" % i) for i in range(3)]
    w_pg_v = w_pg.rearrange("(k p) n -> p k n", p=128)
    pT_v = pT_own.rearrange("(k p) t -> p k t", p=128)
    Bout = Buf("out")

    def p5_m(j, m, n):
        b = j % 2
        tok = slice(j * 512, (j + 1) * 512)
        ppg, Bppg = ps[2 + 2 * (n % 3)], Bps[2 + 2 * (n % 3)]
        ppe, Bppe = ps[3 + 2 * (n % 3)], Bps[3 + 2 * (n % 3)]
        for kc in range(8):
            P.op("pe", lambda e, kc=kc: e.matmul(ppg[:], lhsT=Wpg[:, kc, m * 128:(m + 1) * 128], rhs=h3T[b][:, kc, :], start=(kc == 0), stop=(kc == 7)),
                 reads=[BWpg[m // 2], Bh3T[b]], writes=[Bppg], inc=(kc == 7))
        for kc in range(2):
            P.op("pe", lambda e, kc=kc: e.matmul(ppe[:], lhsT=Wple[:, kc, m * 128:(m + 1) * 128], rhs=ptb5[b][:, kc, :], start=(kc == 0), stop=(kc == 1)),
                 reads=[BWple, Bptb5[b]], writes=[Bppe], inc=(kc == 1))
        sb_, o_ = n % 2, n % 3
        P.op("act", lambda e: e.activation(out=sg[sb_][:], in_=ppg[:], func=AF.Sigmoid), reads=[Bppg], writes=[Bsg[sb_]])
        P.op("dve", lambda e: e.tensor_tensor(out=sg[sb_][:], in0=sg[sb_][:], in1=ppe[:], op=ALU.mult), reads=[Bsg[sb_], Bppe], writes=[Bsg[sb_]])
        P.op("dve", lambda e: e.tensor_tensor(out=ost[o_][:], in0=sg[sb_][:], in1=xT[:, m, tok], op=ALU.add),
             reads=[Bsg[sb_], BxT[m][j]], writes=[Bost[o_]])
        P.dma("sp", lambda e: e.dma_start(out=outT[m * 128:(m + 1) * 128, tok], in_=ost[o_][:]), reads=[Bost[o_]], writes=[Buf("o")])

    def p5_pre(j):
        b = j % 2
        tok = slice(j * 512, (j + 1) * 512)
        P.dma("sp", lambda e: e.dma_start(out=pst[b][:], in_=pT_v[:, :, tok]), writes=[Bpst[b]])
        P.op("pool", lambda e: e.tensor_copy(out=ptb5[b][:], in_=pst[b][:]), reads=[Bpst[b]], writes=[Bptb5[b]])
        rnorm_chunk(xT[:, :, tok], [BxT[m][j] for m in range(8)], h3T[b][:], Bh3T[b], sqc3[:], Bsqc3, Rt3, BR3, ps[j % 2], Bps[j % 2], 512)

    p5_pre(0)
    BWpg = [load_w(Wpg[:, :, c * 256:(c + 1) * 256], w_pg_v[:, :, c * 256:(c + 1) * 256], 8, 256, g3T) for c in range(4)]
    BWple = load_w(Wple[:], w_ple.rearrange("(k p) n -> p k n", p=128), 2, 1024, None)
    for j in range(4):
        if j + 1 < 4:
            p5_pre(j + 1)
        for m in range(8):
            p5_m(j, m, j * 8 + m)
    P.barrier()
    P.emit()
    return nc


def make_masks():
    k = np.arange(128)[:, None]
    q = np.arange(512)[None, :]

    def diag(jb):
        return np.where((jb * 128 + k) <= q, 0.0, -30000.0).astype(np.float32)
    ones = np.zeros((128, 512), np.float32)
    zeros = np.full((128, 512), -30000.0, np.float32)
    E = [diag(0), diag(1), diag(2), diag(3), zeros, zeros, zeros, zeros]
    O = [ones, ones, ones, ones, diag(0), diag(1), diag(2), diag(3)]
    return np.stack(E, 0), np.stack(O, 0)


def prep_inputs(inp):
    x = np.asarray(inp["x"], np.float32)
    p = np.asarray(inp["p"], np.float32)[0]
    E, O = make_masks()
    shared = {
        "w_in": np.ascontiguousarray(inp["w_in"][0]),
        "w_oa": np.ascontiguousarray(inp["w_out_att"][0]),
        "w_oc": np.ascontiguousarray(inp["w_out_conv"][0]),
        "w_o": np.ascontiguousarray(inp["w_o"][0]),
        "w_r": np.ascontiguousarray(np.concatenate([inp["w_rg"][0], inp["w_re"][0]], axis=1)),
        "w_gate": np.ascontiguousarray(inp["w_gate"][0]),
        "w_up": np.ascontiguousarray(inp["w_up"][0]),
        "w_down": np.ascontiguousarray(inp["w_down"][0]),
        "w_pg": np.ascontiguousarray(inp["w_pg"][0]),
        "w_ple": np.ascontiguousarray(inp["w_ple"][0]),
        "g1T": np.ascontiguousarray(inp["attn_norm_g"][0].reshape(8, 128).T),
        "g2T": np.ascontiguousarray(inp["ffn_norm_g"][0].reshape(8, 128).T),
        "g3T": np.ascontiguousarray(inp["ple_norm_g"][0].reshape(8, 128).T),
        "bf_bc": np.ascontiguousarray(np.broadcast_to(inp["b_f"][0][None, :], (128, 8))),
        "gq_col": np.ascontiguousarray(inp["q_norm_g"][0].reshape(64, 1)),
        "gk_col": np.ascontiguousarray(inp["k_norm_g"][0].reshape(64, 1)),
        "convT": np.ascontiguousarray(inp["conv_w"][0].reshape(3, 4, 128).transpose(2, 1, 0)),
        "br_bc": np.ascontiguousarray(np.broadcast_to(
            np.concatenate([inp["b_rg"][0], inp["b_re"][0]])[None, :], (128, 20))),
    }
    shared = {k: np.asarray(v, np.float32) for k, v in shared.items()}
    maps = []
    for c in range(8):
        b, par = c // 2, c % 2
        chunks = CHUNKS[par]
        xb_T = np.ascontiguousarray(x[b].T)
        own_cols = np.concatenate([np.arange(ci * 512, (ci + 1) * 512) for ci in chunks])
        xh = np.zeros((D, 8), np.float32)
        for j, ci in enumerate(chunks):
            if ci > 0:
                xh[:, 2 * j:2 * j + 2] = xb_T[:, ci * 512 - 2:ci * 512]
        sel = np.zeros((16, 32), np.float32)
        for j, ci in enumerate(chunks):
            for tt in range(4):
                sel[4 * j + tt, 4 * ci + tt] = 1.0
        types = [(E, O)[ci % 2] for ci in chunks]
        mask2 = np.stack([types[0], types[1]], 0)
        assert np.array_equal(types[0], types[2]) and np.array_equal(types[1], types[3])
        m = dict(shared)
        m.update({
            "xT_all": xb_T,
            "xT_own": np.ascontiguousarray(xb_T[:, own_cols]),
            "xhT": xh,
            "pT_own": np.ascontiguousarray(p[b].T[:, own_cols]),
            "sel_own": np.ascontiguousarray(np.broadcast_to(sel[None], (128, 16, 32))),
            "mask2": np.ascontiguousarray(mask2.transpose(2, 0, 1, 3)).astype(ml_dtypes.bfloat16),
        })
        maps.append(m)
    return maps


_NC_CACHE = {}


def kernel(**inputs):
    maps = prep_inputs(inputs)
    if "nc" not in _NC_CACHE:
        _NC_CACHE["nc"] = build()
    nc = _NC_CACHE["nc"]
    res = run_bass_kernel_spmd(nc, maps, core_ids=list(range(8)))
    out = np.empty((4, S, D), np.float32)
    for c in range(8):
        b, par = c // 2, c % 2
        oT = np.asarray(res.results[c]["outT"])
        for j, ci in enumerate(CHUNKS[par]):
            out[b, ci * 512:(ci + 1) * 512, :] = oT[:, j * 512:(j + 1) * 512].T
    return out
```

```python
import numpy as np
import ml_dtypes
import concourse.bass as bass
import concourse.mybir as mybir
from concourse.bass_utils import run_bass_kernel_spmd

F32 = mybir.dt.float32
BF16 = mybir.dt.bfloat16
AF = mybir.ActivationFunctionType
ALU = mybir.AluOpType
AX = mybir.AxisListType

ENGS = ("pe", "act", "dve", "pool", "sp")
NDMASEM = 20
SB_BASE = 16512
SB_END = 229376 - 2048
EPS = 1e-6

D = 1024
S = 4096
NH = 8
HD = 64
NTOK = 2048
NE = 16
DE = 256
CHUNKS = ((0, 3, 4, 7), (1, 2, 5, 6))


class Tk:
    __slots__ = ("sem", "val")

    def __init__(self, sem, val):
        self.sem = sem
        self.val = val


class Buf:
    __slots__ = ("name", "w", "r")

    def __init__(self, name=""):
        self.name = name
        self.w = None
        self.r = []


class Prog:
    def __init__(self, nc):
        self.nc = nc
        self.ops = {e: [] for e in ENGS}
        self.cnt = {e: 0 for e in ENGS}
        self.seen = {e: {} for e in ENGS}
        self.pend = {e: [] for e in ENGS}
        self.dma_n = {e: 0 for e in ENGS}
        self.dma_last = {}

    def _need(self, eng, waits, t):
        if t is None:
            return
        if t.sem == "pe" and eng == "pe":
            return
        if t.val is None:
            raise RuntimeError("dependency on op without resolved ticket (missing inc)")
        if self.seen[eng].get(t.sem, 0) >= t.val:
            return
        if waits.get(t.sem, 0) < t.val:
            waits[t.sem] = t.val

    def _deps(self, eng, reads, writes, waits):
        for b in reads:
            self._need(eng, waits, b.w)
        for b in writes:
            self._need(eng, waits, b.w)
            for t in b.r:
                self._need(eng, waits, t)
        for s, v in waits.items():
            self.seen[eng][s] = v

    def _mark(self, tk, reads, writes):
        for b in reads:
            b.r.append(tk)
            if len(b.r) > 64:
                b.r = b.r[-48:]
        for b in writes:
            b.w = tk
            b.r = []

    def op(self, eng, fn, reads=(), writes=(), inc=True):
        waits = {}
        self._deps(eng, reads, writes, waits)
        if inc:
            self.cnt[eng] += 1
            tk = Tk(eng, self.cnt[eng])
            for p in self.pend[eng]:
                p.val = tk.val
            self.pend[eng] = []
        else:
            tk = Tk(eng, None)
            self.pend[eng].append(tk)
        self._mark(tk, reads, writes)
        self.ops[eng].append((fn, list(waits.items()), (eng, 1) if inc else None))
        return tk

    def dma(self, eng, fn, reads=(), writes=()):
        n = self.dma_n[eng]
        self.dma_n[eng] += 1
        semname = "d_%s_%d" % (eng, n % NDMASEM)
        waits = {}
        prev = self.dma_last.get(semname)
        if prev is not None:
            self._need(eng, waits, prev)
        self._deps(eng, reads, writes, waits)
        tk = Tk(semname, 16 * (n // NDMASEM + 1))
        self.dma_last[semname] = tk
        self._mark(tk, reads, writes)
        self.ops[eng].append((fn, list(waits.items()), (semname, 16)))
        return tk

    def barrier(self):
        for e in ENGS:
            assert not self.pend[e], "barrier with pending un-inc'ed ops on " + e
        tks = [Tk(e, self.cnt[e]) for e in ENGS if self.cnt[e] > 0]
        tks += list(self.dma_last.values())
        for e in ENGS:
            waits = {}
            for t in tks:
                if t.sem != e:
                    self._need(e, waits, t)
            for s, v in waits.items():
                self.seen[e][s] = v
            self.ops[e].append((None, list(waits.items()), None))

    def emit(self):
        nc = self.nc
        from contextlib import ExitStack
        semnames = set()
        for e in ENGS:
            for fn, waits, inc in self.ops[e]:
                for s, v in waits:
                    semnames.add(s)
                if inc:
                    semnames.add(inc[0])
        with ExitStack() as st:
            sems = {}
            for s in sorted(semnames):
                sems[s] = st.enter_context(nc.semaphore("s_" + s))
            block = st.enter_context(nc.Block())

            def run(e, engobj):
                for fn, waits, inc in self.ops[e]:
                    for s, v in waits:
                        engobj.wait_ge(sems[s], v)
                    if fn is None:
                        continue
                    ins = fn(engobj)
                    if inc:
                        ins.then_inc(sems[inc[0]], inc[1])

            @block.tensor
            def _(eng):
                run("pe", eng)

            @block.scalar
            def _(eng):
                run("act", eng)

            @block.vector
            def _(eng):
                run("dve", eng)

            @block.gpsimd
            def _(eng):
                run("pool", eng)

            @block.sync
            def _(eng):
                run("sp", eng)


class Arena:
    def __init__(self, nc):
        self.nc = nc
        self.off = SB_BASE
        self.n = 0

    def mark(self):
        return self.off

    def release(self, m):
        self.off = m

    def alloc(self, shape, dt):
        nbytes = int(np.prod(shape[1:])) * (4 if dt == F32 else 2)
        nbytes = (nbytes + 63) // 64 * 64
        assert self.off + nbytes <= SB_END, "SBUF overflow: %d + %d" % (self.off, nbytes)
        self.n += 1
        t = self.nc.alloc_sbuf_tensor_at("t%d" % self.n, list(shape), dt, offset=self.off)
        self.off += nbytes
        return t


def bc_mid(ap2, n):
    p, a = ap2.shape
    return ap2.unsqueeze(2).to_broadcast([p, a, n])


def build(stage=99, nkv=32, nq=16):
    nc = bass.Bass("TRN2", target_bir_lowering=False)
    P = Prog(nc)
    A = Arena(nc)

    def finish_debug(items):
        P.barrier()
        for name, ap, shape, dt in items:
            o = nc.dram_tensor(name, list(shape), dt, kind="ExternalOutput").ap()
            P.dma("sp", lambda e, o=o, ap=ap: e.dma_start(out=o, in_=ap))
        P.barrier()
        P.emit()
        return nc

    def din(name, shape, dt=F32):
        return nc.dram_tensor(name, list(shape), dt, kind="ExternalInput").ap()

    xT_all = din("xT_all", [D, S])
    xT_own = din("xT_own", [D, NTOK])
    xhT = din("xhT", [D, 8])
    pT_own = din("pT_own", [256, NTOK])
    w_in = din("w_in", [D, 5128])
    w_oa = din("w_oa", [512, D])
    w_oc = din("w_oc", [512, D])
    w_o = din("w_o", [D, D])
    w_r = din("w_r", [D, 20])
    w_gate = din("w_gate", [NE, D, DE])
    w_up = din("w_up", [NE, D, DE])
    w_down = din("w_down", [NE, DE, D])
    w_pg = din("w_pg", [D, D])
    w_ple = din("w_ple", [256, D])
    g1T_d = din("g1T", [128, 8])
    g2T_d = din("g2T", [128, 8])
    g3T_d = din("g3T", [128, 8])
    bf_d = din("bf_bc", [128, 8])
    gq_d = din("gq_col", [64, 1])
    gk_d = din("gk_col", [64, 1])
    conv_d = din("convT", [128, 4, 3])
    br_d = din("br_bc", [128, 20])
    sel_d = din("sel_own", [128, 16, 32])
    mask_d = din("mask2", [128, 2, 8, 512], BF16)
    if stage == 2:
        dbg = nc.dram_tensor("dbg", [64, 8, NTOK], BF16, kind="ExternalOutput").ap()
    elif stage == 99:
        outT = nc.dram_tensor("outT", [D, NTOK], F32, kind="ExternalOutput").ap()

    import os
    ps = [nc.alloc_psum_tensor("ps%d" % i, [128, 512], F32) for i in range(int(os.environ.get("NPS", "8")))]
    Bps = [Buf("ps%d" % i) for i in range(len(ps))]

    ident = A.alloc([128, 128], BF16)
    tmpf = A.alloc([128, 128], F32)
    tri = A.alloc([128, 128], F32)
    Emat = A.alloc([128, 128], F32)
    ones_bf = A.alloc([128, 128], BF16)
    ones_f = A.alloc([128, 64], F32)
    g1T = A.alloc([128, 8], F32)
    g2T = A.alloc([128, 8], F32)
    g3T = A.alloc([128, 8], F32)
    bf_bc = A.alloc([128, 8], F32)
    gqkT = A.alloc([65, 1], F32)
    gk_t = A.alloc([64, 1], F32)
    Bconst = Buf("const")
    Btmpf = Buf("tmpf")

    P.op("pool", lambda e: e.memset(tmpf[:], 1.0), writes=[Btmpf])
    P.op("pool", lambda e: e.affine_select(out=tmpf[:], in_=tmpf[:], pattern=[[-1, 128]], compare_op=ALU.is_equal,
                                           fill=0.0, base=0, channel_multiplier=1), reads=[Btmpf], writes=[Btmpf])
    P.op("dve", lambda e: e.tensor_copy(out=ident[:], in_=tmpf[:]), reads=[Btmpf], writes=[Bconst])
    P.op("pool", lambda e: e.memset(tri[:], 1.0), writes=[Bconst])
    P.op("pool", lambda e: e.affine_select(out=tri[:], in_=tri[:], pattern=[[1, 128]], compare_op=ALU.is_ge,
                                           fill=0.0, base=0, channel_multiplier=-1), reads=[Bconst], writes=[Bconst])
    P.op("pool", lambda e: e.memset(Emat[:], 1.0), writes=[Bconst])
    P.op("pool", lambda e: e.affine_select(out=Emat[:], in_=Emat[:], pattern=[[0, 128]], compare_op=ALU.is_equal,
                                           fill=0.0, base=-127, channel_multiplier=1), reads=[Bconst], writes=[Bconst])
    P.op("pool", lambda e: e.memset(ones_bf[:], 1.0), writes=[Bconst])
    P.op("pool", lambda e: e.memset(ones_f[:], 1.0), writes=[Bconst])
    P.op("pool", lambda e: e.memset(gqkT[:], 1.0), writes=[Bconst])
    P.dma("sp", lambda e: e.dma_start(out=gqkT[0:64, :], in_=gq_d), writes=[Bconst])
    for dst, src in ((g1T, g1T_d), (g2T, g2T_d), (g3T, g3T_d), (bf_bc, bf_d), (gk_t, gk_d)):
        P.dma("sp", lambda e, dst=dst, src=src: e.dma_start(out=dst[:], in_=src), writes=[Bconst])
    P.op("dve", lambda e: e.scalar_tensor_tensor(out=gqkT[0:64, :], in0=gqkT[0:64, :], scalar=HD ** -0.5, in1=gk_t[:],
                                                 op0=ALU.mult, op1=ALU.mult), reads=[Bconst], writes=[Bconst])
    P.barrier()
    if stage == "c":
        return finish_debug([("tri", tri[:], [128, 128], F32), ("Emat", Emat[:], [128, 128], F32),
                             ("ident", ident[:], [128, 128], BF16), ("gqk", gqkT[:], [65, 1], F32)])

    C0 = A.mark()
    yT = A.alloc([64, NH, NTOK], BF16)
    ByT = [Buf("yT%d" % j) for j in range(4)]
    m_attn = A.mark()
    KT = A.alloc([65, NH, S], BF16)
    QT = A.alloc([65, NH, NTOK], BF16)
    V = A.alloc([128, 32, NH, 65], BF16)
    negc = A.alloc([128, 32, NH], F32)
    BKT = [Buf("KT%d" % i) for i in range(32)]
    BQT = [Buf("QT%d" % i) for i in range(16)]
    BV = [Buf("V%d" % i) for i in range(32)]
    Bnegc = [Buf("negc%d" % i) for i in range(32)]

    m_p1 = A.mark()
    A.release(C0)
    Wqkv = A.alloc([128, 8, 1536], BF16)
    Wf = A.alloc([128, 8, 8], BF16)
    sqk = [A.alloc([128, 512], F32) for _ in range(2)]
    assert A.off <= C0 + 32768
    A.release(m_p1)
    wst = [A.alloc([128, 8, 256], F32) for _ in range(2)]
    Bwst = [Buf("wst0"), Buf("wst1")]
    BW = Buf("Wqkv")
    xst = [A.alloc([128, 8, 128], F32) for _ in range(2)]
    xb = [A.alloc([128, 8, 128], BF16) for _ in range(2)]
    sq = [A.alloc([128, 8, 128], BF16) for _ in range(2)]
    Bxst = [Buf("xst0"), Buf("xst1")]
    Bxb = [Buf("xb0"), Buf("xb1")]
    Bsq = [Buf("sq0"), Buf("sq1")]
    Bsqk = [Buf("sqk0"), Buf("sqk1")]
    Kaug = [A.alloc([128, NH, 65], BF16) for _ in range(2)]
    BKaug = [Buf("Kaug0"), Buf("Kaug1")]
    tmpq = A.alloc([128, 512], F32)
    Btmpq = Buf("tmpq")
    selw = A.alloc([128, 16, 32], F32)
    seltmp = A.alloc([128, 32, NH], F32)
    Bseltmp = Buf("seltmp")
    NSM = 4
    sm = [A.alloc([128, 64], F32) for _ in range(NSM)]
    Bsm = [Buf("sm%d" % i) for i in range(NSM)]

    w_in_v = w_in.rearrange("(k p) n -> p k n", p=128)
    BWq, BWk, BWv, BWf = Buf("Wq"), Buf("Wk"), Buf("Wv"), Buf("Wf")
    wpiece_n = [0]

    def load_piece(piece, Bdst):
        n_ = wpiece_n[0]
        wpiece_n[0] += 1
        wb = wst[n_ % 2]
        P.dma("sp", lambda e: e.dma_start(out=wb[:], in_=w_in_v[:, :, piece * 256:(piece + 1) * 256]), writes=[Bwst[n_ % 2]])
        for kc in range(8):
            P.op("act", lambda e, kc=kc: e.activation(out=Wqkv[:, kc, piece * 256:(piece + 1) * 256], in_=wb[:, kc, :], func=AF.Copy,
                                                      scale=g1T[:, kc:kc + 1]), reads=[Bwst[n_ % 2], Bconst], writes=[Bdst])

    def load_wf():
        n_ = wpiece_n[0]
        wpiece_n[0] += 1
        wb = wst[n_ % 2]
        P.dma("sp", lambda e: e.dma_start(out=wb[:, :, 0:8], in_=w_in_v[:, :, 1536:1544]), writes=[Bwst[n_ % 2]])
        for kc in range(8):
            P.op("act", lambda e, kc=kc: e.activation(out=Wf[:, kc, :], in_=wb[:, kc, 0:8], func=AF.Copy, scale=g1T[:, kc:kc + 1]),
                 reads=[Bwst[n_ % 2], Bconst], writes=[BWf])
    load_wf()
    load_piece(2, BWk)
    load_piece(3, BWk)
    load_piece(4, BWv)
    load_piece(5, BWv)
    P.dma("sp", lambda e: e.dma_start(out=selw[:], in_=sel_d), writes=[Bconst])
    for j in range(2):
        P.op("pool", lambda e, j=j: e.memset(Kaug[j][:, :, 64:65], 1.0), writes=[BKaug[j]])
    for i0 in range(0, 32, 8):
        P.op("pool", lambda e, i0=i0: e.memset(V[:, i0:i0 + 8, :, 64:65], 1.0), writes=[BV[i] for i in range(i0, i0 + 8)])

    xT_all_v = xT_all.rearrange("(k p) t -> p k t", p=128)
    xT_own_v = xT_own.rearrange("(k p) t -> p k t", p=128)

    def load_cast(src_v, gi, cnt):
        b = cnt % 2
        P.dma("sp", lambda e: e.dma_start(out=xst[b][:], in_=src_v[:, :, gi * 128:(gi + 1) * 128]), writes=[Bxst[b]])
        P.op("pool", lambda e: e.tensor_copy(out=xb[b][:], in_=xst[b][:]), reads=[Bxst[b]], writes=[Bxb[b]])
        P.op("act", lambda e: e.activation(out=sq[b][:], in_=xst[b][:], func=AF.Square), reads=[Bxst[b]], writes=[Bsq[b]])
        return b

    BmiscS = [Buf("mS0"), Buf("mS1")]
    BmiscF = [Buf("mF0"), Buf("mF1")]
    BmiscC = [Buf("mC0"), Buf("mC1")]

    def rms_stats_pe(b, misc, BmS):
        for kc in range(8):
            P.op("pe", lambda e, kc=kc: e.matmul(misc[:, 0:1], lhsT=sq[b][:, kc, :],
                                                  rhs=ones_bf[:, 0:1], start=(kc == 0), stop=(kc == 7)),
                 reads=[Bsq[b], Bconst], writes=[BmS], inc=(kc == 7))

    def rms_stats_act(misc, BmS, smt, Bs):
        P.op("act", lambda e: e.activation(out=smt[:, 0:1], in_=misc[:, 0:1], func=AF.Ln, bias=EPS, scale=1.0 / D),
             reads=[BmS], writes=[Bs])
        P.op("act", lambda e: e.activation(out=smt[:, 1:2], in_=smt[:, 0:1], func=AF.Exp, scale=-0.5), reads=[Bs], writes=[Bs])
        P.op("act", lambda e: e.activation(out=smt[:, 2:3], in_=smt[:, 0:1], func=AF.Exp, scale=-1.0,
                                           bias=float(-np.log(64.0))), reads=[Bs], writes=[Bs])

    def head_norm_scale(pX, BpX, smt, Bs, sqb, Bsqb):
        P.op("act", lambda e: e.activation(out=sqb[:], in_=pX[:], func=AF.Square), reads=[BpX], writes=[Bsqb])
        P.op("dve", lambda e: e.tensor_reduce(out=smt[:, 24:32], in_=sqb[:].rearrange("p (h d) -> p h d", h=NH),
                                              axis=AX.X, op=ALU.add), reads=[Bsqb], writes=[Bs])
        P.op("dve", lambda e: e.tensor_scalar(out=smt[:, 32:40], in0=smt[:, 24:32], scalar1=smt[:, 2:3], scalar2=None,
                                              op0=ALU.mult), reads=[Bs], writes=[Bs])
        P.op("act", lambda e: e.activation(out=smt[:, 32:40], in_=smt[:, 32:40], func=AF.Ln, bias=EPS), reads=[Bs], writes=[Bs])
        P.op("act", lambda e: e.activation(out=smt[:, 40:48], in_=smt[:, 32:40], func=AF.Exp, scale=-0.5), reads=[Bs], writes=[Bs])
        P.op("dve", lambda e: e.tensor_scalar(out=smt[:, 40:48], in0=smt[:, 40:48], scalar1=smt[:, 1:2], scalar2=None,
                                              op0=ALU.mult), reads=[Bs], writes=[Bs])

    if stage == "w":
        return finish_debug([("Wqkv", Wqkv[:], [128, 8, 1536], BF16), ("Wf", Wf[:], [128, 8, 8], BF16)])
    def kv_A(i):
        b = load_cast(xT_all_v, i, i)
        par = i % 2
        pK, BpK = ps[par], Bps[par]
        pV, BpV = ps[2 + par], Bps[2 + par]
        misc = ps[4 + par]
        BmS = BmiscS[par]
        for kc in range(8):
            st, sp_ = (kc == 0), (kc == 7)
            P.op("pe", lambda e, kc=kc, st=st, sp_=sp_: e.matmul(misc[:, 8:16], lhsT=xb[b][:, kc, :], rhs=Wf[:, kc, :], start=st, stop=sp_),
                 reads=[Bxb[b], BWf], writes=[BmS], inc=sp_)
        rms_stats_pe(b, misc, BmS)
        for kc in range(8):
            st, sp_ = (kc == 0), (kc == 7)
            P.op("pe", lambda e, kc=kc, st=st, sp_=sp_: e.matmul(pK[:], lhsT=xb[b][:, kc, :], rhs=Wqkv[:, kc, 512:1024], start=st, stop=sp_),
                 reads=[Bxb[b], BWk], writes=[BpK], inc=sp_)
            P.op("pe", lambda e, kc=kc, st=st, sp_=sp_: e.matmul(pV[:], lhsT=xb[b][:, kc, :], rhs=Wqkv[:, kc, 1024:1536], start=st, stop=sp_),
                 reads=[Bxb[b], BWv], writes=[BpV], inc=sp_)

    def kv_B1(i):
        par = i % 2
        pK, BpK = ps[par], Bps[par]
        pV, BpV = ps[2 + par], Bps[2 + par]
        misc = ps[4 + par]
        BmS = BmF = BmiscS[par]
        smt, Bs = sm[i % NSM], Bsm[i % NSM]
        rms_stats_act(misc, BmS, smt, Bs)
        P.op("dve", lambda e: e.scalar_tensor_tensor(out=smt[:, 8:16], in0=misc[:, 8:16], scalar=smt[:, 1:2], in1=bf_bc[:],
                                                     op0=ALU.mult, op1=ALU.add), reads=[BmF, Bs, Bconst], writes=[Bs])
        P.op("act", lambda e: e.activation(out=smt[:, 16:24], in_=smt[:, 8:16], func=AF.Exp, scale=-1.0), reads=[Bs], writes=[Bs])
        P.op("act", lambda e: e.activation(out=smt[:, 16:24], in_=smt[:, 16:24], func=AF.Ln, bias=1.0), reads=[Bs], writes=[Bs])
        head_norm_scale(pK, BpK, smt, Bs, sqk[par], Bsqk[par])
        P.op("dve", lambda e: e.tensor_tensor(out=Kaug[par][:, :, 0:64], in0=pK[:].rearrange("p (h d) -> p h d", h=NH),
                                              in1=bc_mid(smt[:, 40:48], 64), op=ALU.mult),
             reads=[BpK, Bs], writes=[BKaug[par]])
        P.op("act", lambda e: e.activation(out=V[:, i, :, 0:64], in_=pV[:].rearrange("p (h d) -> p h d", h=NH),
                                           func=AF.Copy, scale=smt[:, 1:2]), reads=[BpV, Bs], writes=[BV[i]])

    def kv_B2(i):
        par = i % 2
        misc = ps[4 + par]
        BmC = BmiscS[par]
        pT, BpT = ps[6 + par], Bps[6 + par]
        smt, Bs = sm[i % NSM], Bsm[i % NSM]
        P.op("pe", lambda e: e.matmul(misc[:, 16:24], lhsT=tri[:], rhs=smt[:, 16:24], start=True, stop=(i == 0)),
             reads=[Bs, Bconst], writes=[BmC], inc=(i == 0))
        if i > 0:
            P.op("pe", lambda e: e.matmul(misc[:, 16:24], lhsT=Emat[:], rhs=negc[:, i - 1, :], start=False, stop=True),
                 reads=[Bnegc[i - 1], Bconst], writes=[BmC], inc=True)
        P.op("dve", lambda e: e.tensor_copy(out=negc[:, i, :], in_=misc[:, 16:24]), reads=[BmC], writes=[Bnegc[i]])
        pTb = pT[:].bitcast(BF16).rearrange("p (h t) -> p h t", h=NH)
        for h in range(NH):
            P.op("pe", lambda e, h=h: e.transpose(out=pTb[0:65, h, :], in_=Kaug[par][:, h, :], identity=ident[:]),
                 reads=[BKaug[par], Bconst], writes=[BpT], inc=(h == NH - 1))
        P.op("dve", lambda e: e.tensor_copy(out=KT[:, :, i * 128:(i + 1) * 128], in_=pTb[0:65, :, :]),
             reads=[BpT], writes=[BKT[i]])

    def q_A(t):
        b = load_cast(xT_own_v, t, 32 + t)
        par = t % 2
        pQ, BpQ = ps[t % 4], Bps[t % 4]
        misc = ps[4 + par]
        rms_stats_pe(b, misc, BmiscS[par])
        for kc in range(8):
            st, sp_ = (kc == 0), (kc == 7)
            P.op("pe", lambda e, kc=kc, st=st, sp_=sp_: e.matmul(pQ[:], lhsT=xb[b][:, kc, :], rhs=Wqkv[:, kc, 0:512], start=st, stop=sp_),
                 reads=[Bxb[b], BWq], writes=[BpQ], inc=sp_)

    def q_B1(t):
        par = t % 2
        pQ, BpQ = ps[t % 4], Bps[t % 4]
        misc = ps[4 + par]
        smt, Bs = sm[t % NSM], Bsm[t % NSM]
        rms_stats_act(misc, BmiscS[par], smt, Bs)
        head_norm_scale(pQ, BpQ, smt, Bs, sqk[par], Bsqk[par])
        P.op("dve", lambda e: e.tensor_tensor(out=Kaug[par][:, :, 0:64], in0=pQ[:].rearrange("p (h d) -> p h d", h=NH),
                                              in1=bc_mid(smt[:, 40:48], 64), op=ALU.mult),
             reads=[BpQ, Bs], writes=[BKaug[par]])
        P.op("dve", lambda e: e.tensor_tensor(out=seltmp[:], in0=negc[:], in1=bc_mid(selw[:, t, :], NH), op=ALU.mult),
             reads=Bnegc + [Bconst], writes=[Bseltmp])
        P.op("dve", lambda e: e.tensor_reduce(out=smt[:, 48:56], in_=seltmp[:].rearrange("p i h -> p h i"), axis=AX.X, op=ALU.add),
             reads=[Bseltmp], writes=[Bs])
        P.op("dve", lambda e: e.tensor_scalar(out=Kaug[par][:, :, 64:65], in0=smt[:, 48:56].unsqueeze(2), scalar1=-1.0, scalar2=None,
                                              op0=ALU.mult), reads=[Bs], writes=[BKaug[par]])

    def q_B2(t):
        par = t % 2
        pT, BpT = ps[6 + par], Bps[6 + par]
        pTb = pT[:].bitcast(BF16).rearrange("p (h t) -> p h t", h=NH)
        for h in range(NH):
            P.op("pe", lambda e, h=h: e.transpose(out=pTb[0:65, h, :], in_=Kaug[par][:, h, :], identity=ident[:]),
                 reads=[BKaug[par], Bconst], writes=[BpT], inc=(h == NH - 1))
        P.op("act", lambda e: e.activation(out=QT[:, :, t * 128:(t + 1) * 128], in_=pTb[0:65, :, :], func=AF.Copy,
                                           scale=gqkT[0:65, 0:1]), reads=[BpT, Bconst], writes=[BQT[t]])

    tiles = [(kv_A, kv_B1, kv_B2, i) for i in range(nkv)] + [(q_A, q_B1, q_B2, t) for t in range(nq)]
    if stage == "k":
        tiles = tiles[:nkv]
    tiles[0][0](tiles[0][3])
    LAGQ = True
    for n_, (fa, fb1, fb2, ix) in enumerate(tiles):
        if n_ + 1 < len(tiles):
            tiles[n_ + 1][0](tiles[n_ + 1][3])
        fb1(ix)
        if n_ < nkv or not LAGQ:
            fb2(ix)
        elif n_ - 1 >= nkv:
            tiles[n_ - 1][2](tiles[n_ - 1][3])
        if n_ == 2:
            load_piece(0, BWq)
        if n_ == 4:
            load_piece(1, BWq)
    if LAGQ and len(tiles) > nkv:
        tiles[-1][2](tiles[-1][3])
    if stage == "k":
        return finish_debug([("negc", negc[:], [128, 32, NH], F32), ("KT", KT[:, :, 0:nkv * 128], [65, NH, nkv * 128], BF16),
                             ("V", V[:, 0:nkv], [128, nkv, NH, 65], BF16)])
    if stage == "q":
        return finish_debug([("QT", QT[:, :, 0:nq * 128], [65, NH, nq * 128], BF16)])
    P.barrier()
    A.release(m_p1)

    m_p2 = A.mark()
    NPT = 4
    PT = [A.alloc([128, 512], BF16) for _ in range(NPT)]
    BPT = [Buf("PT%d" % i) for i in range(NPT)]
    maskt = A.alloc([128, 2, 8, 512], BF16)
    Osb = [A.alloc([65, 512], F32) for _ in range(2)]
    BOsb = [Buf("Osb0"), Buf("Osb1")]
    rden = [A.alloc([65, 512], F32) for _ in range(2)]
    Brden = [Buf("rden0"), Buf("rden1")]
    Bmask = Buf("mask")
    P.dma("sp", lambda e: e.dma_start(out=maskt[:], in_=mask_d), writes=[Bmask])

    PSB = (0, 1, 2, 7)
    LA = 3
    steps = []
    hj_of = {}
    for h in range(NH):
        for j in range(4):
            hj_of[(h, j)] = len(hj_of)
            for kb in range(8 * (j + 1)):
                steps.append((h, j, kb))
    nst = len(steps)

    def emit_qk(s_):
        h, j, kb = steps[s_]
        pS, BpS = ps[PSB[s_ % 4]], Bps[PSB[s_ % 4]]
        mk = kb - 8 * j
        P.op("pe", lambda e: e.matmul(pS[:], lhsT=KT[:, h, kb * 128:(kb + 1) * 128],
                                      rhs=QT[:, h, j * 512:(j + 1) * 512], start=True, stop=(mk < 0)),
             reads=[BKT[kb]] + BQT[4 * j:4 * j + 4], writes=[BpS], inc=(mk < 0))
        if mk >= 0:
            P.op("pe", lambda e: e.matmul(pS[:], lhsT=ident[:], rhs=maskt[:, j % 2, mk, :], start=False, stop=True),
                 reads=[Bmask, Bconst], writes=[BpS], inc=True)

    def emit_exp_pv(s_):
        h, j, kb = steps[s_]
        nkb = 8 * (j + 1)
        hj = hj_of[(h, j)]
        pS, BpS = ps[PSB[s_ % 4]], Bps[PSB[s_ % 4]]
        pt, Bpt = PT[s_ % NPT], BPT[s_ % NPT]
        pO, BpO = ps[3 + (hj % 2)], Bps[3 + (hj % 2)]
        P.op("act", lambda e: e.activation(out=pt[:], in_=pS[:], func=AF.Exp, bias=negc[:, kb, h:h + 1], scale=1.0),
             reads=[BpS, Bnegc[kb]], writes=[Bpt])
        P.op("pe", lambda e: e.matmul(pO[0:65, :], lhsT=V[:, kb, h, :], rhs=pt[:], start=(kb == 0), stop=(kb == nkb - 1)),
             reads=[Bpt, BV[kb]], writes=[BpO], inc=(kb == nkb - 1))

    def norm_a(h, j):
        hj = hj_of[(h, j)]
        pO, BpO = ps[3 + (hj % 2)], Bps[3 + (hj % 2)]
        ob, Bob = Osb[hj % 2], BOsb[hj % 2]
        rd, Brd = rden[hj % 2], Brden[hj % 2]
        P.op("dve", lambda e: e.tensor_copy(out=ob[:], in_=pO[0:65, :]), reads=[BpO], writes=[Bob])
        P.op("act", lambda e: e.activation(out=rd[64:65, :], in_=ob[64:65, :], func=AF.Ln), reads=[Bob], writes=[Brd])
        P.op("act", lambda e: e.activation(out=rd[64:65, :], in_=rd[64:65, :], func=AF.Exp, scale=-1.0), reads=[Brd], writes=[Brd])

    def norm_b(h, j):
        hj = hj_of[(h, j)]
        ob, Bob = Osb[hj % 2], BOsb[hj % 2]
        rd, Brd = rden[hj % 2], Brden[hj % 2]
        pB, BpB = ps[5 + (hj % 2)], Bps[5 + (hj % 2)]
        P.op("pe", lambda e: e.matmul(pB[0:64, :], lhsT=ones_f[64:65, 0:64], rhs=rd[64:65, :], start=True, stop=True),
             reads=[Brd, Bconst], writes=[BpB], inc=True)
        P.op("dve", lambda e: e.tensor_tensor(out=yT[:, h, j * 512:(j + 1) * 512], in0=ob[0:64, :], in1=pB[0:64, :], op=ALU.mult),
             reads=[Bob, BpB], writes=[ByT[j]])

    for s_ in range(min(LA, nst)):
        emit_qk(s_)
    deferred = []
    for s_ in range(nst):
        if s_ + LA < nst:
            emit_qk(s_ + LA)
        emit_exp_pv(s_)
        h, j, kb = steps[s_]
        if kb == 8 * (j + 1) - 1:
            norm_a(h, j)
            deferred.append((s_ + 4, h, j))
        while deferred and deferred[0][0] <= s_:
            _, h2_, j2_ = deferred.pop(0)
            norm_b(h2_, j2_)
    for _, h2_, j2_ in deferred:
        norm_b(h2_, j2_)
    P.barrier()
    A.release(m_p2)

    if stage == 2:
        Bd = Buf("dbg")
        P.dma("sp", lambda e: e.dma_start(out=dbg, in_=yT[:]), reads=ByT, writes=[Bd])
        P.barrier()
        P.emit()
        return nc

    A.release(C0 + 65536)
    NSTG = 3
    stg = [A.alloc([128, 2048], F32) for _ in range(NSTG)]
    Bstg = [Buf("stg%d" % i) for i in range(NSTG)]
    stg_n = [0]

    wbufs = {}

    def load_w(dst, src, K, N, gT=None, parts=128, key=None):
        i = stg_n[0] % NSTG
        stg_n[0] += 1
        st_ = stg[i][0:parts, 0:K * N].rearrange("p (k n) -> p k n", k=K)
        Bst = Bstg[i]
        if key is None:
            Bd_ = Buf("w")
        else:
            Bd_ = wbufs.setdefault(key, Buf("w" + key))
        P.dma("sp", lambda e: e.dma_start(out=st_, in_=src), writes=[Bst])
        if gT is None:
            P.op("act", lambda e: e.activation(out=dst, in_=st_, func=AF.Copy), reads=[Bst], writes=[Bd_])
        else:
            for kc in range(K):
                P.op("act", lambda e, kc=kc: e.activation(out=dst[:, kc, :], in_=st_[:, kc, :], func=AF.Copy, scale=gT[:, kc:kc + 1]),
                     reads=[Bst, Bconst], writes=[Bd_])
        return Bd_

    def rnorm_chunk(src_f32, Bsrc, dst_bf, Bdst, sqc, Bsqc, Rt, BR, pR, BpR, ntok):
        Bsrc_l = Bsrc if isinstance(Bsrc, list) else [Bsrc]
        P.op("act", lambda e: e.activation(out=sqc, in_=src_f32, func=AF.Square), reads=Bsrc_l, writes=[Bsqc])
        for kc in range(8):
            P.op("pe", lambda e, kc=kc: e.matmul(pR[:, 0:ntok], lhsT=ones_bf[:], rhs=sqc[:, kc, :], start=(kc == 0), stop=(kc == 7)),
                 reads=[Bsqc, Bconst], writes=[BpR], inc=(kc == 7))
        P.op("act", lambda e: e.activation(out=Rt[:, 0:ntok], in_=pR[:, 0:ntok], func=AF.Ln, bias=EPS, scale=1.0 / D), reads=[BpR], writes=[BR])
        P.op("act", lambda e: e.activation(out=Rt[:, 0:ntok], in_=Rt[:, 0:ntok], func=AF.Exp, scale=-0.5), reads=[BR], writes=[BR])
        P.op("dve", lambda e: e.tensor_tensor(out=dst_bf, in0=src_f32, in1=Rt[:, 0:ntok].unsqueeze(1).to_broadcast([128, 8, ntok]), op=ALU.mult),
             reads=Bsrc_l + [BR], writes=[Bdst])

    m_p3 = A.mark()
    h1T = A.alloc([128, 8, NTOK], BF16)
    Bh1T = [Buf("h1T%d" % j) for j in range(4)]
    hhT = A.alloc([128, 8, 8], BF16)
    BhhT = Buf("hhT")
    BmT_ = None
    m_3a = A.mark()
    xc = [A.alloc([128, 8, 512], F32) for _ in range(2)]
    Bxc = [Buf("xc0"), Buf("xc1")]
    sqc_l = [A.alloc([128, 8, 512], BF16) for _ in range(2)]
    Bsqc_l = [Buf("sqc0"), Buf("sqc1")]
    Rt_l = [A.alloc([128, 512], F32) for _ in range(2)]
    BR_l = [Buf("R0"), Buf("R1")]
    sqc, Bsqc, Rt, BR = sqc_l[0], Bsqc_l[0], Rt_l[0], BR_l[0]
    xh = A.alloc([128, 8, 8], F32)
    Bxh = Buf("xh")

    def p3a_a(j):
        b = j % 2
        P.dma("sp", lambda e: e.dma_start(out=xc[b][:], in_=xT_own_v[:, :, j * 512:(j + 1) * 512]), writes=[Bxc[b]])
        P.op("act", lambda e: e.activation(out=sqc_l[b][:], in_=xc[b][:], func=AF.Square), reads=[Bxc[b]], writes=[Bsqc_l[b]])
        pR, BpR = ps[b], Bps[b]
        for kc in range(8):
            P.op("pe", lambda e, kc=kc: e.matmul(pR[:], lhsT=ones_bf[:], rhs=sqc_l[b][:, kc, :], start=(kc == 0), stop=(kc == 7)),
                 reads=[Bsqc_l[b], Bconst], writes=[BpR], inc=(kc == 7))

    def p3a_b(j):
        b = j % 2
        pR, BpR = ps[b], Bps[b]
        P.op("act", lambda e: e.activation(out=Rt_l[b][:], in_=pR[:], func=AF.Ln, bias=EPS, scale=1.0 / D), reads=[BpR], writes=[BR_l[b]])
        P.op("act", lambda e: e.activation(out=Rt_l[b][:], in_=Rt_l[b][:], func=AF.Exp, scale=-0.5), reads=[BR_l[b]], writes=[BR_l[b]])
        P.op("dve", lambda e: e.tensor_tensor(out=h1T[:, :, j * 512:(j + 1) * 512], in0=xc[b][:],
                                              in1=Rt_l[b][:].unsqueeze(1).to_broadcast([128, 8, 512]), op=ALU.mult),
             reads=[Bxc[b], BR_l[b]], writes=[Bh1T[j]])
    p3a_a(0)
    for j in range(4):
        if j + 1 < 4:
            p3a_a(j + 1)
        p3a_b(j)
    P.dma("sp", lambda e: e.dma_start(out=xh[:], in_=xhT.rearrange("(k p) t -> p k t", p=128)), writes=[Bxh])
    rnorm_chunk(xh[:], Bxh, hhT[:], BhhT, sqc[:, :, 0:8], Bsqc, Rt, BR, ps[2], Bps[2], 8)
    P.barrier()
    if stage == "3a":
        return finish_debug([("h1T", h1T[:], [128, 8, NTOK], BF16), ("hhT", hhT[:], [128, 8, 8], BF16)])
    A.release(m_3a)

    BmT = A.alloc([128, 4, NTOK], BF16)
    BBmT = [Buf("BmT%d" % j) for j in range(4)]
    m_3b = A.mark()
    convw = A.alloc([128, 4, 3], F32)
    P.dma("sp", lambda e: e.dma_start(out=convw[:], in_=conv_d), writes=[Bconst])
    wcv = [[A.alloc([128, 8, 128], BF16) for _ in range(3)] for _ in range(2)]
    ubuf = [A.alloc([128, 514], F32) for _ in range(2)]
    Bubuf = [Buf("u0"), Buf("u1")]
    cct = [A.alloc([128, 514], F32) for _ in range(2)]
    Bcct = [Buf("cct0"), Buf("cct1")]
    tcv = [A.alloc([128, 512], F32) for _ in range(2)]
    Btcv = [Buf("tcv0"), Buf("tcv1")]

    def p3b(ci, j, n, Bw):
        b = n % 2
        pcb, pcc, pcu = ps[3 * b], ps[3 * b + 1], ps[3 * b + 2]
        Bpcb, Bpcc, Bpcu = Bps[3 * b], Bps[3 * b + 1], Bps[3 * b + 2]
        ph, Bph = ps[6 + b], Bps[6 + b]
        w3 = wcv[ci % 2]
        for wi, (pp, Bpp) in enumerate(((pcb, Bpcb), (pcc, Bpcc), (pcu, Bpcu))):
            for kc in range(8):
                P.op("pe", lambda e, kc=kc, wi=wi, pp=pp: e.matmul(pp[:], lhsT=w3[wi][:, kc, :], rhs=h1T[:, kc, j * 512:(j + 1) * 512],
                                                                    start=(kc == 0), stop=(kc == 7)),
                     reads=[Bw[wi], Bh1T[j]], writes=[Bpp], inc=(kc == 7))
        for wi in (1, 2):
            for kc in range(8):
                P.op("pe", lambda e, kc=kc, wi=wi: e.matmul(ph[:, 2 * wi:2 * wi + 2], lhsT=w3[wi][:, kc, :], rhs=hhT[:, kc, 2 * j:2 * j + 2],
                                                             start=(kc == 0), stop=(kc == 7)),
                     reads=[Bw[wi], BhhT], writes=[Bph], inc=(kc == 7))
        ct, Bct = cct[b], Bcct[b]
        ub, Bub = ubuf[b], Bubuf[b]
        tv, Btv = tcv[b], Btcv[b]
        P.op("act", lambda e: e.activation(out=ct[:, 2:514], in_=pcc[:], func=AF.Copy), reads=[Bpcc], writes=[Bct])
        P.op("act", lambda e: e.activation(out=ct[:, 0:2], in_=ph[:, 2:4], func=AF.Copy), reads=[Bph], writes=[Bct])
        P.op("dve", lambda e: e.tensor_tensor(out=ub[:, 2:514], in0=ct[:, 2:514], in1=pcu[:], op=ALU.mult), reads=[Bct, Bpcu], writes=[Bub])
        P.op("dve", lambda e: e.tensor_tensor(out=ub[:, 0:2], in0=ct[:, 0:2], in1=ph[:, 4:6], op=ALU.mult), reads=[Bct, Bph], writes=[Bub])
        P.op("dve", lambda e: e.tensor_scalar(out=tv[:], in0=ub[:, 0:512], scalar1=convw[:, ci, 0:1], scalar2=None, op0=ALU.mult),
             reads=[Bub, Bconst], writes=[Btv])
        P.op("dve", lambda e: e.scalar_tensor_tensor(out=tv[:], in0=ub[:, 1:513], scalar=convw[:, ci, 1:2], in1=tv[:], op0=ALU.mult, op1=ALU.add),
             reads=[Bub, Btv, Bconst], writes=[Btv])
        P.op("dve", lambda e: e.scalar_tensor_tensor(out=tv[:], in0=ub[:, 2:514], scalar=convw[:, ci, 2:3], in1=tv[:], op0=ALU.mult, op1=ALU.add),
             reads=[Bub, Btv, Bconst], writes=[Btv])
        P.op("dve", lambda e: e.tensor_tensor(out=BmT[:, ci, j * 512:(j + 1) * 512], in0=tv[:], in1=pcb[:], op=ALU.mult),
             reads=[Btv, Bpcb], writes=[BBmT[j]])

    def load_3b(ci):
        Bw_ = []
        for wi, base in enumerate((1544, 2056, 2568)):
            c0 = base + ci * 128
            Bw_.append(load_w(wcv[ci % 2][wi][:], w_in_v[:, :, c0:c0 + 128], 8, 128, g1T, key="cv%d_%d" % (ci % 2, wi)))
        return Bw_
    n3b = 0
    Bw_next = load_3b(0)
    for ci in range(4):
        Bw = Bw_next
        if ci + 1 < 4:
            Bw_next = load_3b(ci + 1)
        for j in range(4):
            p3b(ci, j, n3b, Bw)
            n3b += 1
    P.barrier()
    if stage == "3b":
        return finish_debug([("BmT", BmT[:], [128, 4, NTOK], BF16)])
    A.release(m_3b)

    mT = nc.alloc_sbuf_tensor_at("mT", [128, 8, NTOK], BF16, offset=SB_END - 32768)
    BmTb = [Buf("mT%d" % j) for j in range(4)]
    m_3c = A.mark()
    wga = [A.alloc([128, 8, 128], BF16) for _ in range(2)]
    wgb = [A.alloc([128, 8, 128], BF16) for _ in range(2)]
    wA = [A.alloc([64, 8, 128], BF16) for _ in range(2)]
    wB = [A.alloc([128, 4, 128], BF16) for _ in range(2)]
    sga = [A.alloc([128, 512], F32) for _ in range(2)]
    sgb = [A.alloc([128, 512], F32) for _ in range(2)]
    Bsga = [Buf("sga0"), Buf("sga1")]
    Bsgb = [Buf("sgb0"), Buf("sgb1")]
    w_oa_v = w_oa.rearrange("(h p) n -> p h n", p=64)
    w_oc_v = w_oc.rearrange("(k p) n -> p k n", p=128)

    def p3c(m, j, n, Bw):
        b = n % 2
        pga, pgb, pA, pB_ = ps[4 * b], ps[4 * b + 1], ps[4 * b + 2], ps[4 * b + 3]
        Bpga, Bpgb, BpA, BpB_ = Bps[4 * b], Bps[4 * b + 1], Bps[4 * b + 2], Bps[4 * b + 3]
        wb_ = m % 2
        tok = slice(j * 512, (j + 1) * 512)
        for kc in range(8):
            P.op("pe", lambda e, kc=kc: e.matmul(pga[:], lhsT=wga[wb_][:, kc, :], rhs=h1T[:, kc, tok], start=(kc == 0), stop=(kc == 7)),
                 reads=[Bw[0], Bh1T[j]], writes=[Bpga], inc=(kc == 7))
        for kc in range(8):
            P.op("pe", lambda e, kc=kc: e.matmul(pgb[:], lhsT=wgb[wb_][:, kc, :], rhs=h1T[:, kc, tok], start=(kc == 0), stop=(kc == 7)),
                 reads=[Bw[1], Bh1T[j]], writes=[Bpgb], inc=(kc == 7))
        for h in range(8):
            P.op("pe", lambda e, h=h: e.matmul(pA[:], lhsT=wA[wb_][:, h, :], rhs=yT[:, h, tok], start=(h == 0), stop=(h == 7)),
                 reads=[Bw[2], ByT[j]], writes=[BpA], inc=(h == 7))
        for kc in range(4):
            P.op("pe", lambda e, kc=kc: e.matmul(pB_[:], lhsT=wB[wb_][:, kc, :], rhs=BmT[:, kc, tok], start=(kc == 0), stop=(kc == 3)),
                 reads=[Bw[3], BBmT[j]], writes=[BpB_], inc=(kc == 3))
        P.op("act", lambda e: e.activation(out=sga[b][:], in_=pga[:], func=AF.Sigmoid), reads=[Bpga], writes=[Bsga[b]])
        P.op("act", lambda e: e.activation(out=sgb[b][:], in_=pgb[:], func=AF.Sigmoid), reads=[Bpgb], writes=[Bsgb[b]])
        P.op("dve", lambda e: e.tensor_tensor(out=sga[b][:], in0=sga[b][:], in1=pA[:], op=ALU.mult), reads=[Bsga[b], BpA], writes=[Bsga[b]])
        P.op("dve", lambda e: e.tensor_tensor(out=sgb[b][:], in0=sgb[b][:], in1=pB_[:], op=ALU.mult), reads=[Bsgb[b], BpB_], writes=[Bsgb[b]])
        P.op("dve", lambda e: e.tensor_tensor(out=mT[:, m, tok], in0=sga[b][:], in1=sgb[b][:], op=ALU.add),
             reads=[Bsga[b], Bsgb[b]], writes=[BmTb[j]])

    def load_3c(m):
        c = m * 128
        return [load_w(wga[m % 2][:], w_in_v[:, :, 3080 + c:3080 + c + 128], 8, 128, g1T, key="ga%d" % (m % 2)),
                load_w(wgb[m % 2][:], w_in_v[:, :, 4104 + c:4104 + c + 128], 8, 128, g1T, key="gb%d" % (m % 2)),
                load_w(wA[m % 2][:], w_oa_v[:, :, c:c + 128], 8, 128, None, parts=64, key="wA%d" % (m % 2)),
                load_w(wB[m % 2][:], w_oc_v[:, :, c:c + 128], 4, 128, None, key="wB%d" % (m % 2))]
    n3c = 0
    Bw_next = load_3c(0)
    for m in range(8):
        Bw = Bw_next
        if m + 1 < 8:
            Bw_next = load_3c(m + 1)
        for j in range(4):
            p3c(m, j, n3c, Bw)
            n3c += 1
    P.barrier()
    A.release(m_p3)
    if stage == "3c":
        return finish_debug([("mT", mT[:], [128, 8, NTOK], BF16)])


    xT = nc.alloc_sbuf_tensor_at("xTres", [128, 8, NTOK], F32, offset=C0)
    BxT = [[Buf("xT%d_%d" % (m, j)) for j in range(4)] for m in range(8)]
    m_3d = A.mark()
    wo = [A.alloc([128, 8, 128], BF16) for _ in range(2)]
    w_o_v = w_o.rearrange("(k p) n -> p k n", p=128)

    def p3d(m, j, n, Bw):
        pp, Bpp = ps[n % 4], Bps[n % 4]
        tok = slice(j * 512, (j + 1) * 512)
        for kc in range(8):
            P.op("pe", lambda e, kc=kc: e.matmul(pp[:], lhsT=wo[m % 2][:, kc, :], rhs=mT[:, kc, tok], start=(kc == 0), stop=(kc == 7)),
                 reads=[Bw, BmTb[j]], writes=[Bpp], inc=(kc == 7))
        P.op("dve", lambda e: e.tensor_tensor(out=xT[:, m, tok], in0=pp[:], in1=xT[:, m, tok], op=ALU.add),
             reads=[Bpp, BxT[m][j]], writes=[BxT[m][j]])

    def p3d_load(m):
        Bw_ = load_w(wo[m % 2][:], w_o_v[:, :, m * 128:(m + 1) * 128], 8, 128, None, key="wo%d" % (m % 2))
        P.dma("sp", lambda e: e.dma_start(out=xT[:, m, :], in_=xT_own[m * 128:(m + 1) * 128, :]), writes=BxT[m])
        return Bw_
    Bw_nx = p3d_load(0)
    for m in range(8):
        Bw_cur = Bw_nx
        if m + 1 < 8:
            Bw_nx = p3d_load(m + 1)
        for j in range(4):
            p3d(m, j, m * 4 + j, Bw_cur)
    P.barrier()
    A.release(m_3d)

    h2T = A.alloc([128, 8, NTOK], BF16)
    Bh2T = [Buf("h2T%d" % j) for j in range(4)]
    comb = A.alloc([128, 16, 16], F32)
    Bcomb = [Buf("comb%d" % t) for t in range(16)]
    m_3e = A.mark()
    sqc2_l = [A.alloc([128, 8, 512], BF16) for _ in range(2)]
    Bsqc2_l = [Buf("sqc2a"), Buf("sqc2b")]
    Rt2_l = [A.alloc([128, 512], F32) for _ in range(2)]
    BR2_l = [Buf("R2a"), Buf("R2b")]
    Wr = A.alloc([128, 8, 20], BF16)
    br_t = A.alloc([128, 20], F32)
    BWr = load_w(Wr[:], w_r.rearrange("(k p) n -> p k n", p=128), 8, 20, g2T)
    P.dma("sp", lambda e: e.dma_start(out=br_t[:], in_=br_d), writes=[Bconst])

    def p3e_a(j):
        b = j % 2
        tok = slice(j * 512, (j + 1) * 512)
        pR, BpR = ps[b], Bps[b]
        P.op("act", lambda e: e.activation(out=sqc2_l[b][:], in_=xT[:, :, tok], func=AF.Square),
             reads=[BxT[m][j] for m in range(8)], writes=[Bsqc2_l[b]])
        for kc in range(8):
            P.op("pe", lambda e, kc=kc: e.matmul(pR[:], lhsT=ones_bf[:], rhs=sqc2_l[b][:, kc, :], start=(kc == 0), stop=(kc == 7)),
                 reads=[Bsqc2_l[b], Bconst], writes=[BpR], inc=(kc == 7))

    def p3e_b(j):
        b = j % 2
        tok = slice(j * 512, (j + 1) * 512)
        pR, BpR = ps[b], Bps[b]
        P.op("act", lambda e: e.activation(out=Rt2_l[b][:], in_=pR[:], func=AF.Ln, bias=EPS, scale=1.0 / D), reads=[BpR], writes=[BR2_l[b]])
        P.op("act", lambda e: e.activation(out=Rt2_l[b][:], in_=Rt2_l[b][:], func=AF.Exp, scale=-0.5), reads=[BR2_l[b]], writes=[BR2_l[b]])
        P.op("dve", lambda e: e.tensor_tensor(out=h2T[:, :, tok], in0=xT[:, :, tok],
                                              in1=Rt2_l[b][:].unsqueeze(1).to_broadcast([128, 8, 512]), op=ALU.mult),
             reads=[BxT[m][j] for m in range(8)] + [BR2_l[b]], writes=[Bh2T[j]])
    p3e_a(0)
    for j in range(4):
        if j + 1 < 4:
            p3e_a(j + 1)
        p3e_b(j)

    pl, Bpl = ps[4], Bps[4]

    def route_mm(t):
        for kc in range(8):
            P.op("pe", lambda e, kc=kc: e.matmul(pl[:, t * 20:(t + 1) * 20], lhsT=h2T[:, kc, t * 128:(t + 1) * 128], rhs=Wr[:, kc, :],
                                                 start=(kc == 0), stop=(kc == 7)),
                 reads=[Bh2T[t // 4], BWr], writes=[Bpl], inc=(kc == 7))
    for t in range(16):
        route_mm(t)

    def ra(n):
        return A.alloc([128, 16, n], F32)
    Lb, dg, ge, oh, tmpr, ein, d1, mk1, e2, d2, sel, wv = ra(20), ra(4), ra(4), ra(4), ra(16), ra(4), ra(4), ra(4), ra(4), ra(4), ra(4), ra(4)
    gmax, gsum, gval, m1, m2, wsum, sc = (A.alloc([128, 16], F32) for _ in range(7))
    BRr = Buf("route")

    def dv(fn, rd=()):
        P.op("dve", fn, reads=[BRr] + list(rd), writes=[BRr])

    def bc4(ap2):
        return ap2.unsqueeze(2).to_broadcast([128, 16, 4])
    dv(lambda e: e.tensor_tensor(out=Lb[:], in0=pl[:, 0:320].rearrange("p (t c) -> p t c", c=20),
                                 in1=br_t[:].unsqueeze(1).to_broadcast([128, 16, 20]), op=ALU.add), rd=[Bpl, Bconst])
    dv(lambda e: e.tensor_reduce(out=gmax[:], in_=Lb[:, :, 0:4], axis=AX.X, op=ALU.max))
    dv(lambda e: e.tensor_tensor(out=dg[:], in0=Lb[:, :, 0:4], in1=bc4(gmax[:]), op=ALU.subtract))
    P.op("act", lambda e: e.activation(out=ge[:], in_=dg[:], func=AF.Exp), reads=[BRr], writes=[BRr])
    dv(lambda e: e.tensor_reduce(out=gsum[:], in_=ge[:], axis=AX.X, op=ALU.add))
    dv(lambda e: e.reciprocal(out=gval[:], in_=gsum[:]))
    dv(lambda e: e.tensor_scalar(out=oh[:], in0=dg[:], scalar1=0.0, scalar2=None, op0=ALU.is_equal))
    dv(lambda e: e.tensor_tensor(out=tmpr[:].rearrange("p t (g j) -> p t g j", g=4), in0=Lb[:, :, 4:20].rearrange("p t (g j) -> p t g j", g=4),
                                 in1=oh[:].unsqueeze(3).to_broadcast([128, 16, 4, 4]), op=ALU.mult))
    dv(lambda e: e.tensor_reduce(out=ein[:], in_=tmpr[:].rearrange("p t (g j) -> p t j g", g=4), axis=AX.X, op=ALU.add))
    dv(lambda e: e.tensor_reduce(out=m1[:], in_=ein[:], axis=AX.X, op=ALU.max))
    dv(lambda e: e.tensor_tensor(out=d1[:], in0=ein[:], in1=bc4(m1[:]), op=ALU.subtract))
    dv(lambda e: e.tensor_scalar(out=mk1[:], in0=d1[:], scalar1=0.0, scalar2=None, op0=ALU.is_equal))
    dv(lambda e: e.scalar_tensor_tensor(out=e2[:], in0=mk1[:], scalar=-1e30, in1=d1[:], op0=ALU.mult, op1=ALU.add))
    dv(lambda e: e.tensor_reduce(out=m2[:], in_=e2[:], axis=AX.X, op=ALU.max))
    dv(lambda e: e.tensor_tensor(out=d2[:], in0=e2[:], in1=bc4(m2[:]), op=ALU.subtract))
    dv(lambda e: e.tensor_scalar(out=sel[:], in0=d2[:], scalar1=0.0, scalar2=None, op0=ALU.is_equal))
    dv(lambda e: e.tensor_tensor(out=sel[:], in0=sel[:], in1=mk1[:], op=ALU.add))
    P.op("act", lambda e: e.activation(out=wv[:], in_=d1[:], func=AF.Exp), reads=[BRr], writes=[BRr])
    dv(lambda e: e.tensor_tensor(out=wv[:], in0=wv[:], in1=sel[:], op=ALU.mult))
    dv(lambda e: e.tensor_reduce(out=wsum[:], in_=wv[:], axis=AX.X, op=ALU.add))
    dv(lambda e: e.reciprocal(out=sc[:], in_=wsum[:]))
    dv(lambda e: e.tensor_tensor(out=sc[:], in0=sc[:], in1=gval[:], op=ALU.mult))
    dv(lambda e: e.tensor_tensor(out=wv[:], in0=wv[:], in1=bc4(sc[:]), op=ALU.mult))
    P.op("dve", lambda e: e.tensor_tensor(out=comb[:].rearrange("p t (g j) -> p t g j", g=4),
                                          in0=oh[:].unsqueeze(3).to_broadcast([128, 16, 4, 4]),
                                          in1=wv[:].unsqueeze(2).to_broadcast([128, 16, 4, 4]), op=ALU.mult),
         reads=[BRr], writes=Bcomb)
    P.barrier()
    if stage == "3e":
        return finish_debug([("xT", xT[:], [128, 8, NTOK], F32), ("comb", comb[:], [128, 16, 16], F32)])
    A.release(m_3e)

    m_p4 = A.mark()
    Wgu = [A.alloc([128, 8, 512], BF16) for _ in range(2)]
    Wd = [A.alloc([128, 2, 1024], BF16) for _ in range(2)]
    BWg = [Buf("Wg0"), Buf("Wg1")]
    BWu = [Buf("Wu0"), Buf("Wu1")]
    BWd = [Buf("Wd0"), Buf("Wd1")]
    mstg = [A.alloc([128, 2048], F32) for _ in range(6)]
    Bmstg = [Buf("mstg%d" % i) for i in range(6)]
    sa = [A.alloc([128, 256], F32) for _ in range(2)]
    Bsa = [Buf("sa0"), Buf("sa1")]
    hid = [A.alloc([128, 256], BF16) for _ in range(2)]
    Bhid = [Buf("hid0"), Buf("hid1")]
    hidT = [A.alloc([128, 2, 512], BF16) for _ in range(2)]
    BhidT = [Buf("hidT0"), Buf("hidT1")]

    def moe_wdma(e):
        base = (e % 2) * 3
        srcs = (w_gate[e].rearrange("(k p) n -> p k n", p=128), w_up[e].rearrange("(k p) n -> p k n", p=128),
                w_down[e].rearrange("(k p) n -> p k n", p=128))
        shp = ((8, 256), (8, 256), (2, 1024))
        for q_ in range(3):
            K_, N_ = shp[q_]
            st_ = mstg[base + q_][:, 0:K_ * N_].rearrange("p (k n) -> p k n", k=K_)
            P.dma("sp", lambda e_, st_=st_, src=srcs[q_]: e_.dma_start(out=st_, in_=src), writes=[Bmstg[base + q_]])

    def moe_wcast_ops(e):
        base = (e % 2) * 3
        we = e % 2
        sg_ = mstg[base][:, 0:2048].rearrange("p (k n) -> p k n", k=8)
        su_ = mstg[base + 1][:, 0:2048].rearrange("p (k n) -> p k n", k=8)
        sd_ = mstg[base + 2][:, 0:2048].rearrange("p (k n) -> p k n", k=2)
        ops_ = []
        for kc in range(8):
            ops_.append(lambda kc=kc: P.op("act", lambda e_: e_.activation(out=Wgu[we][:, kc, 0:256], in_=sg_[:, kc, :], func=AF.Copy,
                                                                      scale=g2T[:, kc:kc + 1]),
                                           reads=[Bmstg[base], Bconst], writes=[BWg[we]]))
        for kc in range(8):
            ops_.append(lambda kc=kc: P.op("act", lambda e_: e_.activation(out=Wgu[we][:, kc, 256:512], in_=su_[:, kc, :], func=AF.Copy,
                                                                      scale=g2T[:, kc:kc + 1]),
                                           reads=[Bmstg[base + 1], Bconst], writes=[BWu[we]]))
        for fc in range(2):
            ops_.append(lambda fc=fc: P.op("act", lambda e_: e_.activation(out=Wd[we][:, fc, :], in_=sd_[:, fc, :], func=AF.Copy),
                                           reads=[Bmstg[base + 2]], writes=[BWd[we]]))
        return ops_

    def moe_wcast(e):
        for f_ in moe_wcast_ops(e):
            f_()

    def moe_A(u):
        e, t = u // 16, u % 16
        j = t // 4
        we = e % 2
        pau, Bpau = ps[u % 3], Bps[u % 3]
        for kc in range(8):
            P.op("pe", lambda e_, kc=kc: e_.matmul(pau[:], lhsT=h2T[:, kc, t * 128:(t + 1) * 128], rhs=Wgu[we][:, kc, :],
                                                   start=(kc == 0), stop=(kc == 7)),
                 reads=[Bh2T[j], BWg[we], BWu[we]], writes=[Bpau], inc=(kc == 7))

    def moe_B(u):
        e, t = u // 16, u % 16
        j, tt_ = t // 4, t % 4
        b = u % 2
        pau, Bpau = ps[u % 3], Bps[u % 3]
        ptr, Bptr = ps[3 + b], Bps[3 + b]
        P.op("act", lambda e_: e_.activation(out=sa[b][:], in_=pau[:, 0:256], func=AF.Silu), reads=[Bpau], writes=[Bsa[b]])
        P.op("dve", lambda e_: e_.scalar_tensor_tensor(out=hid[b][:], in0=sa[b][:], scalar=comb[:, t, e:e + 1], in1=pau[:, 256:512],
                                                       op0=ALU.mult, op1=ALU.mult), reads=[Bsa[b], Bpau, Bcomb[t]], writes=[Bhid[b]])
        ptb = ptr[:].bitcast(BF16).rearrange("p (f t) -> p f t", t=128)
        for fc in range(2):
            P.op("pe", lambda e_, fc=fc: e_.transpose(out=ptb[:, fc, :], in_=hid[b][:, fc * 128:(fc + 1) * 128], identity=ident[:]),
                 reads=[Bhid[b], Bconst], writes=[Bptr], inc=(fc == 1))
        hb = (e * 4 + j) % 2
        P.op("act", lambda e_: e_.activation(out=hidT[hb][:, :, tt_ * 128:(tt_ + 1) * 128], in_=ptb[:, 0:2, :], func=AF.Copy),
             reads=[Bptr], writes=[BhidT[hb]])

    dn_n = [0]

    def moe_C1(e, j, m):
        we = e % 2
        hb = (e * 4 + j) % 2
        tok = slice(j * 512, (j + 1) * 512)
        n = dn_n[0]
        dn_n[0] += 1
        pd, Bpd = ps[5 + n % 3], Bps[5 + n % 3]
        for fc in range(2):
            P.op("pe", lambda e_, fc=fc: e_.matmul(pd[:], lhsT=Wd[we][:, fc, m * 128:(m + 1) * 128], rhs=hidT[hb][:, fc, :],
                                                   start=(fc == 0), stop=(fc == 1)),
                 reads=[BWd[we], BhidT[hb]], writes=[Bpd], inc=(fc == 1))
        P.op("dve", lambda e_: e_.tensor_tensor(out=xT[:, m, tok], in0=pd[:], in1=xT[:, m, tok], op=ALU.add),
             reads=[Bpd, BxT[m][j]], writes=[BxT[m][j]])

    NU = NE * 16
    moe_wdma(0)
    moe_wcast(0)
    moe_wdma(1)
    moe_A(0)
    moe_A(1)
    pendC = []
    pendW = []
    for u in range(NU):
        e, t = u // 16, u % 16
        if u + 2 < NU:
            moe_A(u + 2)
        moe_B(u)
        k_ = 0
        while pendC and pendC[0][0] <= u and k_ < 2:
            _, e2_, j2_, m2_ = pendC.pop(0)
            moe_C1(e2_, j2_, m2_)
            k_ += 1
        if t % 4 == 3:
            for m_ in range(8):
                pendC.append((u + 1, e, t // 4, m_))
        if t == 5:
            assert not [c for c in pendC if c[1] < e]
            if e + 1 < NE:
                pendW = moe_wcast_ops(e + 1)
            if e + 2 < NE:
                moe_wdma(e + 2)
        for _ in range(2):
            if pendW:
                pendW.pop(0)()
    for _, e2_, j2_, m2_ in pendC:
        moe_C1(e2_, j2_, m2_)
    P.barrier()
    if stage == "4":
        return finish_debug([("xT", xT[:], [128, 8, NTOK], F32)])
    A.release(m_p4)
    A.release(C0 + 65536)

    stg5 = [A.alloc([128, 2048], F32) for _ in range(NSTG)]
    for i_ in range(NSTG):
        stg[i_] = stg5[i_]
    Wpg = A.alloc([128, 8, 1024], BF16)
    Wple = A.alloc([128, 2, 1024], BF16)
    h3T = [A.alloc([128, 8, 512], BF16) for _ in range(2)]
    Bh3T = [Buf("h3T0"), Buf("h3T1")]
    sqc3 = A.alloc([128, 8, 512], BF16)
    Bsqc3 = Buf("sqc3")
    Rt3 = A.alloc([128, 512], F32)
    BR3 = Buf("R3")
    pst = [A.alloc([128, 2, 512], F32) for _ in range(2)]
    Bpst = [Buf("pst0"), Buf("pst1")]
    ptb5 = [A.alloc([128, 2, 512], BF16) for _ in range(2)]
    Bptb5 = [Buf("ptb0"), Buf("ptb1")]
    sg = [A.alloc([128, 512], F32) for _ in range(2)]
    Bsg = [Buf("sg0"), Buf("sg1")]
    ost = [A.alloc([128, 512], F32) for _ in range(3)]
    Bost = [Buf("ost%d" % i) for i in range(3)]
    w_pg_v = w_pg.rearrange("(k p) n -> p k n", p=128)
    pT_v = pT_own.rearrange("(k p) t -> p k t", p=128)
    Bout = Buf("out")

    def p5_m(j, m, n):
        b = j % 2
        tok = slice(j * 512, (j + 1) * 512)
        ppg, Bppg = ps[2 + 2 * (n % 3)], Bps[2 + 2 * (n % 3)]
        ppe, Bppe = ps[3 + 2 * (n % 3)], Bps[3 + 2 * (n % 3)]
        for kc in range(8):
            P.op("pe", lambda e, kc=kc: e.matmul(ppg[:], lhsT=Wpg[:, kc, m * 128:(m + 1) * 128], rhs=h3T[b][:, kc, :], start=(kc == 0), stop=(kc == 7)),
                 reads=[BWpg[m // 2], Bh3T[b]], writes=[Bppg], inc=(kc == 7))
        for kc in range(2):
            P.op("pe", lambda e, kc=kc: e.matmul(ppe[:], lhsT=Wple[:, kc, m * 128:(m + 1) * 128], rhs=ptb5[b][:, kc, :], start=(kc == 0), stop=(kc == 1)),
                 reads=[BWple, Bptb5[b]], writes=[Bppe], inc=(kc == 1))
        sb_, o_ = n % 2, n % 3
        P.op("act", lambda e: e.activation(out=sg[sb_][:], in_=ppg[:], func=AF.Sigmoid), reads=[Bppg], writes=[Bsg[sb_]])
        P.op("dve", lambda e: e.tensor_tensor(out=sg[sb_][:], in0=sg[sb_][:], in1=ppe[:], op=ALU.mult), reads=[Bsg[sb_], Bppe], writes=[Bsg[sb_]])
        P.op("dve", lambda e: e.tensor_tensor(out=ost[o_][:], in0=sg[sb_][:], in1=xT[:, m, tok], op=ALU.add),
             reads=[Bsg[sb_], BxT[m][j]], writes=[Bost[o_]])
        P.dma("sp", lambda e: e.dma_start(out=outT[m * 128:(m + 1) * 128, tok], in_=ost[o_][:]), reads=[Bost[o_]], writes=[Buf("o")])

    def p5_pre(j):
        b = j % 2
        tok = slice(j * 512, (j + 1) * 512)
        P.dma("sp", lambda e: e.dma_start(out=pst[b][:], in_=pT_v[:, :, tok]), writes=[Bpst[b]])
        P.op("pool", lambda e: e.tensor_copy(out=ptb5[b][:], in_=pst[b][:]), reads=[Bpst[b]], writes=[Bptb5[b]])
        rnorm_chunk(xT[:, :, tok], [BxT[m][j] for m in range(8)], h3T[b][:], Bh3T[b], sqc3[:], Bsqc3, Rt3, BR3, ps[j % 2], Bps[j % 2], 512)

    p5_pre(0)
    BWpg = [load_w(Wpg[:, :, c * 256:(c + 1) * 256], w_pg_v[:, :, c * 256:(c + 1) * 256], 8, 256, g3T) for c in range(4)]
    BWple = load_w(Wple[:], w_ple.rearrange("(k p) n -> p k n", p=128), 2, 1024, None)
    for j in range(4):
        if j + 1 < 4:
            p5_pre(j + 1)
        for m in range(8):
            p5_m(j, m, j * 8 + m)
    P.barrier()
    P.emit()
    return nc


def make_masks():
    k = np.arange(128)[:, None]
    q = np.arange(512)[None, :]

    def diag(jb):
        return np.where((jb * 128 + k) <= q, 0.0, -30000.0).astype(np.float32)
    ones = np.zeros((128, 512), np.float32)
    zeros = np.full((128, 512), -30000.0, np.float32)
    E = [diag(0), diag(1), diag(2), diag(3), zeros, zeros, zeros, zeros]
    O = [ones, ones, ones, ones, diag(0), diag(1), diag(2), diag(3)]
    return np.stack(E, 0), np.stack(O, 0)


def prep_inputs(inp):
    x = np.asarray(inp["x"], np.float32)
    p = np.asarray(inp["p"], np.float32)[0]
    E, O = make_masks()
    shared = {
        "w_in": np.ascontiguousarray(inp["w_in"][0]),
        "w_oa": np.ascontiguousarray(inp["w_out_att"][0]),
        "w_oc": np.ascontiguousarray(inp["w_out_conv"][0]),
        "w_o": np.ascontiguousarray(inp["w_o"][0]),
        "w_r": np.ascontiguousarray(np.concatenate([inp["w_rg"][0], inp["w_re"][0]], axis=1)),
        "w_gate": np.ascontiguousarray(inp["w_gate"][0]),
        "w_up": np.ascontiguousarray(inp["w_up"][0]),
        "w_down": np.ascontiguousarray(inp["w_down"][0]),
        "w_pg": np.ascontiguousarray(inp["w_pg"][0]),
        "w_ple": np.ascontiguousarray(inp["w_ple"][0]),
        "g1T": np.ascontiguousarray(inp["attn_norm_g"][0].reshape(8, 128).T),
        "g2T": np.ascontiguousarray(inp["ffn_norm_g"][0].reshape(8, 128).T),
        "g3T": np.ascontiguousarray(inp["ple_norm_g"][0].reshape(8, 128).T),
        "bf_bc": np.ascontiguousarray(np.broadcast_to(inp["b_f"][0][None, :], (128, 8))),
        "gq_col": np.ascontiguousarray(inp["q_norm_g"][0].reshape(64, 1)),
        "gk_col": np.ascontiguousarray(inp["k_norm_g"][0].reshape(64, 1)),
        "convT": np.ascontiguousarray(inp["conv_w"][0].reshape(3, 4, 128).transpose(2, 1, 0)),
        "br_bc": np.ascontiguousarray(np.broadcast_to(
            np.concatenate([inp["b_rg"][0], inp["b_re"][0]])[None, :], (128, 20))),
    }
    shared = {k: np.asarray(v, np.float32) for k, v in shared.items()}
    maps = []
    for c in range(8):
        b, par = c // 2, c % 2
        chunks = CHUNKS[par]
        xb_T = np.ascontiguousarray(x[b].T)
        own_cols = np.concatenate([np.arange(ci * 512, (ci + 1) * 512) for ci in chunks])
        xh = np.zeros((D, 8), np.float32)
        for j, ci in enumerate(chunks):
            if ci > 0:
                xh[:, 2 * j:2 * j + 2] = xb_T[:, ci * 512 - 2:ci * 512]
        sel = np.zeros((16, 32), np.float32)
        for j, ci in enumerate(chunks):
            for tt in range(4):
                sel[4 * j + tt, 4 * ci + tt] = 1.0
        types = [(E, O)[ci % 2] for ci in chunks]
        mask2 = np.stack([types[0], types[1]], 0)
        assert np.array_equal(types[0], types[2]) and np.array_equal(types[1], types[3])
        m = dict(shared)
        m.update({
            "xT_all": xb_T,
            "xT_own": np.ascontiguousarray(xb_T[:, own_cols]),
            "xhT": xh,
            "pT_own": np.ascontiguousarray(p[b].T[:, own_cols]),
            "sel_own": np.ascontiguousarray(np.broadcast_to(sel[None], (128, 16, 32))),
            "mask2": np.ascontiguousarray(mask2.transpose(2, 0, 1, 3)).astype(ml_dtypes.bfloat16),
        })
        maps.append(m)
    return maps


_NC_CACHE = {}


def kernel(**inputs):
    maps = prep_inputs(inputs)
    if "nc" not in _NC_CACHE:
        _NC_CACHE["nc"] = build()
    nc = _NC_CACHE["nc"]
    res = run_bass_kernel_spmd(nc, maps, core_ids=list(range(8)))
    out = np.empty((4, S, D), np.float32)
    for c in range(8):
        b, par = c // 2, c % 2
        oT = np.asarray(res.results[c]["outT"])
        for j, ci in enumerate(CHUNKS[par]):
            out[b, ci * 512:(ci + 1) * 512, :] = oT[:, j * 512:(j + 1) * 512].T
    return out
```

```python
import numpy as np
import ml_dtypes
import concourse.bass as bass
import concourse.mybir as mybir
from concourse.bass_utils import run_bass_kernel_spmd

F32 = mybir.dt.float32
BF16 = mybir.dt.bfloat16
AF = mybir.ActivationFunctionType
ALU = mybir.AluOpType
AX = mybir.AxisListType

ENGS = ("pe", "act", "dve", "pool", "sp")
NDMASEM = 20
SB_BASE = 16512
SB_END = 229376 - 2048
EPS = 1e-6

D = 1024
S = 4096
NH = 8
HD = 64
NTOK = 2048
NE = 16
DE = 256
CHUNKS = ((0, 3, 4, 7), (1, 2, 5, 6))


class Tk:
    __slots__ = ("sem", "val")

    def __init__(self, sem, val):
        self.sem = sem
        self.val = val


class Buf:
    __slots__ = ("name", "w", "r")

    def __init__(self, name=""):
        self.name = name
        self.w = None
        self.r = []


class Prog:
    def __init__(self, nc):
        self.nc = nc
        self.ops = {e: [] for e in ENGS}
        self.cnt = {e: 0 for e in ENGS}
        self.seen = {e: {} for e in ENGS}
        self.pend = {e: [] for e in ENGS}
        self.dma_n = {e: 0 for e in ENGS}
        self.dma_last = {}

    def _need(self, eng, waits, t):
        if t is None:
            return
        if t.sem == "pe" and eng == "pe":
            return
        if t.val is None:
            raise RuntimeError("dependency on op without resolved ticket (missing inc)")
        if self.seen[eng].get(t.sem, 0) >= t.val:
            return
        if waits.get(t.sem, 0) < t.val:
            waits[t.sem] = t.val

    def _deps(self, eng, reads, writes, waits):
        for b in reads:
            self._need(eng, waits, b.w)
        for b in writes:
            self._need(eng, waits, b.w)
            for t in b.r:
                self._need(eng, waits, t)
        for s, v in waits.items():
            self.seen[eng][s] = v

    def _mark(self, tk, reads, writes):
        for b in reads:
            b.r.append(tk)
            if len(b.r) > 64:
                b.r = b.r[-48:]
        for b in writes:
            b.w = tk
            b.r = []

    def op(self, eng, fn, reads=(), writes=(), inc=True):
        waits = {}
        self._deps(eng, reads, writes, waits)
        if inc:
            self.cnt[eng] += 1
            tk = Tk(eng, self.cnt[eng])
            for p in self.pend[eng]:
                p.val = tk.val
            self.pend[eng] = []
        else:
            tk = Tk(eng, None)
            self.pend[eng].append(tk)
        self._mark(tk, reads, writes)
        self.ops[eng].append((fn, list(waits.items()), (eng, 1) if inc else None))
        return tk

    def dma(self, eng, fn, reads=(), writes=()):
        n = self.dma_n[eng]
        self.dma_n[eng] += 1
        semname = "d_%s_%d" % (eng, n % NDMASEM)
        waits = {}
        prev = self.dma_last.get(semname)
        if prev is not None:
            self._need(eng, waits, prev)
        self._deps(eng, reads, writes, waits)
        tk = Tk(semname, 16 * (n // NDMASEM + 1))
        self.dma_last[semname] = tk
        self._mark(tk, reads, writes)
        self.ops[eng].append((fn, list(waits.items()), (semname, 16)))
        return tk

    def barrier(self):
        for e in ENGS:
            assert not self.pend[e], "barrier with pending un-inc'ed ops on " + e
        tks = [Tk(e, self.cnt[e]) for e in ENGS if self.cnt[e] > 0]
        tks += list(self.dma_last.values())
        for e in ENGS:
            waits = {}
            for t in tks:
                if t.sem != e:
                    self._need(e, waits, t)
            for s, v in waits.items():
                self.seen[e][s] = v
            self.ops[e].append((None, list(waits.items()), None))

    def emit(self):
        nc = self.nc
        from contextlib import ExitStack
        semnames = set()
        for e in ENGS:
            for fn, waits, inc in self.ops[e]:
                for s, v in waits:
                    semnames.add(s)
                if inc:
                    semnames.add(inc[0])
        with ExitStack() as st:
            sems = {}
            for s in sorted(semnames):
                sems[s] = st.enter_context(nc.semaphore("s_" + s))
            block = st.enter_context(nc.Block())

            def run(e, engobj):
                for fn, waits, inc in self.ops[e]:
                    for s, v in waits:
                        engobj.wait_ge(sems[s], v)
                    if fn is None:
                        continue
                    ins = fn(engobj)
                    if inc:
                        ins.then_inc(sems[inc[0]], inc[1])

            @block.tensor
            def _(eng):
                run("pe", eng)

            @block.scalar
            def _(eng):
                run("act", eng)

            @block.vector
            def _(eng):
                run("dve", eng)

            @block.gpsimd
            def _(eng):
                run("pool", eng)

            @block.sync
            def _(eng):
                run("sp", eng)


class Arena:
    def __init__(self, nc):
        self.nc = nc
        self.off = SB_BASE
        self.n = 0

    def mark(self):
        return self.off

    def release(self, m):
        self.off = m

    def alloc(self, shape, dt):
        nbytes = int(np.prod(shape[1:])) * (4 if dt == F32 else 2)
        nbytes = (nbytes + 63) // 64 * 64
        assert self.off + nbytes <= SB_END, "SBUF overflow: %d + %d" % (self.off, nbytes)
        self.n += 1
        t = self.nc.alloc_sbuf_tensor_at("t%d" % self.n, list(shape), dt, offset=self.off)
        self.off += nbytes
        return t


def bc_mid(ap2, n):
    p, a = ap2.shape
    return ap2.unsqueeze(2).to_broadcast([p, a, n])


def build(stage=99, nkv=32, nq=16):
    nc = bass.Bass("TRN2", target_bir_lowering=False)
    P = Prog(nc)
    A = Arena(nc)

    def finish_debug(items):
        P.barrier()
        for name, ap, shape, dt in items:
            o = nc.dram_tensor(name, list(shape), dt, kind="ExternalOutput").ap()
            P.dma("sp", lambda e, o=o, ap=ap: e.dma_start(out=o, in_=ap))
        P.barrier()
        P.emit()
        return nc

    def din(name, shape, dt=F32):
        return nc.dram_tensor(name, list(shape), dt, kind="ExternalInput").ap()

    xT_all = din("xT_all", [D, S])
    xT_own = din("xT_own", [D, NTOK])
    xhT = din("xhT", [D, 8])
    pT_own = din("pT_own", [256, NTOK])
    w_in = din("w_in", [D, 5128])
    w_oa = din("w_oa", [512, D])
    w_oc = din("w_oc", [512, D])
    w_o = din("w_o", [D, D])
    w_r = din("w_r", [D, 20])
    w_gate = din("w_gate", [NE, D, DE])
    w_up = din("w_up", [NE, D, DE])
    w_down = din("w_down", [NE, DE, D])
    w_pg = din("w_pg", [D, D])
    w_ple = din("w_ple", [256, D])
    g1T_d = din("g1T", [128, 8])
    g2T_d = din("g2T", [128, 8])
    g3T_d = din("g3T", [128, 8])
    bf_d = din("bf_bc", [128, 8])
    gq_d = din("gq_col", [64, 1])
    gk_d = din("gk_col", [64, 1])
    conv_d = din("convT", [128, 4, 3])
    br_d = din("br_bc", [128, 20])
    sel_d = din("sel_own", [128, 16, 32])
    mask_d = din("mask2", [128, 2, 8, 512], BF16)
    if stage == 2:
        dbg = nc.dram_tensor("dbg", [64, 8, NTOK], BF16, kind="ExternalOutput").ap()
    elif stage == 99:
        outT = nc.dram_tensor("outT", [D, NTOK], F32, kind="ExternalOutput").ap()

    import os
    ps = [nc.alloc_psum_tensor("ps%d" % i, [128, 512], F32) for i in range(int(os.environ.get("NPS", "8")))]
    Bps = [Buf("ps%d" % i) for i in range(len(ps))]

    ident = A.alloc([128, 128], BF16)
    tmpf = A.alloc([128, 128], F32)
    tri = A.alloc([128, 128], F32)
    Emat = A.alloc([128, 128], F32)
    ones_bf = A.alloc([128, 128], BF16)
    ones_f = A.alloc([128, 64], F32)
    g1T = A.alloc([128, 8], F32)
    g2T = A.alloc([128, 8], F32)
    g3T = A.alloc([128, 8], F32)
    bf_bc = A.alloc([128, 8], F32)
    gqkT = A.alloc([65, 1], F32)
    gk_t = A.alloc([64, 1], F32)
    Bconst = Buf("const")
    Btmpf = Buf("tmpf")

    P.op("pool", lambda e: e.memset(tmpf[:], 1.0), writes=[Btmpf])
    P.op("pool", lambda e: e.affine_select(out=tmpf[:], in_=tmpf[:], pattern=[[-1, 128]], compare_op=ALU.is_equal,
                                           fill=0.0, base=0, channel_multiplier=1), reads=[Btmpf], writes=[Btmpf])
    P.op("dve", lambda e: e.tensor_copy(out=ident[:], in_=tmpf[:]), reads=[Btmpf], writes=[Bconst])
    P.op("pool", lambda e: e.memset(tri[:], 1.0), writes=[Bconst])
    P.op("pool", lambda e: e.affine_select(out=tri[:], in_=tri[:], pattern=[[1, 128]], compare_op=ALU.is_ge,
                                           fill=0.0, base=0, channel_multiplier=-1), reads=[Bconst], writes=[Bconst])
    P.op("pool", lambda e: e.memset(Emat[:], 1.0), writes=[Bconst])
    P.op("pool", lambda e: e.affine_select(out=Emat[:], in_=Emat[:], pattern=[[0, 128]], compare_op=ALU.is_equal,
                                           fill=0.0, base=-127, channel_multiplier=1), reads=[Bconst], writes=[Bconst])
    P.op("pool", lambda e: e.memset(ones_bf[:], 1.0), writes=[Bconst])
    P.op("pool", lambda e: e.memset(ones_f[:], 1.0), writes=[Bconst])
    P.op("pool", lambda e: e.memset(gqkT[:], 1.0), writes=[Bconst])
    P.dma("sp", lambda e: e.dma_start(out=gqkT[0:64, :], in_=gq_d), writes=[Bconst])
    for dst, src in ((g1T, g1T_d), (g2T, g2T_d), (g3T, g3T_d), (bf_bc, bf_d), (gk_t, gk_d)):
        P.dma("sp", lambda e, dst=dst, src=src: e.dma_start(out=dst[:], in_=src), writes=[Bconst])
    P.op("dve", lambda e: e.scalar_tensor_tensor(out=gqkT[0:64, :], in0=gqkT[0:64, :], scalar=HD ** -0.5, in1=gk_t[:],
                                                 op0=ALU.mult, op1=ALU.mult), reads=[Bconst], writes=[Bconst])
    P.barrier()
    if stage == "c":
        return finish_debug([("tri", tri[:], [128, 128], F32), ("Emat", Emat[:], [128, 128], F32),
                             ("ident", ident[:], [128, 128], BF16), ("gqk", gqkT[:], [65, 1], F32)])

    C0 = A.mark()
    yT = A.alloc([64, NH, NTOK], BF16)
    ByT = [Buf("yT%d" % j) for j in range(4)]
    m_attn = A.mark()
    KT = A.alloc([65, NH, S], BF16)
    QT = A.alloc([65, NH, NTOK], BF16)
    V = A.alloc([128, 32, NH, 65], BF16)
    negc = A.alloc([128, 32, NH], F32)
    BKT = [Buf("KT%d" % i) for i in range(32)]
    BQT = [Buf("QT%d" % i) for i in range(16)]
    BV = [Buf("V%d" % i) for i in range(32)]
    Bnegc = [Buf("negc%d" % i) for i in range(32)]

    m_p1 = A.mark()
    A.release(C0)
    Wqkv = A.alloc([128, 8, 1536], BF16)
    Wf = A.alloc([128, 8, 8], BF16)
    sqk = [A.alloc([128, 512], F32) for _ in range(2)]
    assert A.off <= C0 + 32768
    A.release(m_p1)
    wst = [A.alloc([128, 8, 256], F32) for _ in range(2)]
    Bwst = [Buf("wst0"), Buf("wst1")]
    BW = Buf("Wqkv")
    xst = [A.alloc([128, 8, 128], F32) for _ in range(2)]
    xb = [A.alloc([128, 8, 128], BF16) for _ in range(2)]
    sq = [A.alloc([128, 8, 128], BF16) for _ in range(2)]
    Bxst = [Buf("xst0"), Buf("xst1")]
    Bxb = [Buf("xb0"), Buf("xb1")]
    Bsq = [Buf("sq0"), Buf("sq1")]
    Bsqk = [Buf("sqk0"), Buf("sqk1")]
    Kaug = [A.alloc([128, NH, 65], BF16) for _ in range(2)]
    BKaug = [Buf("Kaug0"), Buf("Kaug1")]
    tmpq = A.alloc([128, 512], F32)
    Btmpq = Buf("tmpq")
    selw = A.alloc([128, 16, 32], F32)
    seltmp = A.alloc([128, 32, NH], F32)
    Bseltmp = Buf("seltmp")
    NSM = 4
    sm = [A.alloc([128, 64], F32) for _ in range(NSM)]
    Bsm = [Buf("sm%d" % i) for i in range(NSM)]

    w_in_v = w_in.rearrange("(k p) n -> p k n", p=128)
    BWq, BWk, BWv, BWf = Buf("Wq"), Buf("Wk"), Buf("Wv"), Buf("Wf")
    wpiece_n = [0]

    def load_piece(piece, Bdst):
        n_ = wpiece_n[0]
        wpiece_n[0] += 1
        wb = wst[n_ % 2]
        P.dma("sp", lambda e: e.dma_start(out=wb[:], in_=w_in_v[:, :, piece * 256:(piece + 1) * 256]), writes=[Bwst[n_ % 2]])
        for kc in range(8):
            P.op("act", lambda e, kc=kc: e.activation(out=Wqkv[:, kc, piece * 256:(piece + 1) * 256], in_=wb[:, kc, :], func=AF.Copy,
                                                      scale=g1T[:, kc:kc + 1]), reads=[Bwst[n_ % 2], Bconst], writes=[Bdst])

    def load_wf():
        n_ = wpiece_n[0]
        wpiece_n[0] += 1
        wb = wst[n_ % 2]
        P.dma("sp", lambda e: e.dma_start(out=wb[:, :, 0:8], in_=w_in_v[:, :, 1536:1544]), writes=[Bwst[n_ % 2]])
        for kc in range(8):
            P.op("act", lambda e, kc=kc: e.activation(out=Wf[:, kc, :], in_=wb[:, kc, 0:8], func=AF.Copy, scale=g1T[:, kc:kc + 1]),
                 reads=[Bwst[n_ % 2], Bconst], writes=[BWf])
    load_wf()
    load_piece(2, BWk)
    load_piece(3, BWk)
    load_piece(4, BWv)
    load_piece(5, BWv)
    P.dma("sp", lambda e: e.dma_start(out=selw[:], in_=sel_d), writes=[Bconst])
    for j in range(2):
        P.op("pool", lambda e, j=j: e.memset(Kaug[j][:, :, 64:65], 1.0), writes=[BKaug[j]])
    for i0 in range(0, 32, 8):
        P.op("pool", lambda e, i0=i0: e.memset(V[:, i0:i0 + 8, :, 64:65], 1.0), writes=[BV[i] for i in range(i0, i0 + 8)])

    xT_all_v = xT_all.rearrange("(k p) t -> p k t", p=128)
    xT_own_v = xT_own.rearrange("(k p) t -> p k t", p=128)

    def load_cast(src_v, gi, cnt):
        b = cnt % 2
        P.dma("sp", lambda e: e.dma_start(out=xst[b][:], in_=src_v[:, :, gi * 128:(gi + 1) * 128]), writes=[Bxst[b]])
        P.op("pool", lambda e: e.tensor_copy(out=xb[b][:], in_=xst[b][:]), reads=[Bxst[b]], writes=[Bxb[b]])
        P.op("act", lambda e: e.activation(out=sq[b][:], in_=xst[b][:], func=AF.Square), reads=[Bxst[b]], writes=[Bsq[b]])
        return b

    BmiscS = [Buf("mS0"), Buf("mS1")]
    BmiscF = [Buf("mF0"), Buf("mF1")]
    BmiscC = [Buf("mC0"), Buf("mC1")]

    def rms_stats_pe(b, misc, BmS):
        for kc in range(8):
            P.op("pe", lambda e, kc=kc: e.matmul(misc[:, 0:1], lhsT=sq[b][:, kc, :],
                                                  rhs=ones_bf[:, 0:1], start=(kc == 0), stop=(kc == 7)),
                 reads=[Bsq[b], Bconst], writes=[BmS], inc=(kc == 7))

    def rms_stats_act(misc, BmS, smt, Bs):
        P.op("act", lambda e: e.activation(out=smt[:, 0:1], in_=misc[:, 0:1], func=AF.Ln, bias=EPS, scale=1.0 / D),
             reads=[BmS], writes=[Bs])
        P.op("act", lambda e: e.activation(out=smt[:, 1:2], in_=smt[:, 0:1], func=AF.Exp, scale=-0.5), reads=[Bs], writes=[Bs])
        P.op("act", lambda e: e.activation(out=smt[:, 2:3], in_=smt[:, 0:1], func=AF.Exp, scale=-1.0,
                                           bias=float(-np.log(64.0))), reads=[Bs], writes=[Bs])

    def head_norm_scale(pX, BpX, smt, Bs, sqb, Bsqb):
        P.op("act", lambda e: e.activation(out=sqb[:], in_=pX[:], func=AF.Square), reads=[BpX], writes=[Bsqb])
        P.op("dve", lambda e: e.tensor_reduce(out=smt[:, 24:32], in_=sqb[:].rearrange("p (h d) -> p h d", h=NH),
                                              axis=AX.X, op=ALU.add), reads=[Bsqb], writes=[Bs])
        P.op("dve", lambda e: e.tensor_scalar(out=smt[:, 32:40], in0=smt[:, 24:32], scalar1=smt[:, 2:3], scalar2=None,
                                              op0=ALU.mult), reads=[Bs], writes=[Bs])
        P.op("act", lambda e: e.activation(out=smt[:, 32:40], in_=smt[:, 32:40], func=AF.Ln, bias=EPS), reads=[Bs], writes=[Bs])
        P.op("act", lambda e: e.activation(out=smt[:, 40:48], in_=smt[:, 32:40], func=AF.Exp, scale=-0.5), reads=[Bs], writes=[Bs])
        P.op("dve", lambda e: e.tensor_scalar(out=smt[:, 40:48], in0=smt[:, 40:48], scalar1=smt[:, 1:2], scalar2=None,
                                              op0=ALU.mult), reads=[Bs], writes=[Bs])

    if stage == "w":
        return finish_debug([("Wqkv", Wqkv[:], [128, 8, 1536], BF16), ("Wf", Wf[:], [128, 8, 8], BF16)])
    def kv_A(i):
        b = load_cast(xT_all_v, i, i)
        par = i % 2
        pK, BpK = ps[par], Bps[par]
        pV, BpV = ps[2 + par], Bps[2 + par]
        misc = ps[4 + par]
        BmS = BmiscS[par]
        for kc in range(8):
            st, sp_ = (kc == 0), (kc == 7)
            P.op("pe", lambda e, kc=kc, st=st, sp_=sp_: e.matmul(misc[:, 8:16], lhsT=xb[b][:, kc, :], rhs=Wf[:, kc, :], start=st, stop=sp_),
                 reads=[Bxb[b], BWf], writes=[BmS], inc=sp_)
        rms_stats_pe(b, misc, BmS)
        for kc in range(8):
            st, sp_ = (kc == 0), (kc == 7)
            P.op("pe", lambda e, kc=kc, st=st, sp_=sp_: e.matmul(pK[:], lhsT=xb[b][:, kc, :], rhs=Wqkv[:, kc, 512:1024], start=st, stop=sp_),
                 reads=[Bxb[b], BWk], writes=[BpK], inc=sp_)
            P.op("pe", lambda e, kc=kc, st=st, sp_=sp_: e.matmul(pV[:], lhsT=xb[b][:, kc, :], rhs=Wqkv[:, kc, 1024:1536], start=st, stop=sp_),
                 reads=[Bxb[b], BWv], writes=[BpV], inc=sp_)

    def kv_B1(i):
        par = i % 2
        pK, BpK = ps[par], Bps[par]
        pV, BpV = ps[2 + par], Bps[2 + par]
        misc = ps[4 + par]
        BmS = BmF = BmiscS[par]
        smt, Bs = sm[i % NSM], Bsm[i % NSM]
        rms_stats_act(misc, BmS, smt, Bs)
        P.op("dve", lambda e: e.scalar_tensor_tensor(out=smt[:, 8:16], in0=misc[:, 8:16], scalar=smt[:, 1:2], in1=bf_bc[:],
                                                     op0=ALU.mult, op1=ALU.add), reads=[BmF, Bs, Bconst], writes=[Bs])
        P.op("act", lambda e: e.activation(out=smt[:, 16:24], in_=smt[:, 8:16], func=AF.Exp, scale=-1.0), reads=[Bs], writes=[Bs])
        P.op("act", lambda e: e.activation(out=smt[:, 16:24], in_=smt[:, 16:24], func=AF.Ln, bias=1.0), reads=[Bs], writes=[Bs])
        head_norm_scale(pK, BpK, smt, Bs, sqk[par], Bsqk[par])
        P.op("dve", lambda e: e.tensor_tensor(out=Kaug[par][:, :, 0:64], in0=pK[:].rearrange("p (h d) -> p h d", h=NH),
                                              in1=bc_mid(smt[:, 40:48], 64), op=ALU.mult),
             reads=[BpK, Bs], writes=[BKaug[par]])
        P.op("act", lambda e: e.activation(out=V[:, i, :, 0:64], in_=pV[:].rearrange("p (h d) -> p h d", h=NH),
                                           func=AF.Copy, scale=smt[:, 1:2]), reads=[BpV, Bs], writes=[BV[i]])

    def kv_B2(i):
        par = i % 2
        misc = ps[4 + par]
        BmC = BmiscS[par]
        pT, BpT = ps[6 + par], Bps[6 + par]
        smt, Bs = sm[i % NSM], Bsm[i % NSM]
        P.op("pe", lambda e: e.matmul(misc[:, 16:24], lhsT=tri[:], rhs=smt[:, 16:24], start=True, stop=(i == 0)),
             reads=[Bs, Bconst], writes=[BmC], inc=(i == 0))
        if i > 0:
            P.op("pe", lambda e: e.matmul(misc[:, 16:24], lhsT=Emat[:], rhs=negc[:, i - 1, :], start=False, stop=True),
                 reads=[Bnegc[i - 1], Bconst], writes=[BmC], inc=True)
        P.op("dve", lambda e: e.tensor_copy(out=negc[:, i, :], in_=misc[:, 16:24]), reads=[BmC], writes=[Bnegc[i]])
        pTb = pT[:].bitcast(BF16).rearrange("p (h t) -> p h t", h=NH)
        for h in range(NH):
            P.op("pe", lambda e, h=h: e.transpose(out=pTb[0:65, h, :], in_=Kaug[par][:, h, :], identity=ident[:]),
                 reads=[BKaug[par], Bconst], writes=[BpT], inc=(h == NH - 1))
        P.op("dve", lambda e: e.tensor_copy(out=KT[:, :, i * 128:(i + 1) * 128], in_=pTb[0:65, :, :]),
             reads=[BpT], writes=[BKT[i]])

    def q_A(t):
        b = load_cast(xT_own_v, t, 32 + t)
        par = t % 2
        pQ, BpQ = ps[t % 4], Bps[t % 4]
        misc = ps[4 + par]
        rms_stats_pe(b, misc, BmiscS[par])
        for kc in range(8):
            st, sp_ = (kc == 0), (kc == 7)
            P.op("pe", lambda e, kc=kc, st=st, sp_=sp_: e.matmul(pQ[:], lhsT=xb[b][:, kc, :], rhs=Wqkv[:, kc, 0:512], start=st, stop=sp_),
                 reads=[Bxb[b], BWq], writes=[BpQ], inc=sp_)

    def q_B1(t):
        par = t % 2
        pQ, BpQ = ps[t % 4], Bps[t % 4]
        misc = ps[4 + par]
        smt, Bs = sm[t % NSM], Bsm[t % NSM]
        rms_stats_act(misc, BmiscS[par], smt, Bs)
        head_norm_scale(pQ, BpQ, smt, Bs, sqk[par], Bsqk[par])
        P.op("dve", lambda e: e.tensor_tensor(out=Kaug[par][:, :, 0:64], in0=pQ[:].rearrange("p (h d) -> p h d", h=NH),
                                              in1=bc_mid(smt[:, 40:48], 64), op=ALU.mult),
             reads=[BpQ, Bs], writes=[BKaug[par]])
        P.op("dve", lambda e: e.tensor_tensor(out=seltmp[:], in0=negc[:], in1=bc_mid(selw[:, t, :], NH), op=ALU.mult),
             reads=Bnegc + [Bconst], writes=[Bseltmp])
        P.op("dve", lambda e: e.tensor_reduce(out=smt[:, 48:56], in_=seltmp[:].rearrange("p i h -> p h i"), axis=AX.X, op=ALU.add),
             reads=[Bseltmp], writes=[Bs])
        P.op("dve", lambda e: e.tensor_scalar(out=Kaug[par][:, :, 64:65], in0=smt[:, 48:56].unsqueeze(2), scalar1=-1.0, scalar2=None,
                                              op0=ALU.mult), reads=[Bs], writes=[BKaug[par]])

    def q_B2(t):
        par = t % 2
        pT, BpT = ps[6 + par], Bps[6 + par]
        pTb = pT[:].bitcast(BF16).rearrange("p (h t) -> p h t", h=NH)
        for h in range(NH):
            P.op("pe", lambda e, h=h: e.transpose(out=pTb[0:65, h, :], in_=Kaug[par][:, h, :], identity=ident[:]),
                 reads=[BKaug[par], Bconst], writes=[BpT], inc=(h == NH - 1))
        P.op("act", lambda e: e.activation(out=QT[:, :, t * 128:(t + 1) * 128], in_=pTb[0:65, :, :], func=AF.Copy,
                                           scale=gqkT[0:65, 0:1]), reads=[BpT, Bconst], writes=[BQT[t]])

    tiles = [(kv_A, kv_B1, kv_B2, i) for i in range(nkv)] + [(q_A, q_B1, q_B2, t) for t in range(nq)]
    if stage == "k":
        tiles = tiles[:nkv]
    tiles[0][0](tiles[0][3])
    LAGQ = True
    for n_, (fa, fb1, fb2, ix) in enumerate(tiles):
        if n_ + 1 < len(tiles):
            tiles[n_ + 1][0](tiles[n_ + 1][3])
        fb1(ix)
        if n_ < nkv or not LAGQ:
            fb2(ix)
        elif n_ - 1 >= nkv:
            tiles[n_ - 1][2](tiles[n_ - 1][3])
        if n_ == 2:
            load_piece(0, BWq)
        if n_ == 4:
            load_piece(1, BWq)
    if LAGQ and len(tiles) > nkv:
        tiles[-1][2](tiles[-1][3])
    if stage == "k":
        return finish_debug([("negc", negc[:], [128, 32, NH], F32), ("KT", KT[:, :, 0:nkv * 128], [65, NH, nkv * 128], BF16),
                             ("V", V[:, 0:nkv], [128, nkv, NH, 65], BF16)])
    if stage == "q":
        return finish_debug([("QT", QT[:, :, 0:nq * 128], [65, NH, nq * 128], BF16)])
    P.barrier()
    A.release(m_p1)

    m_p2 = A.mark()
    NPT = 4
    PT = [A.alloc([128, 512], BF16) for _ in range(NPT)]
    BPT = [Buf("PT%d" % i) for i in range(NPT)]
    maskt = A.alloc([128, 2, 8, 512], BF16)
    Osb = [A.alloc([65, 512], F32) for _ in range(2)]
    BOsb = [Buf("Osb0"), Buf("Osb1")]
    rden = [A.alloc([65, 512], F32) for _ in range(2)]
    Brden = [Buf("rden0"), Buf("rden1")]
    Bmask = Buf("mask")
    P.dma("sp", lambda e: e.dma_start(out=maskt[:], in_=mask_d), writes=[Bmask])

    PSB = (0, 1, 2, 7)
    LA = 3
    steps = []
    hj_of = {}
    for h in range(NH):
        for j in range(4):
            hj_of[(h, j)] = len(hj_of)
            for kb in range(8 * (j + 1)):
                steps.append((h, j, kb))
    nst = len(steps)

    def emit_qk(s_):
        h, j, kb = steps[s_]
        pS, BpS = ps[PSB[s_ % 4]], Bps[PSB[s_ % 4]]
        mk = kb - 8 * j
        P.op("pe", lambda e: e.matmul(pS[:], lhsT=KT[:, h, kb * 128:(kb + 1) * 128],
                                      rhs=QT[:, h, j * 512:(j + 1) * 512], start=True, stop=(mk < 0)),
             reads=[BKT[kb]] + BQT[4 * j:4 * j + 4], writes=[BpS], inc=(mk < 0))
        if mk >= 0:
            P.op("pe", lambda e: e.matmul(pS[:], lhsT=ident[:], rhs=maskt[:, j % 2, mk, :], start=False, stop=True),
                 reads=[Bmask, Bconst], writes=[BpS], inc=True)

    def emit_exp_pv(s_):
        h, j, kb = steps[s_]
        nkb = 8 * (j + 1)
        hj = hj_of[(h, j)]
        pS, BpS = ps[PSB[s_ % 4]], Bps[PSB[s_ % 4]]
        pt, Bpt = PT[s_ % NPT], BPT[s_ % NPT]
        pO, BpO = ps[3 + (hj % 2)], Bps[3 + (hj % 2)]
        P.op("act", lambda e: e.activation(out=pt[:], in_=pS[:], func=AF.Exp, bias=negc[:, kb, h:h + 1], scale=1.0),
             reads=[BpS, Bnegc[kb]], writes=[Bpt])
        P.op("pe", lambda e: e.matmul(pO[0:65, :], lhsT=V[:, kb, h, :], rhs=pt[:], start=(kb == 0), stop=(kb == nkb - 1)),
             reads=[Bpt, BV[kb]], writes=[BpO], inc=(kb == nkb - 1))

    def norm_a(h, j):
        hj = hj_of[(h, j)]
        pO, BpO = ps[3 + (hj % 2)], Bps[3 + (hj % 2)]
        ob, Bob = Osb[hj % 2], BOsb[hj % 2]
        rd, Brd = rden[hj % 2], Brden[hj % 2]
        P.op("dve", lambda e: e.tensor_copy(out=ob[:], in_=pO[0:65, :]), reads=[BpO], writes=[Bob])
        P.op("act", lambda e: e.activation(out=rd[64:65, :], in_=ob[64:65, :], func=AF.Ln), reads=[Bob], writes=[Brd])
        P.op("act", lambda e: e.activation(out=rd[64:65, :], in_=rd[64:65, :], func=AF.Exp, scale=-1.0), reads=[Brd], writes=[Brd])

    def norm_b(h, j):
        hj = hj_of[(h, j)]
        ob, Bob = Osb[hj % 2], BOsb[hj % 2]
        rd, Brd = rden[hj % 2], Brden[hj % 2]
        pB, BpB = ps[5 + (hj % 2)], Bps[5 + (hj % 2)]
        P.op("pe", lambda e: e.matmul(pB[0:64, :], lhsT=ones_f[64:65, 0:64], rhs=rd[64:65, :], start=True, stop=True),
             reads=[Brd, Bconst], writes=[BpB], inc=True)
        P.op("dve", lambda e: e.tensor_tensor(out=yT[:, h, j * 512:(j + 1) * 512], in0=ob[0:64, :], in1=pB[0:64, :], op=ALU.mult),
             reads=[Bob, BpB], writes=[ByT[j]])

    for s_ in range(min(LA, nst)):
        emit_qk(s_)
    deferred = []
    for s_ in range(nst):
        if s_ + LA < nst:
            emit_qk(s_ + LA)
        emit_exp_pv(s_)
        h, j, kb = steps[s_]
        if kb == 8 * (j + 1) - 1:
            norm_a(h, j)
            deferred.append((s_ + 4, h, j))
        while deferred and deferred[0][0] <= s_:
            _, h2_, j2_ = deferred.pop(0)
            norm_b(h2_, j2_)
    for _, h2_, j2_ in deferred:
        norm_b(h2_, j2_)
    P.barrier()
    A.release(m_p2)

    if stage == 2:
        Bd = Buf("dbg")
        P.dma("sp", lambda e: e.dma_start(out=dbg, in_=yT[:]), reads=ByT, writes=[Bd])
        P.barrier()
        P.emit()
        return nc

    A.release(C0 + 65536)
    NSTG = 3
    stg = [A.alloc([128, 2048], F32) for _ in range(NSTG)]
    Bstg = [Buf("stg%d" % i) for i in range(NSTG)]
    stg_n = [0]

    wbufs = {}

    def load_w(dst, src, K, N, gT=None, parts=128, key=None):
        i = stg_n[0] % NSTG
        stg_n[0] += 1
        st_ = stg[i][0:parts, 0:K * N].rearrange("p (k n) -> p k n", k=K)
        Bst = Bstg[i]
        if key is None:
            Bd_ = Buf("w")
        else:
            Bd_ = wbufs.setdefault(key, Buf("w" + key))
        P.dma("sp", lambda e: e.dma_start(out=st_, in_=src), writes=[Bst])
        if gT is None:
            P.op("act", lambda e: e.activation(out=dst, in_=st_, func=AF.Copy), reads=[Bst], writes=[Bd_])
        else:
            for kc in range(K):
                P.op("act", lambda e, kc=kc: e.activation(out=dst[:, kc, :], in_=st_[:, kc, :], func=AF.Copy, scale=gT[:, kc:kc + 1]),
                     reads=[Bst, Bconst], writes=[Bd_])
        return Bd_

    def rnorm_chunk(src_f32, Bsrc, dst_bf, Bdst, sqc, Bsqc, Rt, BR, pR, BpR, ntok):
        Bsrc_l = Bsrc if isinstance(Bsrc, list) else [Bsrc]
        P.op("act", lambda e: e.activation(out=sqc, in_=src_f32, func=AF.Square), reads=Bsrc_l, writes=[Bsqc])
        for kc in range(8):
            P.op("pe", lambda e, kc=kc: e.matmul(pR[:, 0:ntok], lhsT=ones_bf[:], rhs=sqc[:, kc, :], start=(kc == 0), stop=(kc == 7)),
                 reads=[Bsqc, Bconst], writes=[BpR], inc=(kc == 7))
        P.op("act", lambda e: e.activation(out=Rt[:, 0:ntok], in_=pR[:, 0:ntok], func=AF.Ln, bias=EPS, scale=1.0 / D), reads=[BpR], writes=[BR])
        P.op("act", lambda e: e.activation(out=Rt[:, 0:ntok], in_=Rt[:, 0:ntok], func=AF.Exp, scale=-0.5), reads=[BR], writes=[BR])
        P.op("dve", lambda e: e.tensor_tensor(out=dst_bf, in0=src_f32, in1=Rt[:, 0:ntok].unsqueeze(1).to_broadcast([128, 8, ntok]), op=ALU.mult),
             reads=Bsrc_l + [BR], writes=[Bdst])

    m_p3 = A.mark()
    h1T = A.alloc([128, 8, NTOK], BF16)
    Bh1T = [Buf("h1T%d" % j) for j in range(4)]
    hhT = A.alloc([128, 8, 8], BF16)
    BhhT = Buf("hhT")
    BmT_ = None
    m_3a = A.mark()
    xc = [A.alloc([128, 8, 512], F32) for _ in range(2)]
    Bxc = [Buf("xc0"), Buf("xc1")]
    sqc_l = [A.alloc([128, 8, 512], BF16) for _ in range(2)]
    Bsqc_l = [Buf("sqc0"), Buf("sqc1")]
    Rt_l = [A.alloc([128, 512], F32) for _ in range(2)]
    BR_l = [Buf("R0"), Buf("R1")]
    sqc, Bsqc, Rt, BR = sqc_l[0], Bsqc_l[0], Rt_l[0], BR_l[0]
    xh = A.alloc([128, 8, 8], F32)
    Bxh = Buf("xh")

    def p3a_a(j):
        b = j % 2
        P.dma("sp", lambda e: e.dma_start(out=xc[b][:], in_=xT_own_v[:, :, j * 512:(j + 1) * 512]), writes=[Bxc[b]])
        P.op("act", lambda e: e.activation(out=sqc_l[b][:], in_=xc[b][:], func=AF.Square), reads=[Bxc[b]], writes=[Bsqc_l[b]])
        pR, BpR = ps[b], Bps[b]
        for kc in range(8):
            P.op("pe", lambda e, kc=kc: e.matmul(pR[:], lhsT=ones_bf[:], rhs=sqc_l[b][:, kc, :], start=(kc == 0), stop=(kc == 7)),
                 reads=[Bsqc_l[b], Bconst], writes=[BpR], inc=(kc == 7))

    def p3a_b(j):
        b = j % 2
        pR, BpR = ps[b], Bps[b]
        P.op("act", lambda e: e.activation(out=Rt_l[b][:], in_=pR[:], func=AF.Ln, bias=EPS, scale=1.0 / D), reads=[BpR], writes=[BR_l[b]])
        P.op("act", lambda e: e.activation(out=Rt_l[b][:], in_=Rt_l[b][:], func=AF.Exp, scale=-0.5), reads=[BR_l[b]], writes=[BR_l[b]])
        P.op("dve", lambda e: e.tensor_tensor(out=h1T[:, :, j * 512:(j + 1) * 512], in0=xc[b][:],
                                              in1=Rt_l[b][:].unsqueeze(1).to_broadcast([128, 8, 512]), op=ALU.mult),
             reads=[Bxc[b], BR_l[b]], writes=[Bh1T[j]])
    p3a_a(0)
    for j in range(4):
        if j + 1 < 4:
            p3a_a(j + 1)
        p3a_b(j)
    P.dma("sp", lambda e: e.dma_start(out=xh[:], in_=xhT.rearrange("(k p) t -> p k t", p=128)), writes=[Bxh])
    rnorm_chunk(xh[:], Bxh, hhT[:], BhhT, sqc[:, :, 0:8], Bsqc, Rt, BR, ps[2], Bps[2], 8)
    P.barrier()
    if stage == "3a":
        return finish_debug([("h1T", h1T[:], [128, 8, NTOK], BF16), ("hhT", hhT[:], [128, 8, 8], BF16)])
    A.release(m_3a)

    BmT = A.alloc([128, 4, NTOK], BF16)
    BBmT = [Buf("BmT%d" % j) for j in range(4)]
    m_3b = A.mark()
    convw = A.alloc([128, 4, 3], F32)
    P.dma("sp", lambda e: e.dma_start(out=convw[:], in_=conv_d), writes=[Bconst])
    wcv = [[A.alloc([128, 8, 128], BF16) for _ in range(3)] for _ in range(2)]
    ubuf = [A.alloc([128, 514], F32) for _ in range(2)]
    Bubuf = [Buf("u0"), Buf("u1")]
    cct = [A.alloc([128, 514], F32) for _ in range(2)]
    Bcct = [Buf("cct0"), Buf("cct1")]
    tcv = [A.alloc([128, 512], F32) for _ in range(2)]
    Btcv = [Buf("tcv0"), Buf("tcv1")]

    def p3b(ci, j, n, Bw):
        b = n % 2
        pcb, pcc, pcu = ps[3 * b], ps[3 * b + 1], ps[3 * b + 2]
        Bpcb, Bpcc, Bpcu = Bps[3 * b], Bps[3 * b + 1], Bps[3 * b + 2]
        ph, Bph = ps[6 + ci % 2], Bps[6 + ci % 2]
        w3 = wcv[ci % 2]
        for wi, (pp, Bpp) in enumerate(((pcb, Bpcb), (pcc, Bpcc), (pcu, Bpcu))):
            for kc in range(8):
                P.op("pe", lambda e, kc=kc, wi=wi, pp=pp: e.matmul(pp[:], lhsT=w3[wi][:, kc, :], rhs=h1T[:, kc, j * 512:(j + 1) * 512],
                                                                    start=(kc == 0), stop=(kc == 7)),
                     reads=[Bw[wi], Bh1T[j]], writes=[Bpp], inc=(kc == 7))
        if j == 0:
            for wi in (1, 2):
                for kc in range(8):
                    P.op("pe", lambda e, kc=kc, wi=wi: e.matmul(ph[:, 8 * wi:8 * wi + 8], lhsT=w3[wi][:, kc, :], rhs=hhT[:, kc, 0:8],
                                                                 start=(kc == 0), stop=(kc == 7)),
                         reads=[Bw[wi], BhhT], writes=[Bph], inc=(kc == 7))
        ct, Bct = cct[b], Bcct[b]
        ub, Bub = ubuf[b], Bubuf[b]
        tv, Btv = tcv[b], Btcv[b]
        P.op("act", lambda e: e.activation(out=ct[:, 2:514], in_=pcc[:], func=AF.Copy), reads=[Bpcc], writes=[Bct])
        P.op("act", lambda e: e.activation(out=ct[:, 0:2], in_=ph[:, 8 + 2 * j:10 + 2 * j], func=AF.Copy), reads=[Bph], writes=[Bct])
        P.op("dve", lambda e: e.tensor_tensor(out=ub[:, 2:514], in0=ct[:, 2:514], in1=pcu[:], op=ALU.mult), reads=[Bct, Bpcu], writes=[Bub])
        P.op("dve", lambda e: e.tensor_tensor(out=ub[:, 0:2], in0=ct[:, 0:2], in1=ph[:, 16 + 2 * j:18 + 2 * j], op=ALU.mult), reads=[Bct, Bph], writes=[Bub])
        P.op("dve", lambda e: e.tensor_scalar(out=tv[:], in0=ub[:, 0:512], scalar1=convw[:, ci, 0:1], scalar2=None, op0=ALU.mult),
             reads=[Bub, Bconst], writes=[Btv])
        P.op("dve", lambda e: e.scalar_tensor_tensor(out=tv[:], in0=ub[:, 1:513], scalar=convw[:, ci, 1:2], in1=tv[:], op0=ALU.mult, op1=ALU.add),
             reads=[Bub, Btv, Bconst], writes=[Btv])
        P.op("dve", lambda e: e.scalar_tensor_tensor(out=tv[:], in0=ub[:, 2:514], scalar=convw[:, ci, 2:3], in1=tv[:], op0=ALU.mult, op1=ALU.add),
             reads=[Bub, Btv, Bconst], writes=[Btv])
        P.op("dve", lambda e: e.tensor_tensor(out=BmT[:, ci, j * 512:(j + 1) * 512], in0=tv[:], in1=pcb[:], op=ALU.mult),
             reads=[Btv, Bpcb], writes=[BBmT[j]])

    def load_3b(ci):
        Bw_ = []
        for wi, base in enumerate((1544, 2056, 2568)):
            c0 = base + ci * 128
            Bw_.append(load_w(wcv[ci % 2][wi][:], w_in_v[:, :, c0:c0 + 128], 8, 128, g1T, key="cv%d_%d" % (ci % 2, wi)))
        return Bw_
    n3b = 0
    Bw_next = load_3b(0)
    for ci in range(4):
        Bw = Bw_next
        if ci + 1 < 4:
            Bw_next = load_3b(ci + 1)
        for j in range(4):
            p3b(ci, j, n3b, Bw)
            n3b += 1
    P.barrier()
    if stage == "3b":
        return finish_debug([("BmT", BmT[:], [128, 4, NTOK], BF16)])
    A.release(m_3b)

    mT = nc.alloc_sbuf_tensor_at("mT", [128, 8, NTOK], BF16, offset=SB_END - 32768)
    BmTb = [Buf("mT%d" % j) for j in range(4)]
    m_3c = A.mark()
    wga = [A.alloc([128, 8, 128], BF16) for _ in range(2)]
    wgb = [A.alloc([128, 8, 128], BF16) for _ in range(2)]
    wA = [A.alloc([64, 8, 128], BF16) for _ in range(2)]
    wB = [A.alloc([128, 4, 128], BF16) for _ in range(2)]
    sga = [A.alloc([128, 512], F32) for _ in range(2)]
    sgb = [A.alloc([128, 512], F32) for _ in range(2)]
    Bsga = [Buf("sga0"), Buf("sga1")]
    Bsgb = [Buf("sgb0"), Buf("sgb1")]
    w_oa_v = w_oa.rearrange("(h p) n -> p h n", p=64)
    w_oc_v = w_oc.rearrange("(k p) n -> p k n", p=128)

    def p3c(m, j, n, Bw):
        b = n % 2
        pga, pgb, pA, pB_ = ps[4 * b], ps[4 * b + 1], ps[4 * b + 2], ps[4 * b + 3]
        Bpga, Bpgb, BpA, BpB_ = Bps[4 * b], Bps[4 * b + 1], Bps[4 * b + 2], Bps[4 * b + 3]
        wb_ = m % 2
        tok = slice(j * 512, (j + 1) * 512)
        for kc in range(8):
            P.op("pe", lambda e, kc=kc: e.matmul(pga[:], lhsT=wga[wb_][:, kc, :], rhs=h1T[:, kc, tok], start=(kc == 0), stop=(kc == 7)),
                 reads=[Bw[0], Bh1T[j]], writes=[Bpga], inc=(kc == 7))
        for kc in range(8):
            P.op("pe", lambda e, kc=kc: e.matmul(pgb[:], lhsT=wgb[wb_][:, kc, :], rhs=h1T[:, kc, tok], start=(kc == 0), stop=(kc == 7)),
                 reads=[Bw[1], Bh1T[j]], writes=[Bpgb], inc=(kc == 7))
        for h in range(8):
            P.op("pe", lambda e, h=h: e.matmul(pA[:], lhsT=wA[wb_][:, h, :], rhs=yT[:, h, tok], start=(h == 0), stop=(h == 7)),
                 reads=[Bw[2], ByT[j]], writes=[BpA], inc=(h == 7))
        for kc in range(4):
            P.op("pe", lambda e, kc=kc: e.matmul(pB_[:], lhsT=wB[wb_][:, kc, :], rhs=BmT[:, kc, tok], start=(kc == 0), stop=(kc == 3)),
                 reads=[Bw[3], BBmT[j]], writes=[BpB_], inc=(kc == 3))
        P.op("act", lambda e: e.activation(out=sga[b][:], in_=pga[:], func=AF.Sigmoid), reads=[Bpga], writes=[Bsga[b]])
        P.op("act", lambda e: e.activation(out=sgb[b][:], in_=pgb[:], func=AF.Sigmoid), reads=[Bpgb], writes=[Bsgb[b]])
        P.op("dve", lambda e: e.tensor_tensor(out=sga[b][:], in0=sga[b][:], in1=pA[:], op=ALU.mult), reads=[Bsga[b], BpA], writes=[Bsga[b]])
        P.op("dve", lambda e: e.tensor_tensor(out=sgb[b][:], in0=sgb[b][:], in1=pB_[:], op=ALU.mult), reads=[Bsgb[b], BpB_], writes=[Bsgb[b]])
        P.op("dve", lambda e: e.tensor_tensor(out=mT[:, m, tok], in0=sga[b][:], in1=sgb[b][:], op=ALU.add),
             reads=[Bsga[b], Bsgb[b]], writes=[BmTb[j]])

    def load_3c(m):
        c = m * 128
        return [load_w(wga[m % 2][:], w_in_v[:, :, 3080 + c:3080 + c + 128], 8, 128, g1T, key="ga%d" % (m % 2)),
                load_w(wgb[m % 2][:], w_in_v[:, :, 4104 + c:4104 + c + 128], 8, 128, g1T, key="gb%d" % (m % 2)),
                load_w(wA[m % 2][:], w_oa_v[:, :, c:c + 128], 8, 128, None, parts=64, key="wA%d" % (m % 2)),
                load_w(wB[m % 2][:], w_oc_v[:, :, c:c + 128], 4, 128, None, key="wB%d" % (m % 2))]
    n3c = 0
    Bw_next = load_3c(0)
    for m in range(8):
        Bw = Bw_next
        if m + 1 < 8:
            Bw_next = load_3c(m + 1)
        for j in range(4):
            p3c(m, j, n3c, Bw)
            n3c += 1
    P.barrier()
    A.release(m_p3)
    if stage == "3c":
        return finish_debug([("mT", mT[:], [128, 8, NTOK], BF16)])


    xT = nc.alloc_sbuf_tensor_at("xTres", [128, 8, NTOK], F32, offset=C0)
    BxT = [[Buf("xT%d_%d" % (m, j)) for j in range(4)] for m in range(8)]
    m_3d = A.mark()
    wo = [A.alloc([128, 8, 128], BF16) for _ in range(2)]
    w_o_v = w_o.rearrange("(k p) n -> p k n", p=128)

    def p3d(m, j, n, Bw):
        pp, Bpp = ps[n % 4], Bps[n % 4]
        tok = slice(j * 512, (j + 1) * 512)
        for kc in range(8):
            P.op("pe", lambda e, kc=kc: e.matmul(pp[:], lhsT=wo[m % 2][:, kc, :], rhs=mT[:, kc, tok], start=(kc == 0), stop=(kc == 7)),
                 reads=[Bw, BmTb[j]], writes=[Bpp], inc=(kc == 7))
        P.op("dve", lambda e: e.tensor_tensor(out=xT[:, m, tok], in0=pp[:], in1=xT[:, m, tok], op=ALU.add),
             reads=[Bpp, BxT[m][j]], writes=[BxT[m][j]])

    def p3d_load(m):
        Bw_ = load_w(wo[m % 2][:], w_o_v[:, :, m * 128:(m + 1) * 128], 8, 128, None, key="wo%d" % (m % 2))
        P.dma("sp", lambda e: e.dma_start(out=xT[:, m, :], in_=xT_own[m * 128:(m + 1) * 128, :]), writes=BxT[m])
        return Bw_
    Bw_nx = p3d_load(0)
    for m in range(8):
        Bw_cur = Bw_nx
        if m + 1 < 8:
            Bw_nx = p3d_load(m + 1)
        for j in range(4):
            p3d(m, j, m * 4 + j, Bw_cur)
    P.barrier()
    A.release(m_3d)

    h2T = A.alloc([128, 8, NTOK], BF16)
    Bh2T = [Buf("h2T%d" % j) for j in range(4)]
    comb = A.alloc([128, 16, 16], F32)
    Bcomb = [Buf("comb%d" % t) for t in range(16)]
    m_3e = A.mark()
    sqc2_l = [A.alloc([128, 8, 512], BF16) for _ in range(2)]
    Bsqc2_l = [Buf("sqc2a"), Buf("sqc2b")]
    Rt2_l = [A.alloc([128, 512], F32) for _ in range(2)]
    BR2_l = [Buf("R2a"), Buf("R2b")]
    Wr = A.alloc([128, 8, 20], BF16)
    br_t = A.alloc([128, 20], F32)
    BWr = load_w(Wr[:], w_r.rearrange("(k p) n -> p k n", p=128), 8, 20, g2T)
    P.dma("sp", lambda e: e.dma_start(out=br_t[:], in_=br_d), writes=[Bconst])

    def p3e_a(j):
        b = j % 2
        tok = slice(j * 512, (j + 1) * 512)
        pR, BpR = ps[b], Bps[b]
        P.op("act", lambda e: e.activation(out=sqc2_l[b][:], in_=xT[:, :, tok], func=AF.Square),
             reads=[BxT[m][j] for m in range(8)], writes=[Bsqc2_l[b]])
        for kc in range(8):
            P.op("pe", lambda e, kc=kc: e.matmul(pR[:], lhsT=ones_bf[:], rhs=sqc2_l[b][:, kc, :], start=(kc == 0), stop=(kc == 7)),
                 reads=[Bsqc2_l[b], Bconst], writes=[BpR], inc=(kc == 7))

    def p3e_b(j):
        b = j % 2
        tok = slice(j * 512, (j + 1) * 512)
        pR, BpR = ps[b], Bps[b]
        P.op("act", lambda e: e.activation(out=Rt2_l[b][:], in_=pR[:], func=AF.Ln, bias=EPS, scale=1.0 / D), reads=[BpR], writes=[BR2_l[b]])
        P.op("act", lambda e: e.activation(out=Rt2_l[b][:], in_=Rt2_l[b][:], func=AF.Exp, scale=-0.5), reads=[BR2_l[b]], writes=[BR2_l[b]])
        P.op("dve", lambda e: e.tensor_tensor(out=h2T[:, :, tok], in0=xT[:, :, tok],
                                              in1=Rt2_l[b][:].unsqueeze(1).to_broadcast([128, 8, 512]), op=ALU.mult),
             reads=[BxT[m][j] for m in range(8)] + [BR2_l[b]], writes=[Bh2T[j]])
    p3e_a(0)
    for j in range(4):
        if j + 1 < 4:
            p3e_a(j + 1)
        p3e_b(j)

    pl, Bpl = ps[4], Bps[4]

    def route_mm(t):
        for kc in range(8):
            P.op("pe", lambda e, kc=kc: e.matmul(pl[:, t * 20:(t + 1) * 20], lhsT=h2T[:, kc, t * 128:(t + 1) * 128], rhs=Wr[:, kc, :],
                                                 start=(kc == 0), stop=(kc == 7)),
                 reads=[Bh2T[t // 4], BWr], writes=[Bpl], inc=(kc == 7))
    for t in range(16):
        route_mm(t)

    def ra(n):
        return A.alloc([128, 16, n], F32)
    Lb, dg, ge, oh, tmpr, ein, d1, mk1, e2, d2, sel, wv = ra(20), ra(4), ra(4), ra(4), ra(16), ra(4), ra(4), ra(4), ra(4), ra(4), ra(4), ra(4)
    gmax, gsum, gval, m1, m2, wsum, sc = (A.alloc([128, 16], F32) for _ in range(7))
    BRr = Buf("route")

    def dv(fn, rd=()):
        P.op("dve", fn, reads=[BRr] + list(rd), writes=[BRr])

    def bc4(ap2):
        return ap2.unsqueeze(2).to_broadcast([128, 16, 4])
    dv(lambda e: e.tensor_tensor(out=Lb[:], in0=pl[:, 0:320].rearrange("p (t c) -> p t c", c=20),
                                 in1=br_t[:].unsqueeze(1).to_broadcast([128, 16, 20]), op=ALU.add), rd=[Bpl, Bconst])
    dv(lambda e: e.tensor_reduce(out=gmax[:], in_=Lb[:, :, 0:4], axis=AX.X, op=ALU.max))
    dv(lambda e: e.tensor_tensor(out=dg[:], in0=Lb[:, :, 0:4], in1=bc4(gmax[:]), op=ALU.subtract))
    P.op("act", lambda e: e.activation(out=ge[:], in_=dg[:], func=AF.Exp), reads=[BRr], writes=[BRr])
    dv(lambda e: e.tensor_reduce(out=gsum[:], in_=ge[:], axis=AX.X, op=ALU.add))
    dv(lambda e: e.reciprocal(out=gval[:], in_=gsum[:]))
    dv(lambda e: e.tensor_scalar(out=oh[:], in0=dg[:], scalar1=0.0, scalar2=None, op0=ALU.is_equal))
    dv(lambda e: e.tensor_tensor(out=tmpr[:].rearrange("p t (g j) -> p t g j", g=4), in0=Lb[:, :, 4:20].rearrange("p t (g j) -> p t g j", g=4),
                                 in1=oh[:].unsqueeze(3).to_broadcast([128, 16, 4, 4]), op=ALU.mult))
    dv(lambda e: e.tensor_reduce(out=ein[:], in_=tmpr[:].rearrange("p t (g j) -> p t j g", g=4), axis=AX.X, op=ALU.add))
    dv(lambda e: e.tensor_reduce(out=m1[:], in_=ein[:], axis=AX.X, op=ALU.max))
    dv(lambda e: e.tensor_tensor(out=d1[:], in0=ein[:], in1=bc4(m1[:]), op=ALU.subtract))
    dv(lambda e: e.tensor_scalar(out=mk1[:], in0=d1[:], scalar1=0.0, scalar2=None, op0=ALU.is_equal))
    dv(lambda e: e.scalar_tensor_tensor(out=e2[:], in0=mk1[:], scalar=-1e30, in1=d1[:], op0=ALU.mult, op1=ALU.add))
    dv(lambda e: e.tensor_reduce(out=m2[:], in_=e2[:], axis=AX.X, op=ALU.max))
    dv(lambda e: e.tensor_tensor(out=d2[:], in0=e2[:], in1=bc4(m2[:]), op=ALU.subtract))
    dv(lambda e: e.tensor_scalar(out=sel[:], in0=d2[:], scalar1=0.0, scalar2=None, op0=ALU.is_equal))
    dv(lambda e: e.tensor_tensor(out=sel[:], in0=sel[:], in1=mk1[:], op=ALU.add))
    P.op("act", lambda e: e.activation(out=wv[:], in_=d1[:], func=AF.Exp), reads=[BRr], writes=[BRr])
    dv(lambda e: e.tensor_tensor(out=wv[:], in0=wv[:], in1=sel[:], op=ALU.mult))
    dv(lambda e: e.tensor_reduce(out=wsum[:], in_=wv[:], axis=AX.X, op=ALU.add))
    dv(lambda e: e.reciprocal(out=sc[:], in_=wsum[:]))
    dv(lambda e: e.tensor_tensor(out=sc[:], in0=sc[:], in1=gval[:], op=ALU.mult))
    dv(lambda e: e.tensor_tensor(out=wv[:], in0=wv[:], in1=bc4(sc[:]), op=ALU.mult))
    P.op("dve", lambda e: e.tensor_tensor(out=comb[:].rearrange("p t (g j) -> p t g j", g=4),
                                          in0=oh[:].unsqueeze(3).to_broadcast([128, 16, 4, 4]),
                                          in1=wv[:].unsqueeze(2).to_broadcast([128, 16, 4, 4]), op=ALU.mult),
         reads=[BRr], writes=Bcomb)
    P.barrier()
    if stage == "3e":
        return finish_debug([("xT", xT[:], [128, 8, NTOK], F32), ("comb", comb[:], [128, 16, 16], F32)])
    A.release(m_3e)

    m_p4 = A.mark()
    Wgu = [A.alloc([128, 8, 512], BF16) for _ in range(2)]
    Wd = [A.alloc([128, 2, 1024], BF16) for _ in range(2)]
    BWg = [Buf("Wg0"), Buf("Wg1")]
    BWu = [Buf("Wu0"), Buf("Wu1")]
    BWd = [Buf("Wd0"), Buf("Wd1")]
    mstg = [A.alloc([128, 2048], F32) for _ in range(6)]
    Bmstg = [Buf("mstg%d" % i) for i in range(6)]
    sa = [A.alloc([128, 256], F32) for _ in range(2)]
    Bsa = [Buf("sa0"), Buf("sa1")]
    hid = [A.alloc([128, 256], BF16) for _ in range(2)]
    Bhid = [Buf("hid0"), Buf("hid1")]
    hidT = [A.alloc([128, 2, 512], BF16) for _ in range(2)]
    BhidT = [Buf("hidT0"), Buf("hidT1")]

    def moe_wdma(e):
        base = (e % 2) * 3
        srcs = (w_gate[e].rearrange("(k p) n -> p k n", p=128), w_up[e].rearrange("(k p) n -> p k n", p=128),
                w_down[e].rearrange("(k p) n -> p k n", p=128))
        shp = ((8, 256), (8, 256), (2, 1024))
        for q_ in range(3):
            K_, N_ = shp[q_]
            st_ = mstg[base + q_][:, 0:K_ * N_].rearrange("p (k n) -> p k n", k=K_)
            P.dma("sp", lambda e_, st_=st_, src=srcs[q_]: e_.dma_start(out=st_, in_=src), writes=[Bmstg[base + q_]])

    def moe_wcast_ops(e):
        base = (e % 2) * 3
        we = e % 2
        sg_ = mstg[base][:, 0:2048].rearrange("p (k n) -> p k n", k=8)
        su_ = mstg[base + 1][:, 0:2048].rearrange("p (k n) -> p k n", k=8)
        sd_ = mstg[base + 2][:, 0:2048].rearrange("p (k n) -> p k n", k=2)
        ops_ = []
        for kc in range(8):
            ops_.append(lambda kc=kc: P.op("act", lambda e_: e_.activation(out=Wgu[we][:, kc, 0:256], in_=sg_[:, kc, :], func=AF.Copy,
                                                                      scale=g2T[:, kc:kc + 1]),
                                           reads=[Bmstg[base], Bconst], writes=[BWg[we]]))
        for kc in range(8):
            ops_.append(lambda kc=kc: P.op("act", lambda e_: e_.activation(out=Wgu[we][:, kc, 256:512], in_=su_[:, kc, :], func=AF.Copy,
                                                                      scale=g2T[:, kc:kc + 1]),
                                           reads=[Bmstg[base + 1], Bconst], writes=[BWu[we]]))
        for fc in range(2):
            ops_.append(lambda fc=fc: P.op("act", lambda e_: e_.activation(out=Wd[we][:, fc, :], in_=sd_[:, fc, :], func=AF.Copy),
                                           reads=[Bmstg[base + 2]], writes=[BWd[we]]))
        return ops_

    def moe_wcast(e):
        for f_ in moe_wcast_ops(e):
            f_()

    def moe_A(u):
        e, t = u // 16, u % 16
        j = t // 4
        we = e % 2
        pau, Bpau = ps[u % 3], Bps[u % 3]
        for kc in range(8):
            P.op("pe", lambda e_, kc=kc: e_.matmul(pau[:], lhsT=h2T[:, kc, t * 128:(t + 1) * 128], rhs=Wgu[we][:, kc, :],
                                                   start=(kc == 0), stop=(kc == 7)),
                 reads=[Bh2T[j], BWg[we], BWu[we]], writes=[Bpau], inc=(kc == 7))

    def moe_B(u):
        e, t = u // 16, u % 16
        j, tt_ = t // 4, t % 4
        b = u % 2
        pau, Bpau = ps[u % 3], Bps[u % 3]
        ptr, Bptr = ps[3 + b], Bps[3 + b]
        P.op("act", lambda e_: e_.activation(out=sa[b][:], in_=pau[:, 0:256], func=AF.Silu), reads=[Bpau], writes=[Bsa[b]])
        P.op("dve", lambda e_: e_.scalar_tensor_tensor(out=hid[b][:], in0=sa[b][:], scalar=comb[:, t, e:e + 1], in1=pau[:, 256:512],
                                                       op0=ALU.mult, op1=ALU.mult), reads=[Bsa[b], Bpau, Bcomb[t]], writes=[Bhid[b]])
        ptb = ptr[:].bitcast(BF16).rearrange("p (f t) -> p f t", t=128)
        for fc in range(2):
            P.op("pe", lambda e_, fc=fc: e_.transpose(out=ptb[:, fc, :], in_=hid[b][:, fc * 128:(fc + 1) * 128], identity=ident[:]),
                 reads=[Bhid[b], Bconst], writes=[Bptr], inc=(fc == 1))
        hb = (e * 4 + j) % 2
        P.op("act", lambda e_: e_.activation(out=hidT[hb][:, :, tt_ * 128:(tt_ + 1) * 128], in_=ptb[:, 0:2, :], func=AF.Copy),
             reads=[Bptr], writes=[BhidT[hb]])

    dn_n = [0]

    def moe_C1(e, j, m):
        we = e % 2
        hb = (e * 4 + j) % 2
        tok = slice(j * 512, (j + 1) * 512)
        n = dn_n[0]
        dn_n[0] += 1
        pd, Bpd = ps[5 + n % 3], Bps[5 + n % 3]
        for fc in range(2):
            P.op("pe", lambda e_, fc=fc: e_.matmul(pd[:], lhsT=Wd[we][:, fc, m * 128:(m + 1) * 128], rhs=hidT[hb][:, fc, :],
                                                   start=(fc == 0), stop=(fc == 1)),
                 reads=[BWd[we], BhidT[hb]], writes=[Bpd], inc=(fc == 1))
        P.op("dve", lambda e_: e_.tensor_tensor(out=xT[:, m, tok], in0=pd[:], in1=xT[:, m, tok], op=ALU.add),
             reads=[Bpd, BxT[m][j]], writes=[BxT[m][j]])

    NU = NE * 16
    moe_wdma(0)
    moe_wcast(0)
    moe_wdma(1)
    moe_A(0)
    moe_A(1)
    pendC = []
    pendW = []
    for u in range(NU):
        e, t = u // 16, u % 16
        if u + 2 < NU:
            moe_A(u + 2)
        moe_B(u)
        k_ = 0
        while pendC and pendC[0][0] <= u and k_ < 2:
            _, e2_, j2_, m2_ = pendC.pop(0)
            moe_C1(e2_, j2_, m2_)
            k_ += 1
        if t % 4 == 3:
            for m_ in range(8):
                pendC.append((u + 1, e, t // 4, m_))
        if t == 5:
            assert not [c for c in pendC if c[1] < e]
            if e + 1 < NE:
                pendW = moe_wcast_ops(e + 1)
            if e + 2 < NE:
                moe_wdma(e + 2)
        for _ in range(2):
            if pendW:
                pendW.pop(0)()
    for _, e2_, j2_, m2_ in pendC:
        moe_C1(e2_, j2_, m2_)
    P.barrier()
    if stage == "4":
        return finish_debug([("xT", xT[:], [128, 8, NTOK], F32)])
    A.release(m_p4)
    A.release(C0 + 65536)

    stg5 = [A.alloc([128, 2048], F32) for _ in range(NSTG)]
    for i_ in range(NSTG):
        stg[i_] = stg5[i_]
    Wpg = A.alloc([128, 8, 1024], BF16)
    Wple = A.alloc([128, 2, 1024], BF16)
    h3T = [A.alloc([128, 8, 512], BF16) for _ in range(2)]
    Bh3T = [Buf("h3T0"), Buf("h3T1")]
    sqc3 = A.alloc([128, 8, 512], BF16)
    Bsqc3 = Buf("sqc3")
    Rt3 = A.alloc([128, 512], F32)
    BR3 = Buf("R3")
    pst = [A.alloc([128, 2, 512], F32) for _ in range(2)]
    Bpst = [Buf("pst0"), Buf("pst1")]
    ptb5 = [A.alloc([128, 2, 512], BF16) for _ in range(2)]
    Bptb5 = [Buf("ptb0"), Buf("ptb1")]
    sg = [A.alloc([128, 512], F32) for _ in range(2)]
    Bsg = [Buf("sg0"), Buf("sg1")]
    ost = [A.alloc([128, 512], F32) for _ in range(3)]
    Bost = [Buf("ost%d" % i) for i in range(3)]
    w_pg_v = w_pg.rearrange("(k p) n -> p k n", p=128)
    pT_v = pT_own.rearrange("(k p) t -> p k t", p=128)
    Bout = Buf("out")

    def p5_m(j, m, n):
        b = j % 2
        tok = slice(j * 512, (j + 1) * 512)
        ppg, Bppg = ps[2 + 2 * (n % 3)], Bps[2 + 2 * (n % 3)]
        ppe, Bppe = ps[3 + 2 * (n % 3)], Bps[3 + 2 * (n % 3)]
        for kc in range(8):
            P.op("pe", lambda e, kc=kc: e.matmul(ppg[:], lhsT=Wpg[:, kc, m * 128:(m + 1) * 128], rhs=h3T[b][:, kc, :], start=(kc == 0), stop=(kc == 7)),
                 reads=[BWpg[m // 2], Bh3T[b]], writes=[Bppg], inc=(kc == 7))
        for kc in range(2):
            P.op("pe", lambda e, kc=kc: e.matmul(ppe[:], lhsT=Wple[:, kc, m * 128:(m + 1) * 128], rhs=ptb5[b][:, kc, :], start=(kc == 0), stop=(kc == 1)),
                 reads=[BWple, Bptb5[b]], writes=[Bppe], inc=(kc == 1))
        sb_, o_ = n % 2, n % 3
        P.op("act", lambda e: e.activation(out=sg[sb_][:], in_=ppg[:], func=AF.Sigmoid), reads=[Bppg], writes=[Bsg[sb_]])
        P.op("dve", lambda e: e.tensor_tensor(out=sg[sb_][:], in0=sg[sb_][:], in1=ppe[:], op=ALU.mult), reads=[Bsg[sb_], Bppe], writes=[Bsg[sb_]])
        P.op("dve", lambda e: e.tensor_tensor(out=ost[o_][:], in0=sg[sb_][:], in1=xT[:, m, tok], op=ALU.add),
             reads=[Bsg[sb_], BxT[m][j]], writes=[Bost[o_]])
        P.dma("sp", lambda e: e.dma_start(out=outT[m * 128:(m + 1) * 128, tok], in_=ost[o_][:]), reads=[Bost[o_]], writes=[Buf("o")])

    def p5_pre(j):
        b = j % 2
        tok = slice(j * 512, (j + 1) * 512)
        P.dma("sp", lambda e: e.dma_start(out=pst[b][:], in_=pT_v[:, :, tok]), writes=[Bpst[b]])
        P.op("pool", lambda e: e.tensor_copy(out=ptb5[b][:], in_=pst[b][:]), reads=[Bpst[b]], writes=[Bptb5[b]])
        rnorm_chunk(xT[:, :, tok], [BxT[m][j] for m in range(8)], h3T[b][:], Bh3T[b], sqc3[:], Bsqc3, Rt3, BR3, ps[j % 2], Bps[j % 2], 512)

    p5_pre(0)
    BWpg = [load_w(Wpg[:, :, c * 256:(c + 1) * 256], w_pg_v[:, :, c * 256:(c + 1) * 256], 8, 256, g3T) for c in range(4)]
    BWple = load_w(Wple[:], w_ple.rearrange("(k p) n -> p k n", p=128), 2, 1024, None)
    for j in range(4):
        if j + 1 < 4:
            p5_pre(j + 1)
        for m in range(8):
            p5_m(j, m, j * 8 + m)
    P.barrier()
    P.emit()
    return nc


def make_masks():
    k = np.arange(128)[:, None]
    q = np.arange(512)[None, :]

    def diag(jb):
        return np.where((jb * 128 + k) <= q, 0.0, -30000.0).astype(np.float32)
    ones = np.zeros((128, 512), np.float32)
    zeros = np.full((128, 512), -30000.0, np.float32)
    E = [diag(0), diag(1), diag(2), diag(3), zeros, zeros, zeros, zeros]
    O = [ones, ones, ones, ones, diag(0), diag(1), diag(2), diag(3)]
    return np.stack(E, 0), np.stack(O, 0)


def prep_inputs(inp):
    x = np.asarray(inp["x"], np.float32)
    p = np.asarray(inp["p"], np.float32)[0]
    E, O = make_masks()
    shared = {
        "w_in": np.ascontiguousarray(inp["w_in"][0]),
        "w_oa": np.ascontiguousarray(inp["w_out_att"][0]),
        "w_oc": np.ascontiguousarray(inp["w_out_conv"][0]),
        "w_o": np.ascontiguousarray(inp["w_o"][0]),
        "w_r": np.ascontiguousarray(np.concatenate([inp["w_rg"][0], inp["w_re"][0]], axis=1)),
        "w_gate": np.ascontiguousarray(inp["w_gate"][0]),
        "w_up": np.ascontiguousarray(inp["w_up"][0]),
        "w_down": np.ascontiguousarray(inp["w_down"][0]),
        "w_pg": np.ascontiguousarray(inp["w_pg"][0]),
        "w_ple": np.ascontiguousarray(inp["w_ple"][0]),
        "g1T": np.ascontiguousarray(inp["attn_norm_g"][0].reshape(8, 128).T),
        "g2T": np.ascontiguousarray(inp["ffn_norm_g"][0].reshape(8, 128).T),
        "g3T": np.ascontiguousarray(inp["ple_norm_g"][0].reshape(8, 128).T),
        "bf_bc": np.ascontiguousarray(np.broadcast_to(inp["b_f"][0][None, :], (128, 8))),
        "gq_col": np.ascontiguousarray(inp["q_norm_g"][0].reshape(64, 1)),
        "gk_col": np.ascontiguousarray(inp["k_norm_g"][0].reshape(64, 1)),
        "convT": np.ascontiguousarray(inp["conv_w"][0].reshape(3, 4, 128).transpose(2, 1, 0)),
        "br_bc": np.ascontiguousarray(np.broadcast_to(
            np.concatenate([inp["b_rg"][0], inp["b_re"][0]])[None, :], (128, 20))),
    }
    shared = {k: np.asarray(v, np.float32) for k, v in shared.items()}
    maps = []
    for c in range(8):
        b, par = c // 2, c % 2
        chunks = CHUNKS[par]
        xb_T = np.ascontiguousarray(x[b].T)
        own_cols = np.concatenate([np.arange(ci * 512, (ci + 1) * 512) for ci in chunks])
        xh = np.zeros((D, 8), np.float32)
        for j, ci in enumerate(chunks):
            if ci > 0:
                xh[:, 2 * j:2 * j + 2] = xb_T[:, ci * 512 - 2:ci * 512]
        sel = np.zeros((16, 32), np.float32)
        for j, ci in enumerate(chunks):
            for tt in range(4):
                sel[4 * j + tt, 4 * ci + tt] = 1.0
        types = [(E, O)[ci % 2] for ci in chunks]
        mask2 = np.stack([types[0], types[1]], 0)
        assert np.array_equal(types[0], types[2]) and np.array_equal(types[1], types[3])
        m = dict(shared)
        m.update({
            "xT_all": xb_T,
            "xT_own": np.ascontiguousarray(xb_T[:, own_cols]),
            "xhT": xh,
            "pT_own": np.ascontiguousarray(p[b].T[:, own_cols]),
            "sel_own": np.ascontiguousarray(np.broadcast_to(sel[None], (128, 16, 32))),
            "mask2": np.ascontiguousarray(mask2.transpose(2, 0, 1, 3)).astype(ml_dtypes.bfloat16),
        })
        maps.append(m)
    return maps


_NC_CACHE = {}


def kernel(**inputs):
    maps = prep_inputs(inputs)
    if "nc" not in _NC_CACHE:
        _NC_CACHE["nc"] = build()
    nc = _NC_CACHE["nc"]
    res = run_bass_kernel_spmd(nc, maps, core_ids=list(range(8)))
    out = np.empty((4, S, D), np.float32)
    for c in range(8):
        b, par = c // 2, c % 2
        oT = np.asarray(res.results[c]["outT"])
        for j, ci in enumerate(CHUNKS[par]):
            out[b, ci * 512:(ci + 1) * 512, :] = oT[:, j * 512:(j + 1) * 512].T
    return out
```
